# Optimizing a Trainium2 kernel written in Bass

```python
import math
import jax, jax.numpy as jnp
from jax import lax
import numpy as np

D_MODEL = 2048
BATCH = 8
SEQ = 2048
DEPTH = 2

GRID_W = 64
CTX_LEN = 256
FN_WIDTH = D_MODEL // 4
FN_GROUPS = 4
HY_WIDTH = D_MODEL // 4
HG_WIDTH = D_MODEL // 2
HG_HEAD_DIM = 128
HG_HEADS = HG_WIDTH // HG_HEAD_DIM
HG_CHUNK = 64
HG_F_MIN = 1e-6
MIX_WIDTH = FN_WIDTH + HY_WIDTH + HG_WIDTH
HY_OFF = FN_WIDTH
HG_OFF = FN_WIDTH + 3 * HY_WIDTH
IN_WIDTH = FN_WIDTH + 3 * HY_WIDTH + 5 * HG_WIDTH
HY_EMB = 33
HY_BANDS = (HY_EMB - 1) // 2
HY_HIDDEN = 64
HY_SHORT = 3
HY_FAST_DECAY = 0.3
HY_SLOW_DECAY = 1.5
HY_TARGET = 1e-2
N_EXPERTS = 16
EC_CAPACITY = 2
EXPERT_FF = D_MODEL // 2
EPS = 1e-6

kernel_name = "hybrid_fnet_hyena_hgrn2_ec_moe_prefix_dit"

F32 = jnp.float32


def rmsnorm(x, g):
    x32 = x.astype(F32)
    y = x32 * lax.rsqrt(jnp.mean(x32 * x32, axis=-1, keepdims=True) + EPS)
    return (y * g.astype(F32)).astype(x.dtype)


def modulate(h, shift, scale):
    return h * (1 + scale) + shift


def fourier_mix(u, w_fn):
    B, L, _ = u.shape
    ug = u.astype(F32).reshape(B, L, FN_GROUPS, FN_WIDTH // FN_GROUPS)
    y = jnp.fft.fft2(ug, axes=(1, 3), norm="ortho").real.reshape(B, L, FN_WIDTH)
    return y.astype(u.dtype) @ w_fn


def short_conv(u, w, b):
    L = u.shape[1]
    half = HY_SHORT // 2
    up = jnp.pad(u, ((0, 0), (half, half), (0, 0)))
    return sum(up[:, j:j + L] * w[j] for j in range(HY_SHORT)) + b


def hyena_filters(L, w1, b1, w2, b2, w3, b3, w_out, freq):
    pos = jnp.arange(L, dtype=F32)
    t = (pos / max(L - 1, 1))[:, None]
    bands = jnp.linspace(1e-4, HY_BANDS - 1, HY_BANDS, dtype=F32)
    w = 2.0 * math.pi * pos[:, None] / L
    z = jnp.concatenate([t, jnp.cos(bands * w), -jnp.sin(bands * w)], axis=-1)
    hdn = jnp.sin(freq[0] * (z @ w1 + b1))
    hdn = jnp.sin(freq[1] * (hdn @ w2 + b2))
    hdn = jnp.sin(freq[2] * (hdn @ w3 + b3))
    h = (hdn @ w_out).astype(F32).reshape(L, 2, HY_WIDTH)
    max_decay = math.log(HY_TARGET) / HY_FAST_DECAY
    min_decay = math.log(HY_TARGET) / HY_SLOW_DECAY
    deltas = jnp.linspace(min_decay, max_decay, HY_WIDTH, dtype=F32)
    decay = jnp.exp(-t * jnp.abs(deltas))
    h = h * decay[:, None, :]
    return h[:, 0], h[:, 1]


def bidir_long_conv(u, h_f, h_b, d_bias):
    B, L, C = u.shape
    k = jnp.concatenate([h_f, jnp.zeros((1, C), F32), h_b[1:][::-1]], axis=0)
    U = jnp.fft.rfft(u.astype(F32), n=2 * L, axis=1)
    K = jnp.fft.rfft(k, axis=0)
    y = jnp.fft.irfft(U * K[None], n=2 * L, axis=1)[:, :L]
    return (y + u.astype(F32) * d_bias.astype(F32)).astype(u.dtype)


def hyena_mix(u3, conv_w, conv_b, w1, b1, w2, b2, w3, b3, w_out, freq, d_bias):
    L = u3.shape[1]
    uc = short_conv(u3, conv_w, conv_b)
    v = uc[..., :HY_WIDTH]
    x1 = uc[..., HY_WIDTH:2 * HY_WIDTH]
    x0 = uc[..., 2 * HY_WIDTH:]
    h_f, h_b = hyena_filters(L, w1, b1, w2, b2, w3, b3, w_out, freq)
    return x0 * bidir_long_conv(x1 * v, h_f, h_b, d_bias)


def hgrn2_chunk_scan(q, k, logf, v, s0):
    B, L, H, _ = q.shape
    DV = v.shape[-1]
    n = L // HG_CHUNK

    def chunks(a):
        return a.reshape(B, n, HG_CHUNK, H, a.shape[-1]).transpose(1, 0, 3, 2, 4)

    mask = jnp.tril(jnp.ones((HG_CHUNK, HG_CHUNK), dtype=bool))[:, :, None]

    def step(S, inp):
        qc, kc, gc, vc = inp
        b = jnp.cumsum(gc, axis=2)
        o_inter = jnp.einsum('bhtk,bhkv->bhtv', qc * jnp.exp(b), S)
        diff = b[:, :, :, None, :] - b[:, :, None, :, :]
        decay = jnp.where(mask, jnp.exp(jnp.where(mask, diff, 0.0)), 0.0)
        a = jnp.einsum('bhtk,bhsk,bhtsk->bhts', qc, kc, decay)
        o = o_inter + jnp.einsum('bhts,bhsv->bhtv', a, vc)
        b_last = b[:, :, -1]
        S = jnp.exp(b_last)[..., None] * S + jnp.einsum(
            'bhsk,bhsv->bhkv', kc * jnp.exp(b_last[:, :, None] - b), vc)
        return S, o

    S, o = lax.scan(step, s0, (chunks(q), chunks(k), chunks(logf), chunks(v)))
    o = o.transpose(1, 0, 3, 2, 4).reshape(B, L, H, DV)
    return o, S


def hgrn2_bidir(p_hg, lb_f, lb_b, s0_f, s0_b):
    B, L, _ = p_hg.shape
    W = HG_WIDTH
    zq, zf, zb, zi, g = (p_hg[..., j * W:(j + 1) * W] for j in range(5))

    def heads(a):
        return a.astype(F32).reshape(B, L, HG_HEADS, HG_HEAD_DIM)

    q = heads(jax.nn.silu(zq))
    v = heads(zi)

    def direction(z, lb, s0, flip):
        lb = lb.astype(F32)
        f = jnp.maximum(lb + (1.0 - lb) * jax.nn.sigmoid(z.astype(F32)), HG_F_MIN)
        logf = heads(jnp.log(f))
        k = heads(1.0 - f)
        qq, vv = q, v
        if flip:
            qq, k, logf, vv = qq[:, ::-1], k[:, ::-1], logf[:, ::-1], vv[:, ::-1]
        o, S = hgrn2_chunk_scan(qq, k, logf, vv, s0)
        if flip:
            o = o[:, ::-1]
        return o, S

    o_f, s_f = direction(zf, lb_f, s0_f, False)
    o_b, s_b = direction(zb, lb_b, s0_b, True)
    return o_f + o_b, g, s_f, s_b


def hgrn2_out(o, g, gain):
    B, L = o.shape[:2]
    y = o * lax.rsqrt(jnp.mean(o * o, axis=-1, keepdims=True) + EPS)
    y = y * gain.astype(F32).reshape(HG_HEADS, HG_HEAD_DIM)
    return (y.reshape(B, L, HG_WIDTH) * jax.nn.silu(g.astype(F32))).astype(g.dtype)


def ec_moe(h, w_router, w_gate, w_up, w_down):
    B, L, D = h.shape
    cap = EC_CAPACITY * L // N_EXPERTS
    aff = jax.nn.softmax((h @ w_router).astype(F32), axis=-1)
    gates, idx = lax.top_k(jnp.swapaxes(aff, 1, 2), cap)
    xs = jax.vmap(lambda hb, ib: hb[ib])(h, idx)
    hid = jax.nn.silu(jnp.einsum('becd,edf->becf', xs, w_gate)) * jnp.einsum('becd,edf->becf', xs, w_up)
    ys = jnp.einsum('becf,efd->becd', hid, w_down) * gates[..., None].astype(h.dtype)
    return jax.vmap(lambda ib, yb: jnp.zeros((L, D), yb.dtype).at[ib.reshape(-1)].add(yb.reshape(-1, D)))(idx, ys)


def setup_inputs(seed: int = 0) -> dict:
    key = jax.random.key(seed)
    ks = jax.random.split(key, 32)
    nrm = lambda k, shape, s: jax.random.normal(k, shape, F32) * s
    D = D_MODEL
    return {
        "x": nrm(ks[0], (BATCH, SEQ, D), 1.0),
        "c": nrm(ks[1], (BATCH, D), 1.0),
        "ctx": nrm(ks[2], (BATCH, CTX_LEN, D), 1.0),
        "c_ctx": nrm(ks[3], (D,), 1.0),
        "norm_mix_g": 1.0 + nrm(ks[4], (DEPTH, D), 0.05),
        "norm_ffn_g": 1.0 + nrm(ks[5], (DEPTH, D), 0.05),
        "final_norm_g": 1.0 + nrm(ks[6], (D,), 0.05),
        "w_mod": nrm(ks[7], (DEPTH, D, 6 * D), D ** -0.5),
        "b_mod": nrm(ks[8], (DEPTH, 6 * D), 0.02),
        "w_in": nrm(ks[9], (DEPTH, D, IN_WIDTH), D ** -0.5),
        "w_out": nrm(ks[10], (DEPTH, MIX_WIDTH, D), MIX_WIDTH ** -0.5),
        "w_fnet": nrm(ks[11], (DEPTH, FN_WIDTH, FN_WIDTH), FN_WIDTH ** -0.5),
        "hy_conv_w": nrm(ks[12], (DEPTH, HY_SHORT, 3 * HY_WIDTH), HY_SHORT ** -0.5),
        "hy_conv_b": nrm(ks[13], (DEPTH, 3 * HY_WIDTH), 0.02),
        "hy_w1": nrm(ks[14], (DEPTH, HY_EMB, HY_HIDDEN), HY_EMB ** -0.5),
        "hy_b1": nrm(ks[15], (DEPTH, HY_HIDDEN), 0.02),
        "hy_w2": nrm(ks[16], (DEPTH, HY_HIDDEN, HY_HIDDEN), HY_HIDDEN ** -0.5),
        "hy_b2": nrm(ks[17], (DEPTH, HY_HIDDEN), 0.02),
        "hy_w3": nrm(ks[18], (DEPTH, HY_HIDDEN, HY_HIDDEN), HY_HIDDEN ** -0.5),
        "hy_b3": nrm(ks[19], (DEPTH, HY_HIDDEN), 0.02),
        "hy_w_out": nrm(ks[20], (DEPTH, HY_HIDDEN, 2 * HY_WIDTH), 0.1 * HY_HIDDEN ** -0.5),
        "hy_freq": 1.0 + nrm(ks[21], (DEPTH, 3, HY_HIDDEN), 0.1),
        "hy_bias": nrm(ks[22], (DEPTH, HY_WIDTH), 0.5),
        "hg_lb": nrm(ks[23], (DEPTH, 2, HG_WIDTH), 0.5),
        "hg_norm_g": 1.0 + nrm(ks[24], (DEPTH, HG_WIDTH), 0.05),
        "w_router": nrm(ks[25], (DEPTH, D, N_EXPERTS), D ** -0.5),
        "w_gate": nrm(ks[26], (DEPTH, N_EXPERTS, D, EXPERT_FF), D ** -0.5),
        "w_up": nrm(ks[27], (DEPTH, N_EXPERTS, D, EXPERT_FF), D ** -0.5),
        "w_down": nrm(ks[28], (DEPTH, N_EXPERTS, EXPERT_FF, D), EXPERT_FF ** -0.5),
    }


def reference(x, c, ctx, c_ctx, norm_mix_g, norm_ffn_g, final_norm_g, w_mod, b_mod, w_in, w_out, w_fnet,
              hy_conv_w, hy_conv_b, hy_w1, hy_b1, hy_w2, hy_b2, hy_w3, hy_b3, hy_w_out, hy_freq, hy_bias,
              hg_lb, hg_norm_g, w_router, w_gate, w_up, w_down):
    B = x.shape[0]
    p = jax.nn.softmax(hg_lb.astype(F32), axis=0)
    lbs = jnp.cumsum(p, axis=0) - p[0:1]
    xc = ctx
    for l in range(DEPTH):
        last = l == DEPTH - 1
        mod = (jax.nn.silu(c) @ w_mod[l] + b_mod[l])[:, None, :]
        mod_c = jax.nn.silu(c_ctx) @ w_mod[l] + b_mod[l]
        sh1, sc1, g1, sh2, sc2, g2 = jnp.split(mod, 6, axis=-1)
        csh1, csc1, cg1, csh2, csc2, cg2 = jnp.split(mod_c, 6, axis=-1)
        hy_args = (hy_conv_w[l], hy_conv_b[l], hy_w1[l], hy_b1[l], hy_w2[l], hy_b2[l],
                   hy_w3[l], hy_b3[l], hy_w_out[l], hy_freq[l], hy_bias[l])

        def mixers_out(h, o_hg, g_hg):
            y_fn = fourier_mix(h[..., :FN_WIDTH], w_fnet[l])
            y_hy = hyena_mix(h[..., HY_OFF:HG_OFF], *hy_args)
            y_hg = hgrn2_out(o_hg, g_hg, hg_norm_g[l])
            return jnp.concatenate([y_fn, y_hy, y_hg], axis=-1) @ w_out[l]

        h = modulate(rmsnorm(x, norm_mix_g[l]), sh1, sc1) @ w_in[l]
        hc = modulate(rmsnorm(xc, norm_mix_g[l]), csh1, csc1) @ w_in[l]
        zero = jnp.zeros((B, HG_HEADS, HG_HEAD_DIM, HG_HEAD_DIM), F32)
        o_c, g_c, s_f, s_b = hgrn2_bidir(hc[..., HG_OFF:], lbs[l, 0], lbs[l, 1], zero, zero)
        o_x, g_x, _, _ = hgrn2_bidir(h[..., HG_OFF:], lbs[l, 0], lbs[l, 1], s_f, s_b)
        x = x + g1 * mixers_out(h, o_x, g_x)
        if not last:
            xc = xc + cg1 * mixers_out(hc, o_c, g_c)
        x = x + g2 * ec_moe(modulate(rmsnorm(x, norm_ffn_g[l]), sh2, sc2),
                            w_router[l], w_gate[l], w_up[l], w_down[l])
        if not last:
            xc = xc + cg2 * ec_moe(modulate(rmsnorm(xc, norm_ffn_g[l]), csh2, csc2),
                                   w_router[l], w_gate[l], w_up[l], w_down[l])
    return rmsnorm(x, final_norm_g)
```

```python
import math
from contextlib import ExitStack

import numpy as np
import ml_dtypes

import concourse.bass as bass
import concourse.mybir as mybir
from concourse.bass_utils import run_bass_kernel_spmd

F32 = mybir.dt.float32
BF16 = mybir.dt.bfloat16
U32 = mybir.dt.uint32
AF = mybir.ActivationFunctionType
ALU = mybir.AluOpType
AX = mybir.AxisListType

D = 2048
SEQ = 2048
CTX = 256
DEPTH = 2
NE = 16
FF = 1024
INW = 7168
HY_OFF = 512
HG_OFF = 2048
HGW = 1024
NH = 8
EPS = 1e-6
PI = math.pi
TRACE_SITES = False
DBG = {}


class Prog:
    ENGS = ("pe", "act", "dve", "pool", "sp")
    HND = {"pe": "tensor", "act": "scalar", "dve": "vector", "pool": "gpsimd", "sp": "sync"}

    def __init__(self, nc, n_dma_sems=12):
        self.nc = nc
        self.ops = []
        self.n_dma_sems = n_dma_sems
        self.bar = set()
        self.since_bar = []
        self.trace_sites = False
        self.names = {}

    def op(self, eng, fn, reads=(), writes=(), dma=False):
        if len(self.ops) >= DBG.get("max_ops", 10 ** 9):
            return -1
        site = None
        if self.trace_sites:
            import sys as _sys
            f = _sys._getframe(1)
            site = []
            while f is not None and len(site) < 4:
                if f.f_code.co_name not in ("MM", "TR", "ACT", "TS", "TT", "STT", "CP", "MS", "DMA"):
                    site.append(f"{f.f_code.co_name}:{f.f_lineno}")
                f = f.f_back
        self.ops.append(dict(eng=eng, fn=fn, reads=list(reads), writes=list(writes), dma=dma,
                             bar=frozenset(self.bar), site=site))
        self.since_bar.append(len(self.ops) - 1)
        return len(self.ops) - 1

    def barrier(self):
        last = {}
        dm = []
        for i in self.since_bar:
            o = self.ops[i]
            if o["dma"]:
                dm.append(i)
            else:
                last[o["eng"]] = i
        new = set(last.values()) | set(dm)
        for i in self.bar:
            o = self.ops[i]
            if not o["dma"] and o["eng"] not in last:
                new.add(i)
        self.bar = new
        self.since_bar = []

    def emit(self, final_wait_eng="sp"):
        nc = self.nc
        ops = self.ops
        last_w = {}
        readers = {}
        for i, o in enumerate(ops):
            deps = set(o["bar"])
            for r in o["reads"]:
                if r in last_w:
                    deps.add(last_w[r])
            for w in o["writes"]:
                if w in last_w:
                    deps.add(last_w[w])
                for j in readers.get(w, ()):
                    deps.add(j)
            deps.discard(i)
            if o["eng"] == "pe":
                deps = {j for j in deps if ops[j]["eng"] != "pe"}
            latest = {}
            keep = set()
            for j in deps:
                if ops[j]["dma"]:
                    keep.add(j)
                else:
                    e2 = ops[j]["eng"]
                    if j > latest.get(e2, -1):
                        latest[e2] = j
            deps = keep | set(latest.values())
            o["deps"] = deps
            for r in o["reads"]:
                readers.setdefault(r, []).append(i)
            for w in o["writes"]:
                last_w[w] = i
                readers[w] = []
        needed = set()
        for o in ops:
            needed |= o["deps"]
        tail = [i for i, o in enumerate(ops) if o["dma"] and i not in needed]
        needed |= set(tail)
        cnt = {e: 0 for e in self.ENGS}
        dcnt = {e: 0 for e in self.ENGS}
        for i, o in enumerate(ops):
            e = o["eng"]
            if o["dma"]:
                k = dcnt[e]
                dcnt[e] += 1
                o["sem"] = ("d", e, k % self.n_dma_sems)
                o["val"] = 16 * (k // self.n_dma_sems + 1)
                o["prev_val"] = 16 * (k // self.n_dma_sems)
            elif i in needed:
                cnt[e] += 1
                o["sem"] = ("c", e)
                o["val"] = cnt[e]
            else:
                o["sem"] = None
        st = ExitStack()
        sems = {}
        for e in self.ENGS:
            if cnt[e]:
                sems[("c", e)] = st.enter_context(nc.semaphore(f"s_{e}"))
            for k in range(min(dcnt[e], self.n_dma_sems)):
                sems[("d", e, k)] = st.enter_context(nc.semaphore(f"d_{e}{k}"))
        block = st.enter_context(nc.Block())

        def make(e):
            def body(eh):
                waited = {}

                def wait(semkey, val):
                    if waited.get(semkey, 0) >= val:
                        return
                    eh.wait_ge(sems[semkey], val)
                    waited[semkey] = val

                for i, o in enumerate(ops):
                    if o["eng"] != e:
                        continue
                    for j in sorted(o["deps"]):
                        wait(ops[j]["sem"], ops[j]["val"])
                    if o["dma"] and o["prev_val"] > 0:
                        wait(o["sem"], o["prev_val"])
                    ins = o["fn"](eh)
                    if o["site"] is not None:
                        self.names[ins.ins.name] = o["site"]
                    if o["sem"] is not None:
                        ins.then_inc(sems[o["sem"]], 16 if o["dma"] else 1)
                if e == final_wait_eng:
                    for i in tail:
                        wait(ops[i]["sem"], ops[i]["val"])
            return body

        for e in self.ENGS:
            if any(o["eng"] == e for o in ops) or e == final_wait_eng:
                getattr(block, self.HND[e])(make(e))
        st.close()
        return dict(cnt=cnt, dcnt=dcnt, nops=len(ops))


def _bf(a):
    return np.ascontiguousarray(a.astype(np.float32)).astype(ml_dtypes.bfloat16)


def host_consts():
    c = {}
    c["ident_bf"] = _bf(np.eye(128))
    c["ident_f"] = np.eye(128, dtype=np.float32)
    c["ones_f"] = np.ones((128, 128), np.float32)
    k = np.arange(128, dtype=np.float64)
    ang = 2 * np.pi * ((k[:, None] * k[None, :]) % 128) / 128
    c["fc_cs"] = _bf(np.concatenate([np.cos(ang), np.sin(ang)], axis=1) / math.sqrt(128))
    for L in (SEQ, CTX):
        TT = L // 128
        FW = min(256, L)
        t = np.arange(L, dtype=np.int64)
        f = np.arange(L, dtype=np.int64)
        ang = 2 * np.pi * ((t[:, None] * f[None, :]) % L) / L
        cs = np.stack([np.cos(ang), -np.sin(ang)], 0) / math.sqrt(L)
        tab = cs.reshape(2, TT, 128, L // FW, FW).transpose(3, 2, 0, 1, 4)
        c[f"fn_tab{L}"] = _bf(tab)
        N4 = 4 * L
        ang = 2 * np.pi * (((2 * f[None, :] + 1) * t[:, None]) % N4) / N4
        cs = np.stack([np.cos(ang), -np.sin(ang)], 0)
        FT = L // 128
        tab = cs.reshape(2, TT, 128, FT, 128).transpose(3, 2, 0, 1, 4)
        c[f"hy_fwd{L}"] = _bf(tab)
        IW = min(256, L)
        csi = cs.transpose(0, 2, 1) / L
        tab = csi.reshape(2, FT, 128, L // IW, IW).transpose(3, 2, 0, 1, 4)
        c[f"hy_inv{L}"] = _bf(tab)
        pos = np.arange(L, dtype=np.float32)
        tt_ = (pos / max(L - 1, 1))[:, None]
        bands = np.linspace(1e-4, 15, 16, dtype=np.float32)
        w = (2.0 * np.float32(math.pi) * pos[:, None] / np.float32(L)).astype(np.float32)
        z = np.concatenate([tt_, np.cos(bands * w), -np.sin(bands * w)], axis=-1).astype(np.float32)
        c[f"hy_z{L}"] = np.ascontiguousarray(z.T)
        c[f"hy_tpos{L}"] = np.ascontiguousarray(np.broadcast_to(tt_[:, 0][None, :], (128, L))).astype(np.float32)
        r = np.ones((128, L), np.float32)
        r[:, ::64] = 0.0
        c[f"hg_rst{L}"] = _bf(r)
    max_decay = math.log(1e-2) / 0.3
    min_decay = math.log(1e-2) / 1.5
    deltas = np.linspace(min_decay, max_decay, 512, dtype=np.float32)
    c["hy_ndelta"] = np.ascontiguousarray((-np.abs(deltas)).reshape(4, 128).T).astype(np.float32)
    s = np.arange(64)
    same = (s[:, None] // 16) == (s[None, :] // 16)
    mf = ((s[:, None] <= s[None, :]) & same).astype(np.float32)
    mb = ((s[:, None] >= s[None, :]) & same).astype(np.float32)
    c["hg_maskD"] = _bf(np.stack([np.concatenate([mf, mf], 0), np.concatenate([mb, mb], 0)], 1))
    ng = np.zeros((2, 3, 64), np.float32)
    for vi in range(3):
        ng[0, vi, s >= 16 * (vi + 1)] = -30000.0
        ng[1, vi, s < 16 * (vi + 1)] = -30000.0
    c["hg_negm"] = np.ascontiguousarray(np.broadcast_to(ng[None], (128, 2, 3, 64))).astype(np.float32)
    c["iota_t"] = np.ascontiguousarray(np.broadcast_to(np.arange(SEQ, dtype=np.float32)[None, :], (128, SEQ)))
    c["pidx"] = (np.arange(16)[None, :] * 128 + np.arange(128)[:, None]).astype(np.float32)
    sm = np.zeros((16, 16, 128), np.float32)
    for e in range(16):
        sm[e, e, :] = 1.0
    c["selmat"] = sm
    return c


_CONSTS = None


def pmaj(v, n):
    v = np.asarray(v, np.float32)
    lead = v.shape[:-1]
    a = v.reshape(lead + (n, 128))
    a = np.moveaxis(a, -1, 0)
    return np.ascontiguousarray(a)


class Builder:
    def __init__(self, nc, debug=()):
        self.nc = nc
        self.P = Prog(nc)
        self.debug = set(debug)
        self.inputs = {}
        self.st = ExitStack()
        self.PS = self.st.enter_context(nc.psum_tensor("PS", [128, 8, 512], F32))
        self.psi = 0
        self.uid = 0

    def din(self, name, shape, dt=F32):
        t = self.nc.dram_tensor(name, list(shape), dt, kind="ExternalInput").ap()
        self.inputs[name] = t
        return t

    def dscr(self, name, shape, dt):
        kind = "ExternalOutput" if name in self.debug else "Internal"
        return self.nc.dram_tensor(name, list(shape), dt, kind=kind).ap()

    def sb(self, st, name, shape, dt):
        self.uid += 1
        return st.enter_context(self.nc.sbuf_tensor(f"{name}_{self.uid}", list(shape), dt))

    def bank(self):
        b = self.psi
        self.psi = (self.psi + 1) % 8
        return b

    def MM(self, out, lhsT, rhs, start, stop, r, w):
        self.P.op("pe", lambda e: e.matmul(out, lhsT, rhs, start=start, stop=stop), r, w)

    def TR(self, out, in_, ident, r, w):
        self.P.op("pe", lambda e: e.transpose(out, in_, ident), r, w)

    def ACT(self, out, in_, func, r, w, bias=None, scale=None):
        kw = {}
        if bias is not None:
            kw["bias"] = bias
        if scale is not None:
            kw["scale"] = scale
        self.P.op("act", lambda e: e.activation(out=out, in_=in_, func=func, **kw), r, w)

    def TS(self, eng, out, in0, s1, s2, op0, op1, r, w):
        if op1 is None:
            self.P.op(eng, lambda e: e.tensor_scalar(out, in0, s1, None, op0), r, w)
        else:
            self.P.op(eng, lambda e: e.tensor_scalar(out, in0, s1, s2, op0, op1), r, w)

    def TT(self, eng, out, in0, in1, op, r, w):
        self.P.op(eng, lambda e: e.tensor_tensor(out, in0, in1, op), r, w)

    def STT(self, out, in0, scalar, in1, op0, op1, r, w):
        self.P.op("dve", lambda e: e.scalar_tensor_tensor(out, in0, scalar, in1, op0, op1), r, w)

    def CP(self, eng, out, in_, r, w):
        if eng == "act":
            self.P.op("act", lambda e: e.activation(out=out, in_=in_, func=AF.Copy), r, w)
        else:
            self.P.op(eng, lambda e: e.tensor_copy(out, in_), r, w)

    def MS(self, eng, ap, val, w):
        self.P.op(eng, lambda e: e.memset(ap, val), (), w)

    def DMA(self, eng, out, in_, r, w):
        self.P.op(eng, lambda e: e.dma_start(out=out, in_=in_), r, w, dma=True)


def tiles(L, w=512):
    w = min(w, L)
    return [(i * w, w) for i in range(L // w)]


def build(stage=99, debug=(), skip=()):
    nc = bass.Bass("TRN2", target_bir_lowering=False)
    B = Builder(nc, debug)
    P = B.P
    P.trace_sites = TRACE_SITES
    consts = host_consts()

    xT_in = B.din("xT", [D, SEQ])
    cxT_in = B.din("ctxT", [D, CTX])
    cT_in = B.din("cT", [128, 16, 2])
    gmix_in = B.din("gmix", [128, DEPTH, 16])
    gffn_in = B.din("gffn", [128, DEPTH, 16])
    gfin_in = B.din("gfin", [128, 16])
    bmod_in = B.din("bmod", [128, DEPTH, 96])
    w_mod = B.din("w_mod", [DEPTH, D, 6 * D])
    w_in = B.din("w_in", [DEPTH, D, INW])
    w_out = B.din("w_out", [DEPTH, D, D])
    w_fnet = B.din("w_fnet", [DEPTH, 512, 512])
    hycw_in = B.din("hycw", [128, DEPTH, 3, 12])
    hycb_in = B.din("hycb", [128, DEPTH, 12])
    hyw1_in = B.din("hy_w1", [DEPTH, 33, 64])
    hyw2_in = B.din("hy_w2", [DEPTH, 64, 64])
    hyw3_in = B.din("hy_w3", [DEPTH, 64, 64])
    hywo_in = B.din("hy_w_out", [DEPTH, 64, 1024])
    hyb_in = B.din("hyb", [64, DEPTH, 3])
    hyfr_in = B.din("hyfr", [64, DEPTH, 3])
    hydb_in = B.din("hydb", [128, DEPTH, 4])
    hglb_in = B.din("hglb", [128, DEPTH, 2, 8])
    hgg_in = B.din("hgg", [128, DEPTH, 8])
    wr_in = B.din("w_router", [DEPTH, D, NE])
    if DBG.get("small"):
        wg_in = B.din("w_gate", [1, 1, 128, FF])
        wu_in = B.din("w_up", [1, 1, 128, FF])
        wd_in = B.din("w_down", [1, 1, 128, D])
    else:
        wg_in = B.din("w_gate", [DEPTH, NE, D, FF])
        wu_in = B.din("w_up", [DEPTH, NE, D, FF])
        wd_in = B.din("w_down", [DEPTH, NE, FF, D])
    cd = {}
    for k, v in consts.items():
        dt = BF16 if v.dtype == ml_dtypes.bfloat16 else F32
        cd[k] = B.din(k, v.shape, dt)

    out_d = nc.dram_tensor("outT", [D, SEQ], F32, kind="ExternalOutput").ap()
    xres = {SEQ: B.dscr("xres", [D, SEQ], F32), CTX: B.dscr("xcres", [D, CTX], F32)}
    cat_d = {SEQ: B.dscr("cat_x", [D, SEQ], BF16), CTX: B.dscr("cat_c", [D, CTX], BF16)}
    hyk_d = {L: B.dscr(f"hyk{L}", [L // 128, 128, 2, 512], F32) for L in (SEQ, CTX)}
    ys_d = B.dscr("ys_d", [16, 128, 2 * NE, 128], BF16)

    G = B.st
    ident_bf = B.sb(G, "ident_bf", [128, 128], BF16)
    ident_f = B.sb(G, "ident_f", [128, 128], F32)
    ones_f = B.sb(G, "ones_f", [128, 128], F32)
    modT = B.sb(G, "modT", [128, 96, 2], F32)
    A1 = B.sb(G, "A1", [128, 16, 2], F32)
    A2 = B.sb(G, "A2", [128, 16, 2], F32)
    gmix = B.sb(G, "gmix", [128, DEPTH, 16], F32)
    gffn = B.sb(G, "gffn", [128, DEPTH, 16], F32)
    gfin = B.sb(G, "gfin", [128, 16], F32)
    S32 = B.sb(G, "S32", [128, 16, 128], F32)
    hglb = B.sb(G, "hglb", [128, DEPTH, 2, 8], F32)
    lbs = B.sb(G, "lbs", [128, 2, 8], F32)
    oml = B.sb(G, "oml", [128, 2, 8], F32)
    hgg = B.sb(G, "hgg", [128, DEPTH, 8], F32)
    epsc = B.sb(G, "epsc", [128, 1], F32)
    B.MS("dve", epsc[:], EPS, ["epsc"])
    B.DMA("sp", ident_bf[:], cd["ident_bf"], (), ["ident_bf"])
    B.DMA("sp", ident_f[:], cd["ident_f"], (), ["ident_f"])
    B.DMA("sp", ones_f[:], cd["ones_f"], (), ["ones_f"])
    B.DMA("sp", gmix[:], gmix_in, (), ["gmix"])
    B.DMA("sp", gffn[:], gffn_in, (), ["gffn"])
    B.DMA("sp", gfin[:], gfin_in, (), ["gfin"])
    B.DMA("sp", hglb[:], hglb_in, (), ["hglb"])
    B.DMA("sp", hgg[:], hgg_in, (), ["hgg"])

    PS = B.PS

    def psk(b):
        return f"ps{b}"

    with ExitStack() as st:
        buf = B.sb(st, "cpy", [128, 2, SEQ], F32)
        for L, src in ((SEQ, xT_in), (CTX, cxT_in)):
            for dc in range(16):
                b_ = dc % 2
                B.DMA("sp", buf[:, b_, :L], src[dc * 128:(dc + 1) * 128, :], (), [f"cpy{b_}"])
                B.DMA("sp", xres[L][dc * 128:(dc + 1) * 128, :], buf[:, b_, :L], [f"cpy{b_}"], [f"xres{L}.{dc}"])
        e0 = B.sb(st, "e0", [128, 2, 8], F32)
        e1 = B.sb(st, "e1", [128, 2, 8], F32)
        B.ACT(e0[:], hglb[:, 0], AF.Exp, ["hglb"], ["e0"])
        B.ACT(e1[:], hglb[:, 1], AF.Exp, ["hglb"], ["e1"])
        B.TT("dve", e0[:], e0[:], e1[:], ALU.add, ["e0", "e1"], ["e0"])
        B.P.op("dve", lambda e: e.reciprocal(e0[:], e0[:]), ["e0"], ["e0"])
        B.TT("dve", lbs[:], e1[:], e0[:], ALU.mult, ["e0", "e1"], ["lbs"])
        B.TS("dve", oml[:], lbs[:], -1.0, 1.0, ALU.mult, ALU.add, ["lbs"], ["oml"])
    P.barrier()

    def phase_mod(l):
        with ExitStack() as st:
            cT = B.sb(st, "cT", [128, 16, 2], F32)
            sc = B.sb(st, "sc", [128, 16, 2], BF16)
            bmod = B.sb(st, "bmod", [128, 96], F32)
            wb = B.sb(st, "wmod", [128, 2, 16, 512], BF16)
            B.DMA("sp", cT[:], cT_in, (), ["cT"])
            B.DMA("sp", bmod[:], bmod_in[:, l, :], (), ["bmod"])
            B.ACT(sc[:], cT[:], AF.Silu, ["cT"], ["sc"])
            wv = w_mod[l].rearrange("(dc p) n -> p dc n", p=128)
            bk = B.bank()
            for blk in range(24):
                bb = blk % 2
                B.DMA("pool", wb[:, bb], wv[:, :, blk * 512:(blk + 1) * 512], (), [f"wmod{bb}"])
                for j in range(4):
                    oc = blk * 4 + j
                    for dc in range(16):
                        B.MM(PS[:, bk, oc * 2:oc * 2 + 2], wb[:, bb, dc, j * 128:(j + 1) * 128], sc[:, dc, :],
                             dc == 0, dc == 15, [f"wmod{bb}", "sc"], [psk(bk)])
            B.TT("dve", modT[:], PS[:, bk, 0:192].rearrange("p (a b) -> p a b", b=2),
                 bmod[:].unsqueeze(2).to_broadcast([128, 96, 2]), ALU.add, [psk(bk), "bmod"], ["modT"])
            for (Ax, g, off, nm) in ((A1, gmix, 16, "A1"), (A2, gffn, 64, "A2")):
                B.TS("dve", Ax[:], modT[:, off:off + 16, :], 1.0, None, ALU.add, None, ["modT"], [nm])
                B.TT("dve", Ax[:], Ax[:], g[:, l, :].unsqueeze(2).to_broadcast([128, 16, 2]), ALU.mult,
                     [nm, "gmix", "gffn"], [nm])
        P.barrier()

    def normmod(st, L, s, Ax, Anm, shoff, xn, xnk, router=None, nbuf=2):
        xld = B.sb(st, "xld", [128, 2, L], F32)
        sq_ = B.sb(st, "sq", [128, nbuf, L], F32)
        rstd = B.sb(st, "rstd", [128, L], F32)
        tmp_ = B.sb(st, "nmtmp", [128, nbuf, L], F32)

        class _V:
            def __init__(self, t, n):
                self.t, self.n = t, n

            def __getitem__(self, key):
                p, b, f = key
                return self.t[p, b % self.n, f]
        sq = _V(sq_, nbuf)
        tmp = _V(tmp_, nbuf)
        tl = tiles(L)
        for dc in range(16):
            b_ = dc % 2
            B.DMA("sp", xld[:, b_, :], xres[L][dc * 128:(dc + 1) * 128, :], [f"xres{L}.{dc}"], [f"xld{b_}"])
            B.ACT(sq[:, b_, :], xld[:, b_, :], AF.Square, [f"xld{b_}"], [f"sq{b_ % nbuf}"])
            for i, (t0, tw) in enumerate(tl):
                B.MM(PS[:, 4 + i, :tw], ones_f[:], sq[:, b_, t0:t0 + tw], dc == 0, dc == 15,
                     [f"sq{b_ % nbuf}", "ones_f"], [psk(4 + i)])
        for i, (t0, tw) in enumerate(tl):
            B.ACT(rstd[:, t0:t0 + tw], PS[:, 4 + i, :tw], AF.Ln, [psk(4 + i)], ["rstd"], bias=epsc[:, 0:1], scale=1.0 / D)
        B.ACT(rstd[:], rstd[:], AF.Exp, ["rstd"], ["rstd"], scale=-0.5)
        for dc in range(16):
            b_ = dc % 2
            B.DMA("sp", xld[:, b_, :], xres[L][dc * 128:(dc + 1) * 128, :], [f"xres{L}.{dc}"], [f"xld{b_}"])
            B.TT("dve", tmp[:, b_, :], xld[:, b_, :], rstd[:], ALU.mult, [f"xld{b_}", "rstd"], [f"nmtmp{b_ % nbuf}"])
            if router is None:
                B.ACT(xn[:, dc, :L], tmp[:, b_, :], AF.Identity, [f"nmtmp{b_ % nbuf}", Anm, "modT"], [f"{xnk}.{dc}"],
                      bias=modT[:, shoff + dc, s:s + 1], scale=Ax[:, dc, s:s + 1])
            else:
                wr32, lb = router
                B.ACT(sq[:, b_, :], tmp[:, b_, :], AF.Identity, [f"nmtmp{b_ % nbuf}", Anm, "modT"], [f"sq{b_ % nbuf}"],
                      bias=modT[:, shoff + dc, s:s + 1], scale=Ax[:, dc, s:s + 1])
                B.CP("pool", xn[:, dc, :L], sq[:, b_, :], [f"sq{b_ % nbuf}"], [f"{xnk}.{dc}"])
                for tt in range(L // 128):
                    B.MM(PS[:, lb, tt * 16:(tt + 1) * 16], sq[:, b_, tt * 128:(tt + 1) * 128], wr32[:, dc, :],
                         dc == 0 and tt == 0, dc == 15 and tt == L // 128 - 1, [f"sq{b_ % nbuf}", "wr32"], [psk(lb)])

    def load_w(wb, wbk, wv, c0, ncols):
        B.DMA("pool", wb[:, :, :ncols], wv[:, :, c0:c0 + ncols], (), [wbk])

    def proj_fm(L, xn, xnk, wb, wbk, ncols, evac, kch=16):
        for m in range(ncols // 128):
            for (t0, tw) in tiles(L):
                bk = B.bank()
                for dc in range(kch):
                    B.MM(PS[:, bk, :tw], wb[:, dc, m * 128:(m + 1) * 128], xn[:, dc, t0:t0 + tw], dc == 0, dc == kch - 1,
                         [wbk, f"{xnk}.{dc}"], [psk(bk)])
                evac(m, t0, tw, PS[:, bk, :tw], psk(bk))

    def phase_fourier(l, L, xn, xnk, wv):
        TT_ = L // 128
        FW = min(256, L)
        with ExitStack() as st:
            wb = B.sb(st, "wfn", [128, 16, 512], BF16)
            hT = B.sb(st, "hTfn", [128, 4, L], BF16)
            AB = B.sb(st, "AB", [128, TT_, 4, 256], BF16)
            cs = B.sb(st, "fc_cs", [128, 256], BF16)
            tab = B.sb(st, "fntab", [128, 2, 2, TT_, FW], BF16)
            yT = B.sb(st, "yTfn", [128, 4, L], BF16)
            wf = B.sb(st, "wfnet", [128, 4, 512], BF16)
            og = B.sb(st, "catfn", [128, 4, L], BF16)
            B.DMA("sp", cs[:], cd["fc_cs"], (), ["fc_cs"])
            B.DMA("pool", wf[:], w_fnet[l].rearrange("(kc p) n -> p kc n", p=128), (), ["wfnet"])
            load_w(wb, "wfn", wv, 0, 512)

            def ev(m, t0, tw, ps, pk):
                B.CP("act", hT[:, m, t0:t0 + tw], ps, [pk], [f"hTfn.{m}"])
            proj_fm(L, xn, xnk, wb, "wfn", 512, ev)
            for tt in range(TT_):
                bk = B.bank()
                for g in range(4):
                    B.MM(PS[:, bk, g * 128:(g + 1) * 128], hT[:, g, tt * 128:(tt + 1) * 128], cs[:, 0:128], True, True,
                         [f"hTfn.{g}", "fc_cs"], [psk(bk)])
                bk2 = B.bank()
                for g in range(4):
                    B.MM(PS[:, bk2, g * 128:(g + 1) * 128], hT[:, g, tt * 128:(tt + 1) * 128], cs[:, 128:256], True, True,
                         [f"hTfn.{g}", "fc_cs"], [psk(bk2)])
                B.CP("act", AB[:, tt, :, 0:128], PS[:, bk, :].rearrange("p (g c) -> p g c", c=128), [psk(bk)], [f"AB.{tt}"])
                B.CP("dve", AB[:, tt, :, 128:256], PS[:, bk2, :].rearrange("p (g c) -> p g c", c=128), [psk(bk2)], [f"AB.{tt}"])
            for i in range(L // FW):
                tb = i % 2
                B.DMA("sp", tab[:, tb], cd[f"fn_tab{L}"][i], (), [f"fntab{tb}"])
                for g in range(4):
                    bk = B.bank()
                    n = 0
                    for tt in range(TT_):
                        for a in range(2):
                            B.MM(PS[:, bk, :FW], AB[:, tt, g, a * 128:(a + 1) * 128], tab[:, tb, a, tt, :], n == 0,
                                 n == 2 * TT_ - 1, [f"AB.{tt}", f"fntab{tb}"], [psk(bk)])
                            n += 1
                    B.CP("act", yT[:, g, i * FW:(i + 1) * FW], PS[:, bk, :FW], [psk(bk)], [f"yTfn.{g}"])

            def ev2(m, t0, tw, ps, pk):
                B.CP("dve", og[:, m, t0:t0 + tw], ps, [pk], [f"catfn.{m}"])
            proj_fm(L, yT, "yTfn", wf, "wfnet", 512, ev2, kch=4)
            for m in range(4):
                B.DMA("sp", cat_d[L][m * 128:(m + 1) * 128, :], og[:, m, :], [f"catfn.{m}"], [f"cat{L}.{m}"])
        P.barrier()

    def wrap_sin(a, m, out, ps, pk, fr, fb, np_, w, outk):
        B.TS("dve", a[:np_, :w], ps, fr, fb, ALU.mult, ALU.add, [pk, "hyfrb"], ["ws_a"])
        B.TS("dve", m[:np_, :w], a[:np_, :w], PI, -2 * PI, ALU.is_gt, ALU.mult, ["ws_a"], ["ws_m"])
        B.TT("dve", a[:np_, :w], a[:np_, :w], m[:np_, :w], ALU.add, ["ws_a", "ws_m"], ["ws_a"])
        B.TS("dve", m[:np_, :w], a[:np_, :w], -PI, 2 * PI, ALU.is_lt, ALU.mult, ["ws_a"], ["ws_m"])
        B.TT("dve", a[:np_, :w], a[:np_, :w], m[:np_, :w], ALU.add, ["ws_a", "ws_m"], ["ws_a"])
        B.ACT(out, a[:np_, :w], AF.Sin, ["ws_a"], [outk])

    def fwd_dft(st, L, src, srck, consume):
        TT_ = L // 128
        tab = B.sb(st, "hyfwd", [128, 2, 2, TT_, 128], BF16)
        for ft in range(L // 128):
            tb = ft % 2
            B.DMA("sp", tab[:, tb], cd[f"hy_fwd{L}"][ft], (), [f"hyfwd{tb}"])
            bre, bim = B.bank(), B.bank()
            for a, bk in ((0, bre), (1, bim)):
                for tt in range(TT_):
                    B.MM(PS[:, bk, :], tab[:, tb, a, tt, :], src[:, tt, :], tt == 0, tt == TT_ - 1,
                         [f"hyfwd{tb}", srck], [psk(bk)])
            consume(ft, bre, bim)

    def to_tm(L, srcT, srck, dst, dstk):
        for tt in range(L // 128):
            bk = B.bank()
            pv = PS[:, bk, :].bitcast(BF16)
            for j in range(4):
                B.TR(pv[:, j * 128:(j + 1) * 128], srcT[:, j, tt * 128:(tt + 1) * 128], ident_bf[:],
                     [srck, "ident_bf"], [psk(bk)])
            B.CP("act" if tt % 2 else "dve", dst[:, tt, :], pv[:, 0:512], [psk(bk)], [dstk])

    def to_tm1(L, src, srck, dst, dstk, j):
        TT_ = L // 128
        for g0 in range(0, TT_, 4):
            n = min(4, TT_ - g0)
            bk = B.bank()
            pv = PS[:, bk, :].bitcast(BF16)
            for q in range(n):
                B.TR(pv[:, q * 128:(q + 1) * 128], src[:, (g0 + q) * 128:(g0 + q + 1) * 128], ident_bf[:], [srck, "ident_bf"], [psk(bk)])
            B.CP("act" if (g0 // 4) % 2 else "dve", dst[:, g0:g0 + n, j * 128:(j + 1) * 128],
                 pv[:, 0:n * 128].rearrange("p (q c) -> p q c", c=128), [psk(bk)], [dstk])

    def phase_hyfilter(l, L):
        TT_ = L // 128
        with ExitStack() as st:
            h3 = B.sb(st, "hyh1", [64, L], F32)
            with ExitStack() as st1:
                zT = B.sb(st1, "hyz", [33, L], F32)
                w1 = B.sb(st1, "hyw1", [33, 64], F32)
                w2 = B.sb(st1, "hyw2", [64, 64], F32)
                w3 = B.sb(st1, "hyw3", [64, 64], F32)
                fr = B.sb(st1, "hyfr", [64, 3], F32)
                fb = B.sb(st1, "hyfb", [64, 3], F32)
                h2 = B.sb(st1, "hyh2", [64, L], F32)
                wsa = B.sb(st1, "ws_a", [64, 512], F32)
                wsm = B.sb(st1, "ws_m", [64, 512], F32)
                h1 = h3
                B.DMA("sp", zT[:], cd[f"hy_z{L}"], (), ["hyz"])
                B.DMA("sp", w1[:], hyw1_in[l], (), ["hyw"])
                B.DMA("sp", w2[:], hyw2_in[l], (), ["hyw"])
                B.DMA("sp", w3[:], hyw3_in[l], (), ["hyw"])
                B.DMA("sp", fr[:], hyfr_in[:, l, :], (), ["hyfrb"])
                B.DMA("sp", fb[:], hyb_in[:, l, :], (), ["hyfrb"])
                B.TT("dve", fb[:], fb[:], fr[:], ALU.mult, ["hyfrb"], ["hyfrb"])
                srcs = [(zT, 33, w1, "hyz"), (h1, 64, w2, "hyh1"), (h2, 64, w3, "hyh2")]
                dsts = [(h1, "hyh1"), (h2, "hyh2"), (h1, "hyh1")]
                for li in range(3):
                    src, kp, wt, sk = srcs[li]
                    dst, dk = dsts[li]
                    for (t0, tw) in tiles(L):
                        bk = B.bank()
                        B.MM(PS[:64, bk, :tw], wt[:kp, :], src[:kp, t0:t0 + tw], True, True, ["hyw", sk], [psk(bk)])
                        wrap_sin(wsa, wsm, dst[:, t0:t0 + tw], PS[:64, bk, :tw], psk(bk), fr[:, li:li + 1], fb[:, li:li + 1], 64, tw, dk)
            P.barrier()
            wo = B.sb(st, "hywo", [64, 1024], F32)
            tpos = B.sb(st, "tpos", [128, L], F32)
            nd = B.sb(st, "ndelta", [128, 4], F32)
            dec = B.sb(st, "decay", [128, L], F32)
            hf = B.sb(st, "hf", [128, L], F32)
            hb = B.sb(st, "hb", [128, L], F32)
            hs = B.sb(st, "hs", [128, L], BF16)
            hd = B.sb(st, "hd", [128, L], BF16)
            hs_tm = B.sb(st, "hs_tm", [128, TT_, 512], BF16)
            hd_tm = B.sb(st, "hd_tm", [128, TT_, 512], BF16)
            kst = B.sb(st, "kst", [128, 2, 2, 512], F32)
            B.DMA("sp", wo[:], hywo_in[l], (), ["hywo"])
            B.DMA("sp", tpos[:], cd[f"hy_tpos{L}"], (), ["tpos"])
            B.DMA("sp", nd[:], cd["hy_ndelta"], (), ["ndelta"])
            for j in range(4):
                B.ACT(dec[:], tpos[:], AF.Exp, ["tpos", "ndelta"], ["decay"], scale=nd[:, j:j + 1])
                for (t0, tw) in tiles(L):
                    b1, b2 = B.bank(), B.bank()
                    B.MM(PS[:, b1, :tw], wo[:, j * 128:(j + 1) * 128], h3[:, t0:t0 + tw], True, True, ["hywo", "hyh1"], [psk(b1)])
                    B.MM(PS[:, b2, :tw], wo[:, 512 + j * 128:512 + (j + 1) * 128], h3[:, t0:t0 + tw], True, True, ["hywo", "hyh1"], [psk(b2)])
                    B.TT("dve", hf[:, t0:t0 + tw], PS[:, b1, :tw], dec[:, t0:t0 + tw], ALU.mult, [psk(b1), "decay"], ["hf"])
                    B.TT("dve", hb[:, t0:t0 + tw], PS[:, b2, :tw], dec[:, t0:t0 + tw], ALU.mult, [psk(b2), "decay"], ["hb"])
                B.MS("dve", hb[:, 0:1], 0.0, ["hb"])
                B.TT("dve", hs[:], hf[:], hb[:], ALU.add, ["hf", "hb"], ["hs"])
                B.TT("dve", hd[:], hf[:], hb[:], ALU.subtract, ["hf", "hb"], ["hd"])
                to_tm1(L, hs, "hs", hs_tm, "hs_tm", j)
                to_tm1(L, hd, "hd", hd_tm, "hd_tm", j)
            tab = B.sb(st, "hyfwdK", [128, 2, 2, TT_, 128], BF16)
            for ft in range(L // 128):
                tb = ft % 2
                B.DMA("sp", tab[:, tb], cd[f"hy_fwd{L}"][ft], (), [f"hyfwdK{tb}"])
                bre, bim = B.bank(), B.bank()
                for tt in range(TT_):
                    B.MM(PS[:, bre, :], tab[:, tb, 0, tt, :], hs_tm[:, tt, :], tt == 0, tt == TT_ - 1, [f"hyfwdK{tb}", "hs_tm"], [psk(bre)])
                for tt in range(TT_):
                    B.MM(PS[:, bim, :], tab[:, tb, 1, tt, :], hd_tm[:, tt, :], tt == 0, tt == TT_ - 1, [f"hyfwdK{tb}", "hd_tm"], [psk(bim)])
                B.CP("act", kst[:, tb, 0, :], PS[:, bre, :], [psk(bre)], [f"kst{tb}"])
                B.CP("dve", kst[:, tb, 1, :], PS[:, bim, :], [psk(bim)], [f"kst{tb}"])
                B.DMA("act", hyk_d[L][ft], kst[:, tb], [f"kst{tb}"], [f"hyk{L}.{ft}"])
        P.barrier()

    def phase_hyena(l, L, xn, xnk, wv):
        TT_ = L // 128
        IW = min(256, L)
        with ExitStack() as st:
            cw = B.sb(st, "hycw", [128, 3, 12], F32)
            cb = B.sb(st, "hycb", [128, 12], F32)
            db = B.sb(st, "hydb", [128, 4], F32)
            zT = B.sb(st, "zT", [128, 4, L], BF16)
            x0T = B.sb(st, "x0T", [128, 4, L], BF16)
            B.DMA("sp", cw[:], hycw_in[:, l], (), ["hycw"])
            B.DMA("sp", cb[:], hycb_in[:, l], (), ["hycw"])
            B.DMA("sp", db[:], hydb_in[:, l], (), ["hycw"])
            with ExitStack() as st1:
                wb = B.sb(st1, "why", [128, 2, 16, 384], BF16)
                hp = B.sb(st1, "hpad", [128, 3, L + 2], BF16)
                acc = B.sb(st1, "hyacc", [128, 2, L], F32)
                B.MS("pool", hp[:, :, 0:1], 0.0, ["hpad0", "hpad1", "hpad2"])
                B.MS("pool", hp[:, :, L + 1:L + 2], 0.0, ["hpad0", "hpad1", "hpad2"])
                for j in range(4):
                    wbb = j % 2
                    for q in range(3):
                        c0 = HY_OFF + q * 512 + j * 128
                        B.DMA("pool", wb[:, wbb, :, q * 128:(q + 1) * 128], wv[:, :, c0:c0 + 128], (), [f"why{wbb}"])

                    def ev(m, t0, tw, ps, pk):
                        B.CP("act", hp[:, m, 1 + t0:1 + t0 + tw], ps, [pk], [f"hpad{m}"])
                    proj_fm(L, xn, xnk, wb[:, wbb], f"why{wbb}", 384, ev)
                    for q in range(3):
                        ch = q * 4 + j
                        a_ = acc[:, q % 2, :]
                        ak = f"hyacc{q % 2}"
                        B.ACT(a_, hp[:, q, 1:L + 1], AF.Identity, [f"hpad{q}", "hycw"], [ak], bias=cb[:, ch:ch + 1], scale=cw[:, 1, ch:ch + 1])
                        B.STT(a_, hp[:, q, 0:L], cw[:, 0, ch:ch + 1], a_, ALU.mult, ALU.add, [f"hpad{q}", "hycw", ak], [ak])
                        if q < 2:
                            B.STT(a_, hp[:, q, 2:L + 2], cw[:, 2, ch:ch + 1], a_, ALU.mult, ALU.add, [f"hpad{q}", "hycw", ak], [ak])
                            if q == 1:
                                B.TT("dve", zT[:, j, :], acc[:, 0, :], acc[:, 1, :], ALU.mult, ["hyacc0", "hyacc1"], ["zT"])
                        else:
                            B.STT(x0T[:, j, :], hp[:, q, 2:L + 2], cw[:, 2, ch:ch + 1], a_, ALU.mult, ALU.add, [f"hpad{q}", "hycw", ak], ["x0T"])
            P.barrier()
            Pre = B.sb(st, "Pre", [128, TT_, 512], BF16)
            Pim = B.sb(st, "Pim", [128, TT_, 512], BF16)
            with ExitStack() as st2:
                z_tm = B.sb(st2, "z_tm", [128, TT_, 512], BF16)
                kk = B.sb(st2, "kk", [128, 2, 2, 512], F32)
                t1 = B.sb(st2, "hyt1", [128, 512], F32)
                t2 = B.sb(st2, "hyt2", [128, 512], F32)
                to_tm(L, zT, "zT", z_tm, "z_tm")

                def consume(ft, bre, bim):
                    kb_ = ft % 2
                    B.DMA("sp", kk[:, kb_], hyk_d[L][ft], [f"hyk{L}.{ft}"], [f"kk{kb_}"])
                    B.TT("dve", t1[:], PS[:, bre, :], kk[:, kb_, 0, :], ALU.mult, [psk(bre), f"kk{kb_}"], ["hyt1"])
                    B.TT("dve", t2[:], PS[:, bim, :], kk[:, kb_, 1, :], ALU.mult, [psk(bim), f"kk{kb_}"], ["hyt2"])
                    B.TT("pool", Pre[:, ft, :], t1[:], t2[:], ALU.subtract, ["hyt1", "hyt2"], ["Pre"])
                    B.TT("dve", t1[:], PS[:, bre, :], kk[:, kb_, 1, :], ALU.mult, [psk(bre), f"kk{kb_}"], ["hyt1"])
                    B.TT("dve", t2[:], PS[:, bim, :], kk[:, kb_, 0, :], ALU.mult, [psk(bim), f"kk{kb_}"], ["hyt2"])
                    B.TT("pool", Pim[:, ft, :], t1[:], t2[:], ALU.add, ["hyt1", "hyt2"], ["Pim"])
                fwd_dft(st2, L, z_tm, "z_tm", consume)
            P.barrier()
            with ExitStack() as st3:
                itab = B.sb(st3, "hyinv", [128, 2, 2, TT_, IW], BF16)
                yo = B.sb(st3, "hyyo", [128, 2, IW], F32)
                og = B.sb(st3, "cathy", [128, 4, L], BF16)
                for it in range(L // IW):
                    tb = it % 2
                    B.DMA("sp", itab[:, tb], cd[f"hy_inv{L}"][it], (), [f"hyinv{tb}"])
                    for j in range(4):
                        bk = B.bank()
                        n = 0
                        for ft in range(TT_):
                            for a, Pm, Pk in ((0, Pre, "Pre"), (1, Pim, "Pim")):
                                B.MM(PS[:, bk, :IW], Pm[:, ft, j * 128:(j + 1) * 128], itab[:, tb, a, ft, :], n == 0, n == 2 * TT_ - 1,
                                     [Pk, f"hyinv{tb}"], [psk(bk)])
                                n += 1
                        yb = (it * 4 + j) % 2
                        B.STT(yo[:, yb, :], zT[:, j, it * IW:(it + 1) * IW], db[:, j:j + 1], PS[:, bk, :IW], ALU.mult, ALU.add,
                              ["zT", "hycw", psk(bk)], [f"hyyo{yb}"])
                        B.TT("pool", og[:, j, it * IW:(it + 1) * IW], yo[:, yb, :], x0T[:, j, it * IW:(it + 1) * IW], ALU.mult,
                             [f"hyyo{yb}", "x0T"], [f"cathy.{j}"])
                for j in range(4):
                    B.DMA("sp", cat_d[L][512 + j * 128:512 + (j + 1) * 128, :], og[:, j, :], [f"cathy.{j}"], [f"cat{L}.{4 + j}"])
        P.barrier()

    def phase_hgrn(l, L, xn, xnk, wv, need_out, first):
        NHALF = 2 if L >= 1024 else 1
        Lh = L // NHALF
        TT_ = L // 128
        TTh = Lh // 128
        NCH = Lh // 64
        with ExitStack() as st:
            wj = B.sb(st, "whg", [128, 2, 16, 128], BF16)
            rst = B.sb(st, "rst", [128, Lh], BF16)
            mskD = B.sb(st, "hgmaskD", [128, 2, 64], BF16)
            negm = B.sb(st, "hgnegm", [128, 2, 3, 64], F32)
            T0 = B.sb(st, "T0", [128, Lh], F32)
            T1 = B.sb(st, "T1", [128, Lh], F32)
            T2 = B.sb(st, "T2", [128, Lh], F32)
            qT2 = B.sb(st, "qT", [128, 2, L], BF16)
            v_tm2 = B.sb(st, "v_tm", [128, 2, TT_, 128], BF16)
            oT = B.sb(st, "oT", [128, L], F32)
            sg = B.sb(st, "sgT", [128, Lh], BF16)
            og = B.sb(st, "cathg", [128, L], BF16)
            kT2 = B.sb(st, "kT", [128, 2, Lh], BF16)
            qb2 = B.sb(st, "qb", [128, 2, Lh], BF16)
            kb2 = B.sb(st, "kb", [128, 2, Lh], BF16)
            qB2 = B.sb(st, "qB", [128, 2, Lh], BF16)
            kdT2 = B.sb(st, "kdT", [128, 2, Lh], BF16)
            kbi2 = B.sb(st, "kbi", [128, 2, 3, Lh], BF16)
            qbi2 = B.sb(st, "qbi", [128, 2, NCH * 48], BF16)
            eb2 = B.sb(st, "eb", [128, 2, NCH], F32)
            kd_tm = B.sb(st, "kd_tm", [128, TTh, 2, 128], BF16)
            Sall = B.sb(st, "Sall", [128, NCH, 128], BF16)
            HC = min(16, NCH)
            Sch = B.sb(st, "Sch", [128, HC + 1, 128], F32)
            Am = B.sb(st, "Am", [128, 2, 4, 64], BF16)
            B.DMA("sp", rst[:], cd[f"hg_rst{L}"][:, 0:Lh], (), ["rst"])
            B.DMA("sp", mskD[:], cd["hg_maskD"], (), ["hgmask"])
            B.DMA("sp", negm[:], cd["hg_negm"], (), ["hgmask"])
            if first:
                B.MS("dve", S32[:], 0.0, ["S32"])
            B.MS("pool", kd_tm[:], 0.0, ["kd_tm"])
            B.MS("pool", Am[:], 0.0, ["Am0", "Am1"])
            v64 = lambda ap: ap.rearrange("p (n c) -> p n c", c=64)
            v16 = lambda ap: ap.rearrange("p (n c) -> p n c", c=16)
            v416 = lambda ap: ap.rearrange("p (n i c) -> p n i c", i=4, c=16)
            wcnt = [0]

            def loadj(h, j):
                wb_ = wcnt[0] % 2
                wcnt[0] += 1
                c0 = HG_OFF + j * HGW + h * 128
                B.DMA("pool", wj[:, wb_], wv[:, :, c0:c0 + 128], (), [f"whg{wb_}"])
                return wj[:, wb_], f"whg{wb_}"

            def proj_win(wb, wbk, t_lo, t_hi, evac):
                for (t0, tw) in tiles(t_hi - t_lo):
                    bk = B.bank()
                    for dc in range(16):
                        B.MM(PS[:, bk, :tw], wb[:, dc, :], xn[:, dc, t_lo + t0:t_lo + t0 + tw], dc == 0, dc == 15,
                             [wbk, f"{xnk}.{dc}"], [psk(bk)])
                    evac(t0, tw, PS[:, bk, :tw], psk(bk))

            def head_start(h):
                hp = h % 2
                wb, wbk = loadj(h, 3)
                for tt in range(TT_):
                    bk = B.bank()
                    for dc in range(16):
                        B.MM(PS[:, bk, :128], xn[:, dc, tt * 128:(tt + 1) * 128], wb[:, dc, :], dc == 0, dc == 15,
                             [f"{xnk}.{dc}", wbk], [psk(bk)])
                    B.CP("act", v_tm2[:, hp, tt, :], PS[:, bk, :128], [psk(bk)], [f"v_tm{hp}"])
                if need_out:
                    wb, wbk = loadj(h, 0)

                    def evq(t0, tw, ps, pk):
                        B.ACT(qT2[:, hp, t0:t0 + tw], ps, AF.Silu, [pk], [f"qT{hp}"])
                    proj_win(wb, wbk, 0, L, evq)

            def prep(item, par):
                h, dr, hf = item
                hp = h % 2
                tb = hf * Lh
                kT, qb, kb, qB, kdT = kT2[:, par, :], qb2[:, par, :], kb2[:, par, :], qB2[:, par, :], kdT2[:, par, :]
                kTk, qbk, kbk, qBk, kdTk, kbik, qbik, ebk = (f"{n}{par}" for n in ("kT", "qb", "kb", "qB", "kdT", "kbi", "qbi", "eb"))
                qTh = qT2[:, hp, tb:tb + Lh]
                qTk = f"qT{hp}"
                wb, wbk = loadj(h, 1 + dr)

                def evf(t0, tw, ps, pk):
                    B.ACT(T0[:, t0:t0 + tw], ps, AF.Sigmoid, [pk], ["T0"])
                proj_win(wb, wbk, tb, tb + Lh, evf)
                if l > 0:
                    B.TS("dve", T0[:], T0[:], oml[:, dr, h:h + 1], lbs[:, dr, h:h + 1], ALU.mult, ALU.add, ["T0", "oml", "lbs"], ["T0"])
                B.TS("dve", T0[:], T0[:], 1e-6, None, ALU.max, None, ["T0"], ["T0"])
                B.TS("pool", kT, T0[:], -1.0, 1.0, ALU.mult, ALU.add, ["T0"], [kTk])
                B.ACT(T0[:], T0[:], AF.Ln, ["T0"], ["T0"])
                P.op("dve", lambda e: e.tensor_tensor_scan(T1[:], rst[:], T0[:], 0.0, ALU.mult, ALU.add),
                     ["rst", "T0"], ["T1"])
                if dr == 1:
                    B.TT("dve", v64(T2[:]), v64(T1[:])[:, :, 63:64].to_broadcast([128, NCH, 64]), v64(T1[:]), ALU.subtract, ["T1"], ["T2"])
                    B.TT("dve", T1[:], T2[:], T0[:], ALU.add, ["T2", "T0"], ["T1"])
                bend = v64(T1[:])[:, :, 63:64] if dr == 0 else v64(T1[:])[:, :, 0:1]
                B.ACT(eb2[:, par, :].unsqueeze(2), bend, AF.Exp, ["T1"], [ebk])
                B.TT("dve", v64(T2[:]), bend.to_broadcast([128, NCH, 64]), v64(T1[:]), ALU.subtract, ["T1"], ["T2"])
                B.ACT(T2[:], T2[:], AF.Exp, ["T2"], ["T2"])
                B.TT("pool", kdT, kT, T2[:], ALU.mult, [kTk, "T2"], [kdTk])
                if need_out:
                    B.TT("dve", v16(T0[:]), v16(T1[:]), v16(T1[:])[:, :, 8:9].to_broadcast([128, Lh // 16, 16]), ALU.subtract, ["T1"], ["T0"])
                    B.ACT(T2[:], T0[:], AF.Exp, ["T0"], ["T2"])
                    B.TT("dve", qb, qTh, T2[:], ALU.mult, [qTk, "T2"], [qbk])
                    B.ACT(T2[:], T0[:], AF.Exp, ["T0"], ["T2"], scale=-1.0)
                    B.TT("pool", kb, kT, T2[:], ALU.mult, [kTk, "T2"], [kbk])
                    B.ACT(T2[:], T1[:], AF.Exp, ["T1"], ["T2"])
                    B.TT("dve", qB, qTh, T2[:], ALU.mult, [qTk, "T2"], [qBk])
                    if dr == 0:
                        rsel = v64(T1[:])[:, :, 15:48:16]
                        i0 = 1
                    else:
                        rsel = v64(T1[:])[:, :, 16:64:16]
                        i0 = 0
                    tq = T0[:, 0:NCH * 48].rearrange("p (n i c) -> p n i c", i=3, c=16)
                    B.TT("dve", tq, v416(T1[:])[:, :, i0:i0 + 3, :], rsel.unsqueeze(3).to_broadcast([128, NCH, 3, 16]), ALU.subtract, ["T1"], ["T0"])
                    B.ACT(T0[:, 0:NCH * 48], T0[:, 0:NCH * 48], AF.Exp, ["T0"], ["T0"])
                    B.TT("dve", qbi2[:, par, :].rearrange("p (n i c) -> p n i c", i=3, c=16), v416(qTh)[:, :, i0:i0 + 3, :], tq, ALU.mult,
                         [qTk, "T0"], [qbik])
                    for vi in range(3):
                        Tx, Txk = (T2, "T2") if vi % 2 == 0 else (T0, "T0")
                        B.TT("dve", v64(Tx[:]), rsel[:, :, vi:vi + 1].to_broadcast([128, NCH, 64]), v64(T1[:]), ALU.subtract, ["T1"], [Txk])
                        B.TT("dve", v64(Tx[:]), v64(Tx[:]), negm[:, dr, vi:vi + 1, :].to_broadcast([128, NCH, 64]), ALU.min, [Txk, "hgmask"], [Txk])
                        B.ACT(Tx[:], Tx[:], AF.Exp, [Txk], [Txk])
                        B.TT("pool", kbi2[:, par, vi, :], kT, Tx[:], ALU.mult, [kTk, Txk], [kbik])

            def chunkwork(item, par):
                h, dr, hf = item
                hp = h % 2
                tb = hf * Lh
                ttb = hf * TTh
                sidx = h * 2 + dr
                kTk, qbk, kbk, qBk, kdTk, kbik, qbik, ebk = (f"{n}{par}" for n in ("kT", "qb", "kb", "qB", "kdT", "kbi", "qbi", "eb"))
                qb, kb, qB, kdT = qb2[:, par, :], kb2[:, par, :], qB2[:, par, :], kdT2[:, par, :]
                qbi = qbi2[:, par, :].rearrange("p (n i c) -> p n i c", i=3, c=16)
                eb = eb2[:, par, :]
                vk = f"v_tm{hp}"
                i0 = 1 if dr == 0 else 0
                for tt in range(TTh):
                    bk = B.bank()
                    pv = PS[:, bk, :].bitcast(BF16)
                    B.TR(pv[:, 0:128], kdT[:, tt * 128:(tt + 1) * 128], ident_bf[:], [kdTk, "ident_bf"], [psk(bk)])
                    B.CP("act", kd_tm[0:64, tt, 0, :], pv[0:64, 0:128], [psk(bk)], ["kd_tm"])
                    B.CP("act", kd_tm[64:128, tt, 1, :], pv[64:128, 0:128], [psk(bk)], ["kd_tm"])
                order = list(range(NCH)) if dr == 0 else list(range(NCH - 1, -1, -1))
                pos_of = {n: p for p, n in enumerate(order)}
                for h0 in range(0, NCH, HC):
                    B.CP("act", Sch[:, 0, :], S32[:, sidx, :], ["S32"], ["Sch"])
                    for gi in range(h0, h0 + HC, 4):
                        grp = order[gi:gi + 4]
                        bk = B.bank()
                        for q, n in enumerate(grp):
                            B.MM(PS[:, bk, q * 128:(q + 1) * 128], kd_tm[:, n // 2, n % 2, :], v_tm2[:, hp, ttb + n // 2, :], True, True,
                                 ["kd_tm", vk], [psk(bk)])
                        for q, n in enumerate(grp):
                            p = gi + q - h0
                            B.STT(Sch[:, p + 1, :], Sch[:, p, :], eb[:, n:n + 1], PS[:, bk, q * 128:(q + 1) * 128], ALU.mult, ALU.add,
                                  ["Sch", ebk, psk(bk)], ["Sch"])
                    if need_out:
                        B.CP("act", Sall[:, h0:h0 + HC, :], Sch[:, 0:HC, :], ["Sch"], ["Sall"])
                    B.CP("pool", S32[:, sidx, :], Sch[:, HC, :], ["Sch"], ["S32"])
                if not need_out:
                    return
                c0 = 16 if dr == 0 else 0
                for gi in range(0, NCH, 4):
                    grp = list(range(gi, gi + 4))
                    ab = (gi // 4) % 2
                    bkD, bkF = B.bank(), B.bank()
                    for q, n in enumerate(grp):
                        ne = n - (n % 2)
                        B.MM(PS[:, bkD, q * 64:(q + 1) * 64], kb[:, ne * 64:(ne + 2) * 64], qb[:, n * 64:(n + 1) * 64], True, True,
                             [kbk, qbk], [psk(bkD)])
                        for vi in range(3):
                            i = vi + i0
                            B.MM(PS[:, bkF, q * 64 + i * 16:q * 64 + (i + 1) * 16], kbi2[:, par, vi, ne * 64:(ne + 2) * 64], qbi[:, n, vi, :], True, True,
                                 [kbik, qbik], [psk(bkF)])
                    for half in range(2):
                        pb = half * 64
                        pd = PS[pb:pb + 64, bkD, 0:256].rearrange("p (q c) -> p q c", c=64)[:, half::2, :]
                        pf = PS[pb:pb + 64, bkF, 0:256].rearrange("p (q c) -> p q c", c=64)[:, half::2, c0:c0 + 48]
                        am = Am[pb:pb + 64, ab, half::2, :]
                        B.TT("dve", am, pd, mskD[pb:pb + 64, dr:dr + 1, :].to_broadcast([64, 2, 64]), ALU.mult,
                             [psk(bkD), "hgmask"], [f"Am{ab}"])
                        B.TT("dve", am[:, :, c0:c0 + 48], am[:, :, c0:c0 + 48], pf, ALU.add, [psk(bkF), f"Am{ab}"], [f"Am{ab}"])
                    bkO = B.bank()
                    for q, n in enumerate(grp):
                        B.MM(PS[:, bkO, q * 64:(q + 1) * 64], Sall[:, pos_of[n], :], qB[:, n * 64:(n + 1) * 64], True, False,
                             ["Sall", qBk], [psk(bkO)])
                        B.MM(PS[:, bkO, q * 64:(q + 1) * 64], v_tm2[:, hp, ttb + n // 2, :], Am[:, ab, q, :], False, True,
                             [vk, f"Am{ab}"], [psk(bkO)])
                    osl = oT[:, tb + gi * 64:tb + (gi + 4) * 64]
                    if dr == 0:
                        B.CP("act", osl, PS[:, bkO, 0:256], [psk(bkO)], ["oT"])
                    else:
                        B.TT("dve", osl, osl, PS[:, bkO, 0:256], ALU.add, [psk(bkO), "oT"], ["oT"])

            def head_end(h):
                wb, wbk = loadj(h, 4)
                for hf in range(NHALF):
                    tb = hf * Lh

                    def evg(t0, tw, ps, pk):
                        B.ACT(sg[:, t0:t0 + tw], ps, AF.Silu, [pk], ["sgT"])
                    proj_win(wb, wbk, tb, tb + Lh, evg)
                    B.ACT(T0[:], oT[:, tb:tb + Lh], AF.Square, ["oT"], ["T0"])
                    for (t0, tw) in tiles(Lh):
                        bk = B.bank()
                        B.MM(PS[:, bk, :tw], ones_f[:], T0[:, t0:t0 + tw], True, True, ["T0", "ones_f"], [psk(bk)])
                        B.ACT(T1[:, t0:t0 + tw], PS[:, bk, :tw], AF.Ln, [psk(bk)], ["T1"], bias=epsc[:, 0:1], scale=1.0 / 128)
                    B.ACT(T1[:], T1[:], AF.Exp, ["T1"], ["T1"], scale=-0.5)
                    B.TT("dve", T1[:], T1[:], oT[:, tb:tb + Lh], ALU.mult, ["T1", "oT"], ["T1"])
                    B.STT(og[:, tb:tb + Lh], T1[:], hgg[:, l, h:h + 1], sg[:], ALU.mult, ALU.mult, ["T1", "hgg", "sgT"], ["cathg"])
                B.DMA("sp", cat_d[L][1024 + h * 128:1024 + (h + 1) * 128, :], og[:], ["cathg"], [f"cat{L}.{8 + h}"])

            nh = DBG.get('heads', NH)
            items = []
            for h in range(nh):
                if NHALF == 2:
                    items += [(h, 0, 0), (h, 0, 1), (h, 1, 1), (h, 1, 0)]
                else:
                    items += [(h, 0, 0), (h, 1, 0)]
            per = len(items) // nh
            head_start(0)
            prep(items[0], 0)
            for k, it in enumerate(items):
                if k + 1 < len(items):
                    nx = items[k + 1]
                    if (k + 1) % per == 0:
                        head_start(nx[0])
                    prep(nx, (k + 1) % 2)
                chunkwork(it, k % 2)
                if need_out and (k + 1) % per == 0:
                    head_end(it[0])
        P.barrier()

    def phase_outproj(l, L, s):
        with ExitStack() as st:
            xn = B.sb(st, "catT", [128, 16, L], BF16)
            wb = B.sb(st, "wo", [128, 2, 16, 512], BF16)
            xl = B.sb(st, "xl", [128, 2, L], F32)
            wv = w_out[l].rearrange("(dc p) n -> p dc n", p=128)
            for dc in range(16):
                B.DMA("sp", xn[:, dc, :L], cat_d[L][dc * 128:(dc + 1) * 128, :], [f"cat{L}.{dc}"], [f"catT.{dc}"])
            for blk in range(4):
                wbb = blk % 2
                B.DMA("pool", wb[:, wbb], wv[:, :, blk * 512:(blk + 1) * 512], (), [f"wo{wbb}"])
                for m4 in range(4):
                    m = blk * 4 + m4
                    xb = m % 2
                    B.DMA("sp", xl[:, xb, :], xres[L][m * 128:(m + 1) * 128, :], [f"xres{L}.{m}"], [f"xl{xb}"])
                    for (t0, tw) in tiles(L):
                        bk = B.bank()
                        for dc in range(16):
                            B.MM(PS[:, bk, :tw], wb[:, wbb, dc, m4 * 128:(m4 + 1) * 128], xn[:, dc, t0:t0 + tw], dc == 0, dc == 15,
                                 [f"wo{wbb}", f"catT.{dc}"], [psk(bk)])
                        B.STT(xl[:, xb, t0:t0 + tw], PS[:, bk, :tw], modT[:, 32 + m, s:s + 1], xl[:, xb, t0:t0 + tw], ALU.mult, ALU.add,
                              [psk(bk), "modT", f"xl{xb}"], [f"xl{xb}"])
                    B.DMA("act", xres[L][m * 128:(m + 1) * 128, :], xl[:, xb, :], [f"xl{xb}"], [f"xres{L}.{m}"])
        P.barrier()

    def phase_moe(l, L, s):
        TT_ = L // 128
        cap = 2 * L // NE
        nit = cap // 8
        JH = (cap + 127) // 128
        jw = min(cap, 128)
        NR = NE * JH
        with ExitStack() as st:
            R = B.sb(st, "moeR", [128, 16 * SEQ], BF16)
            xn_tm = B.sb(st, "xn_tm", [128, TT_, D], BF16)
            wr32 = B.sb(st, "wr32", [128, 16, NE], F32)
            aff = B.sb(st, "aff", [128, TT_, NE], F32)
            mx = B.sb(st, "mx", [128, TT_], F32)
            vals = B.sb(st, "vals", [16, cap], F32)
            idxu = B.sb(st, "idxu", [16, cap], U32)
            idxf = B.sb(st, "idxf", [16, cap], F32)
            idxT = B.sb(st, "idxT", [128, JH, NE], F32)
            valT = B.sb(st, "valT", [128, JH, NE], F32)
            selm = B.sb(st, "selm", [16, 16, 128], F32)
            pidx = B.sb(st, "pidx", [128, 16], F32)
            B.DMA("sp", wr32[:], wr_in[l].rearrange("(dc p) e -> p dc e", p=128), (), ["wr32"])
            B.DMA("sp", selm[:], cd["selmat"], (), ["selm"])
            B.DMA("sp", pidx[:], cd["pidx"], (), ["pidx"])
            xn = R[:, 0:16 * L].rearrange("p (a b) -> p a b", b=L)
            lb = 3
            with ExitStack() as st2:
                affT = B.sb(st2, "affT", [16, L], F32)
                with ExitStack() as st2a:
                    normmod(st2a, L, s, A2, "A2", 48, xn, "xn2", router=(wr32, lb), nbuf=1)
                    lg = PS[:, lb, 0:TT_ * 16].rearrange("p (t e) -> p t e", e=16)
                    P.op("dve", lambda e: e.tensor_reduce(mx[:], lg, AX.X, ALU.max), [psk(lb)], ["mx"])
                    B.TT("dve", aff[:], lg, mx[:].unsqueeze(2).to_broadcast([128, TT_, 16]), ALU.subtract, [psk(lb), "mx"], ["aff"])
                    B.ACT(aff[:], aff[:], AF.Exp, ["aff"], ["aff"])
                    P.op("dve", lambda e: e.tensor_reduce(mx[:], aff[:], AX.X, ALU.add), ["aff"], ["mx"])
                    P.op("dve", lambda e: e.reciprocal(mx[:], mx[:]), ["mx"], ["mx"])
                    B.TT("dve", aff[:], aff[:], mx[:].unsqueeze(2).to_broadcast([128, TT_, 16]), ALU.mult, ["aff", "mx"], ["aff"])
                P.barrier()
                for g0 in range(0, TT_, 4):
                    bk = B.bank()
                    n = min(4, TT_ - g0)
                    for q in range(n):
                        B.TR(PS[:16, bk, q * 128:(q + 1) * 128], aff[:, g0 + q, :], ident_f[:], ["aff", "ident_f"], [psk(bk)])
                    B.CP("act", affT[:, g0 * 128:(g0 + n) * 128], PS[:16, bk, 0:n * 128], [psk(bk)], ["affT"])
                for tt in range(TT_):
                    for q4 in range(4):
                        bk = B.bank()
                        pv = PS[:, bk, :].bitcast(BF16)
                        for q in range(4):
                            dc = q4 * 4 + q
                            B.TR(pv[:, q * 128:(q + 1) * 128], xn[:, dc, tt * 128:(tt + 1) * 128], ident_bf[:], [f"xn2.{dc}", "ident_bf"], [psk(bk)])
                        B.CP("act" if q4 % 2 else "dve", xn_tm[:, tt, q4 * 512:(q4 + 1) * 512], pv[:, 0:512], [psk(bk)], ["xn_tm"])
                for it in range(nit):
                    sl = slice(it * 8, (it + 1) * 8)
                    P.op("dve", lambda e, sl=sl: e.max(vals[:, sl], affT[:]), ["affT"], ["vals"])
                    P.op("dve", lambda e, sl=sl: e.max_index(idxu[:, sl], vals[:, sl], affT[:]), ["affT", "vals"], ["idxu"])
                    if it < nit - 1:
                        P.op("dve", lambda e, sl=sl: e.match_replace(affT[:], vals[:, sl], affT[:], -1.0), ["affT", "vals"], ["affT"])
                B.CP("dve", idxf[:], idxu[:], ["idxu"], ["idxf"])
                for hh in range(JH):
                    bk = B.bank()
                    B.TR(PS[:jw, bk, 0:16], idxf[:, hh * 128:hh * 128 + jw], ident_f[:16, :16], ["idxf", "ident_f"], [psk(bk)])
                    B.TR(PS[:jw, bk, 16:32], vals[:, hh * 128:hh * 128 + jw], ident_f[:16, :16], ["vals", "ident_f"], [psk(bk)])
                    B.CP("dve", idxT[:jw, hh, :], PS[:jw, bk, 0:16], [psk(bk)], ["idxT"])
                    B.CP("dve", valT[:jw, hh, :], PS[:jw, bk, 16:32], [psk(bk)], ["valT"])
            P.barrier()
            EG = min(max(512 // cap, 1), NE)
            gw = EG * cap
            Selg = R[:, 0:TT_ * gw].rearrange("p (a b) -> p a b", b=gw)
            xsT = R[:, 8192:8192 + 16 * gw].rearrange("p (a b) -> p a b", b=gw)
            wg = R[:, 16384:16384 + 8192].rearrange("p (w a b) -> p w a b", w=2, b=256)
            wu = R[:, 24576:24576 + 8192].rearrange("p (w a b) -> p w a b", w=2, b=256)
            with ExitStack() as st3:
                wd = B.sb(st3, "wd", [128, 2, 8, 512], BF16)
                hidT = B.sb(st3, "hidT", [128, 8, cap], BF16)
                sg_ = B.sb(st3, "moesg", [128, 2, cap], F32)
                ysb = B.sb(st3, "ysb", [128, 2, JH, D], BF16)
                for eg in range(NE // EG):
                    bk = B.bank()
                    for q in range(EG):
                        e_ = eg * EG + q
                        B.MM(PS[:, bk, q * cap:(q + 1) * cap], selm[:, e_, :], idxf[:], True, True, ["selm", "idxf"], [psk(bk)])
                    for tt in range(TT_):
                        B.TS("dve", Selg[:, tt, :], PS[:, bk, :gw], pidx[:, tt:tt + 1], None, ALU.is_equal, None, [psk(bk), "pidx"], ["Selg"])
                    for m in range(16):
                        bk2 = B.bank()
                        for tt in range(TT_):
                            B.MM(PS[:, bk2, :gw], xn_tm[:, tt, m * 128:(m + 1) * 128], Selg[:, tt, :], tt == 0, tt == TT_ - 1,
                                 ["xn_tm", "Selg"], [psk(bk2)])
                        B.CP("act" if m % 2 else "dve", xsT[:, m, :], PS[:, bk2, :gw], [psk(bk2)], ["xsT"])
                    for q in range(EG):
                        e_ = eg * EG + q
                        xs = xsT[:, :, q * cap:(q + 1) * cap]
                        gv = wg_in[l, e_].rearrange("(dc p) f -> p dc f", p=128)
                        uv = wu_in[l, e_].rearrange("(dc p) f -> p dc f", p=128)
                        dv = wd_in[l, e_].rearrange("(fc p) d -> p fc d", p=128)
                        for fb_ in range(4):
                            wb_ = fb_ % 2
                            B.DMA("pool", wg[:, wb_], gv[:, :, fb_ * 256:(fb_ + 1) * 256], (), [f"wg{wb_}"])
                            B.DMA("pool", wu[:, wb_], uv[:, :, fb_ * 256:(fb_ + 1) * 256], (), [f"wu{wb_}"])
                            for f2 in range(2):
                                fc = fb_ * 2 + f2
                                bg, bu = B.bank(), B.bank()
                                for dc in range(16):
                                    B.MM(PS[:, bg, :cap], wg[:, wb_, dc, f2 * 128:(f2 + 1) * 128], xs[:, dc, :], dc == 0, dc == 15,
                                         [f"wg{wb_}", "xsT"], [psk(bg)])
                                for dc in range(16):
                                    B.MM(PS[:, bu, :cap], wu[:, wb_, dc, f2 * 128:(f2 + 1) * 128], xs[:, dc, :], dc == 0, dc == 15,
                                         [f"wu{wb_}", "xsT"], [psk(bu)])
                                sb_ = fc % 2
                                B.ACT(sg_[:, sb_, :], PS[:, bg, :cap], AF.Silu, [psk(bg)], [f"moesg{sb_}"])
                                B.TT("dve", hidT[:, fc, :], sg_[:, sb_, :], PS[:, bu, :cap], ALU.mult, [f"moesg{sb_}", psk(bu)], ["hidT"])
                        yb = e_ % 2
                        for db_ in range(4):
                            wb_ = db_ % 2
                            B.DMA("pool", wd[:, wb_], dv[:, :, db_ * 512:(db_ + 1) * 512], (), [f"wd{wb_}"])
                            for hh in range(JH):
                                bk3 = B.bank()
                                for fc in range(8):
                                    B.MM(PS[:jw, bk3, :], hidT[:, fc, hh * 128:hh * 128 + jw], wd[:, wb_, fc, :], fc == 0, fc == 7,
                                         ["hidT", f"wd{wb_}"], [psk(bk3)])
                                B.CP("act" if hh % 2 else "dve", ysb[:jw, yb, hh, db_ * 512:(db_ + 1) * 512], PS[:jw, bk3, :], [psk(bk3)], [f"ysb{yb}"])
                        for hh in range(JH):
                            B.DMA("sp", ys_d[:, :jw, e_ * JH + hh, :].rearrange("m j d -> j m d"),
                                  ysb[:jw, yb, hh, :].rearrange("j (m d) -> j m d", d=128), [f"ysb{yb}"], [f"ys.{e_}.{hh}"])
            P.barrier()
            SelT = R[:, 0:2 * NR * 512].rearrange("p (w r c) -> p w r c", w=2, c=512)
            with ExitStack() as st4:
                ysm = B.sb(st4, "ysm", [128, 2, NR, 128], BF16)
                xl = B.sb(st4, "xl2", [128, 2, 512], F32)
                iot = B.sb(st4, "iot", [128, L], F32)
                B.DMA("sp", iot[:], cd["iota_t"][:, :L], (), ["iot"])
                for ti, (t0, tw) in enumerate(tiles(L)):
                    sb_ = ti % 2
                    for e_ in range(NE):
                        for hh in range(JH):
                            r_ = e_ * JH + hh
                            B.TS("dve", SelT[:jw, sb_, r_, :tw], iot[:jw, t0:t0 + tw], idxT[:jw, hh, e_:e_ + 1], valT[:jw, hh, e_:e_ + 1],
                                 ALU.is_equal, ALU.mult, ["iot", "idxT", "valT"], [f"SelT{sb_}"])
                    for m in range(16):
                        mb = m % 2
                        B.DMA("sp", ysm[:jw, mb], ys_d[m, :jw, 0:NR, :],
                              [f"ys.{e_}.{hh}" for e_ in range(NE) for hh in range(JH)], [f"ysm{mb}"])
                        B.DMA("sp", xl[:, mb, :tw], xres[L][m * 128:(m + 1) * 128, t0:t0 + tw], [f"xres{L}.{m}"], [f"xl2{mb}"])
                        bk = B.bank()
                        for r_ in range(NR):
                            B.MM(PS[:, bk, :tw], ysm[:jw, mb, r_, :], SelT[:jw, sb_, r_, :tw], r_ == 0, r_ == NR - 1,
                                 [f"ysm{mb}", f"SelT{sb_}"], [psk(bk)])
                        B.STT(xl[:, mb, :tw], PS[:, bk, :tw], modT[:, 80 + m, s:s + 1], xl[:, mb, :tw], ALU.mult, ALU.add,
                              [psk(bk), "modT", f"xl2{mb}"], [f"xl2{mb}"])
                        B.DMA("act", xres[L][m * 128:(m + 1) * 128, t0:t0 + tw], xl[:, mb, :tw], [f"xl2{mb}"], [f"xres{L}.{m}"])
        P.barrier()

    def phase_final():
        L = SEQ
        with ExitStack() as st:
            xld = B.sb(st, "fxld", [128, 2, L], F32)
            sq = B.sb(st, "fsq", [128, 2, L], F32)
            rstd = B.sb(st, "frstd", [128, L], F32)
            for dc in range(16):
                b_ = dc % 2
                B.DMA("sp", xld[:, b_, :], xres[L][dc * 128:(dc + 1) * 128, :], [f"xres{L}.{dc}"], [f"fxld{b_}"])
                B.ACT(sq[:, b_, :], xld[:, b_, :], AF.Square, [f"fxld{b_}"], [f"fsq{b_}"])
                for i, (t0, tw) in enumerate(tiles(L)):
                    B.MM(PS[:, 4 + i, :tw], ones_f[:], sq[:, b_, t0:t0 + tw], dc == 0, dc == 15, [f"fsq{b_}", "ones_f"], [psk(4 + i)])
            for i, (t0, tw) in enumerate(tiles(L)):
                B.ACT(rstd[:, t0:t0 + tw], PS[:, 4 + i, :tw], AF.Ln, [psk(4 + i)], ["frstd"], bias=epsc[:, 0:1], scale=1.0 / D)
            B.ACT(rstd[:], rstd[:], AF.Exp, ["frstd"], ["frstd"], scale=-0.5)
            for dc in range(16):
                b_ = dc % 2
                B.DMA("sp", xld[:, b_, :], xres[L][dc * 128:(dc + 1) * 128, :], [f"xres{L}.{dc}"], [f"fxld{b_}"])
                B.STT(sq[:, b_, :], xld[:, b_, :], gfin[:, dc:dc + 1], rstd[:], ALU.mult, ALU.mult, [f"fxld{b_}", "gfin", "frstd"], [f"fsq{b_}"])
                B.DMA("act", out_d[dc * 128:(dc + 1) * 128, :], sq[:, b_, :], [f"fsq{b_}"], [f"out.{dc}"])
        P.barrier()

    nstage = 0

    def go():
        nonlocal nstage
        nstage += 1
        return nstage <= stage and nstage not in skip

    for l in range(DEPTH):
        last = l == DEPTH - 1
        wv = w_in[l].rearrange("(dc p) n -> p dc n", p=128)
        if go():
            phase_mod(l)
        if go():
            with ExitStack() as sx:
                XN = B.sb(sx, "XNc", [128, 16, CTX], BF16)
                with ExitStack() as st:
                    normmod(st, CTX, 1, A1, "A1", 0, XN, "XN")
                P.barrier()
                parts = DBG.get("ctx_parts", ("hgrn", "fourier", "hyena", "outproj"))
                if "hgrn" in parts:
                    phase_hgrn(l, CTX, XN, "XN", wv, need_out=(not last) and DBG.get("ctx_need_out", True), first=True)
                if not last:
                    if "fourier" in parts:
                        phase_fourier(l, CTX, XN, "XN", wv)
                    if "hyena" in parts:
                        phase_hyfilter(l, CTX)
                        phase_hyena(l, CTX, XN, "XN", wv)
            if not last and "outproj" in parts:
                phase_outproj(l, CTX, 1)
        with ExitStack() as sx:
            XN = B.sb(sx, "XNx", [128, 16, SEQ], BF16)
            if go():
                with ExitStack() as st:
                    normmod(st, SEQ, 0, A1, "A1", 0, XN, "XN")
                P.barrier()
            if go():
                phase_fourier(l, SEQ, XN, "XN", wv)
            if go():
                phase_hyfilter(l, SEQ)
                phase_hyena(l, SEQ, XN, "XN", wv)
            if go():
                phase_hgrn(l, SEQ, XN, "XN", wv, need_out=True, first=False)
        if go():
            phase_outproj(l, SEQ, 0)
        if go():
            phase_moe(l, SEQ, 0)
        if go():
            if not last:
                phase_moe(l, CTX, 1)
    if go():
        phase_final()
    else:
        with ExitStack() as st:
            z = B.sb(st, "zz", [128, SEQ], F32)
            B.MS("dve", z[:], 0.0, ["zz"])
            for dc in range(16):
                B.DMA("sp", out_d[dc * 128:(dc + 1) * 128, :], z[:], ["zz"], [f"out.{dc}"])
    info = P.emit()
    info["names"] = P.names
    info["sites"] = [o["site"] for o in P.ops]
    info["in_shapes"] = {k: tuple(v.shape) for k, v in B.inputs.items()}
    B.st.close()
    return nc, consts, info


_PROG = {}


def prep_inputs(inp, b, consts):
    m = {}
    m["xT"] = np.ascontiguousarray(inp["x"][b].T)
    m["ctxT"] = np.ascontiguousarray(inp["ctx"][b].T)
    cc = np.stack([pmaj(inp["c"][b], 16), pmaj(inp["c_ctx"], 16)], axis=-1)
    m["cT"] = np.ascontiguousarray(cc)
    m["gmix"] = pmaj(inp["norm_mix_g"], 16)
    m["gffn"] = pmaj(inp["norm_ffn_g"], 16)
    m["gfin"] = pmaj(inp["final_norm_g"], 16)
    m["bmod"] = pmaj(inp["b_mod"], 96)
    m["hycw"] = pmaj(inp["hy_conv_w"], 12)
    m["hycb"] = pmaj(inp["hy_conv_b"], 12)
    m["hyb"] = np.ascontiguousarray(np.stack([inp["hy_b1"], inp["hy_b2"], inp["hy_b3"]], axis=-1).transpose(1, 0, 2)).astype(np.float32)
    m["hyfr"] = np.ascontiguousarray(np.asarray(inp["hy_freq"], np.float32).transpose(2, 0, 1))
    m["hydb"] = pmaj(inp["hy_bias"], 4)
    m["hglb"] = pmaj(inp["hg_lb"], 8)
    m["hgg"] = pmaj(inp["hg_norm_g"], 8)
    for k in ("w_mod", "w_in", "w_out", "w_fnet", "hy_w1", "hy_w2", "hy_w3", "hy_w_out", "w_router", "w_gate", "w_up", "w_down"):
        m[k] = np.ascontiguousarray(np.asarray(inp[k], np.float32))
    m.update(consts)
    return m


def kernel(**inputs):
    if "full" not in _PROG:
        _PROG["full"] = build()
    nc, consts, info = _PROG["full"]
    n = 8
    in_maps = [prep_inputs(inputs, b, consts) for b in range(n)]
    res = run_bass_kernel_spmd(nc, in_maps, core_ids=list(range(n)))
    out = np.stack([np.ascontiguousarray(res.results[b]["outT"].T) for b in range(n)], axis=0)
    return out.astype(np.float32)
```

```python
import math
from contextlib import ExitStack

import numpy as np
import ml_dtypes

import concourse.bass as bass
import concourse.mybir as mybir
from concourse.bass_utils import run_bass_kernel_spmd

F32 = mybir.dt.float32
BF16 = mybir.dt.bfloat16
U32 = mybir.dt.uint32
AF = mybir.ActivationFunctionType
ALU = mybir.AluOpType
AX = mybir.AxisListType

D = 2048
SEQ = 2048
CTX = 256
DEPTH = 2
NE = 16
FF = 1024
INW = 7168
HY_OFF = 512
HG_OFF = 2048
HGW = 1024
NH = 8
EPS = 1e-6
PI = math.pi
TRACE_SITES = False
DBG = {}


class Prog:
    ENGS = ("pe", "act", "dve", "pool", "sp")
    HND = {"pe": "tensor", "act": "scalar", "dve": "vector", "pool": "gpsimd", "sp": "sync"}

    def __init__(self, nc, n_dma_sems=12):
        self.nc = nc
        self.ops = []
        self.n_dma_sems = n_dma_sems
        self.bar = set()
        self.since_bar = []
        self.trace_sites = False
        self.names = {}

    def op(self, eng, fn, reads=(), writes=(), dma=False):
        if len(self.ops) >= DBG.get("max_ops", 10 ** 9):
            return -1
        site = None
        if self.trace_sites:
            import sys as _sys
            f = _sys._getframe(1)
            site = []
            while f is not None and len(site) < 4:
                if f.f_code.co_name not in ("MM", "TR", "ACT", "TS", "TT", "STT", "CP", "MS", "DMA"):
                    site.append(f"{f.f_code.co_name}:{f.f_lineno}")
                f = f.f_back
        self.ops.append(dict(eng=eng, fn=fn, reads=list(reads), writes=list(writes), dma=dma,
                             bar=frozenset(self.bar), site=site))
        self.since_bar.append(len(self.ops) - 1)
        return len(self.ops) - 1

    def barrier(self):
        last = {}
        dm = []
        for i in self.since_bar:
            o = self.ops[i]
            if o["dma"]:
                dm.append(i)
            else:
                last[o["eng"]] = i
        new = set(last.values()) | set(dm)
        for i in self.bar:
            o = self.ops[i]
            if not o["dma"] and o["eng"] not in last:
                new.add(i)
        self.bar = new
        self.since_bar = []

    def emit(self, final_wait_eng="sp"):
        nc = self.nc
        ops = self.ops
        last_w = {}
        readers = {}
        for i, o in enumerate(ops):
            deps = set(o["bar"])
            for r in o["reads"]:
                if r in last_w:
                    deps.add(last_w[r])
            for w in o["writes"]:
                if w in last_w:
                    deps.add(last_w[w])
                for j in readers.get(w, ()):
                    deps.add(j)
            deps.discard(i)
            if o["eng"] == "pe":
                deps = {j for j in deps if ops[j]["eng"] != "pe"}
            latest = {}
            keep = set()
            for j in deps:
                if ops[j]["dma"]:
                    keep.add(j)
                else:
                    e2 = ops[j]["eng"]
                    if j > latest.get(e2, -1):
                        latest[e2] = j
            deps = keep | set(latest.values())
            o["deps"] = deps
            for r in o["reads"]:
                readers.setdefault(r, []).append(i)
            for w in o["writes"]:
                last_w[w] = i
                readers[w] = []
        needed = set()
        for o in ops:
            needed |= o["deps"]
        tail = [i for i, o in enumerate(ops) if o["dma"] and i not in needed]
        needed |= set(tail)
        cnt = {e: 0 for e in self.ENGS}
        dcnt = {e: 0 for e in self.ENGS}
        for i, o in enumerate(ops):
            e = o["eng"]
            if o["dma"]:
                k = dcnt[e]
                dcnt[e] += 1
                o["sem"] = ("d", e, k % self.n_dma_sems)
                o["val"] = 16 * (k // self.n_dma_sems + 1)
                o["prev_val"] = 16 * (k // self.n_dma_sems)
            elif i in needed:
                cnt[e] += 1
                o["sem"] = ("c", e)
                o["val"] = cnt[e]
            else:
                o["sem"] = None
        st = ExitStack()
        sems = {}
        for e in self.ENGS:
            if cnt[e]:
                sems[("c", e)] = st.enter_context(nc.semaphore(f"s_{e}"))
            for k in range(min(dcnt[e], self.n_dma_sems)):
                sems[("d", e, k)] = st.enter_context(nc.semaphore(f"d_{e}{k}"))
        block = st.enter_context(nc.Block())

        def make(e):
            def body(eh):
                waited = {}

                def wait(semkey, val):
                    if waited.get(semkey, 0) >= val:
                        return
                    eh.wait_ge(sems[semkey], val)
                    waited[semkey] = val

                for i, o in enumerate(ops):
                    if o["eng"] != e:
                        continue
                    for j in sorted(o["deps"]):
                        wait(ops[j]["sem"], ops[j]["val"])
                    if o["dma"] and o["prev_val"] > 0:
                        wait(o["sem"], o["prev_val"])
                    ins = o["fn"](eh)
                    if o["site"] is not None:
                        self.names[ins.ins.name] = o["site"]
                    if o["sem"] is not None:
                        ins.then_inc(sems[o["sem"]], 16 if o["dma"] else 1)
                if e == final_wait_eng:
                    for i in tail:
                        wait(ops[i]["sem"], ops[i]["val"])
            return body

        for e in self.ENGS:
            if any(o["eng"] == e for o in ops) or e == final_wait_eng:
                getattr(block, self.HND[e])(make(e))
        st.close()
        return dict(cnt=cnt, dcnt=dcnt, nops=len(ops))


def _bf(a):
    return np.ascontiguousarray(a.astype(np.float32)).astype(ml_dtypes.bfloat16)


def host_consts():
    c = {}
    c["ident_bf"] = _bf(np.eye(128))
    c["ident_f"] = np.eye(128, dtype=np.float32)
    c["ones_f"] = np.ones((128, 128), np.float32)
    k = np.arange(128, dtype=np.float64)
    ang = 2 * np.pi * ((k[:, None] * k[None, :]) % 128) / 128
    c["fc_cs"] = _bf(np.concatenate([np.cos(ang), np.sin(ang)], axis=1) / math.sqrt(128))
    for L in (SEQ, CTX):
        TT = L // 128
        FW = min(256, L)
        t = np.arange(L, dtype=np.int64)
        f = np.arange(L, dtype=np.int64)
        ang = 2 * np.pi * ((t[:, None] * f[None, :]) % L) / L
        cs = np.stack([np.cos(ang), -np.sin(ang)], 0) / math.sqrt(L)
        tab = cs.reshape(2, TT, 128, L // FW, FW).transpose(3, 2, 0, 1, 4)
        c[f"fn_tab{L}"] = _bf(tab)
        N4 = 4 * L
        ang = 2 * np.pi * (((2 * f[None, :] + 1) * t[:, None]) % N4) / N4
        cs = np.stack([np.cos(ang), -np.sin(ang)], 0)
        FT = L // 128
        tab = cs.reshape(2, TT, 128, FT, 128).transpose(3, 2, 0, 1, 4)
        c[f"hy_fwd{L}"] = _bf(tab)
        IW = min(256, L)
        csi = cs.transpose(0, 2, 1) / L
        tab = csi.reshape(2, FT, 128, L // IW, IW).transpose(3, 2, 0, 1, 4)
        c[f"hy_inv{L}"] = _bf(tab)
        pos = np.arange(L, dtype=np.float32)
        tt_ = (pos / max(L - 1, 1))[:, None]
        bands = np.linspace(1e-4, 15, 16, dtype=np.float32)
        w = (2.0 * np.float32(math.pi) * pos[:, None] / np.float32(L)).astype(np.float32)
        z = np.concatenate([tt_, np.cos(bands * w), -np.sin(bands * w)], axis=-1).astype(np.float32)
        c[f"hy_z{L}"] = np.ascontiguousarray(z.T)
        c[f"hy_tpos{L}"] = np.ascontiguousarray(np.broadcast_to(tt_[:, 0][None, :], (128, L))).astype(np.float32)
        r = np.ones((128, L), np.float32)
        r[:, ::64] = 0.0
        c[f"hg_rst{L}"] = _bf(r)
    max_decay = math.log(1e-2) / 0.3
    min_decay = math.log(1e-2) / 1.5
    deltas = np.linspace(min_decay, max_decay, 512, dtype=np.float32)
    c["hy_ndelta"] = np.ascontiguousarray((-np.abs(deltas)).reshape(4, 128).T).astype(np.float32)
    s = np.arange(64)
    same = (s[:, None] // 16) == (s[None, :] // 16)
    mf = ((s[:, None] <= s[None, :]) & same).astype(np.float32)
    mb = ((s[:, None] >= s[None, :]) & same).astype(np.float32)
    c["hg_maskD"] = _bf(np.stack([np.concatenate([mf, mf], 0), np.concatenate([mb, mb], 0)], 1))
    ng = np.zeros((2, 3, 64), np.float32)
    for vi in range(3):
        ng[0, vi, s >= 16 * (vi + 1)] = -30000.0
        ng[1, vi, s < 16 * (vi + 1)] = -30000.0
    c["hg_negm"] = np.ascontiguousarray(np.broadcast_to(ng[None], (128, 2, 3, 64))).astype(np.float32)
    c["iota_t"] = np.ascontiguousarray(np.broadcast_to(np.arange(SEQ, dtype=np.float32)[None, :], (128, SEQ)))
    c["pidx"] = (np.arange(16)[None, :] * 128 + np.arange(128)[:, None]).astype(np.float32)
    sm = np.zeros((16, 16, 128), np.float32)
    for e in range(16):
        sm[e, e, :] = 1.0
    c["selmat"] = sm
    return c


_CONSTS = None


def pmaj(v, n):
    v = np.asarray(v, np.float32)
    lead = v.shape[:-1]
    a = v.reshape(lead + (n, 128))
    a = np.moveaxis(a, -1, 0)
    return np.ascontiguousarray(a)


class Builder:
    def __init__(self, nc, debug=()):
        self.nc = nc
        self.P = Prog(nc)
        self.debug = set(debug)
        self.inputs = {}
        self.st = ExitStack()
        self.PS = self.st.enter_context(nc.psum_tensor("PS", [128, 8, 512], F32))
        self.psi = 0
        self.uid = 0

    def din(self, name, shape, dt=F32):
        t = self.nc.dram_tensor(name, list(shape), dt, kind="ExternalInput").ap()
        self.inputs[name] = t
        return t

    def dscr(self, name, shape, dt):
        kind = "ExternalOutput" if name in self.debug else "Internal"
        return self.nc.dram_tensor(name, list(shape), dt, kind=kind).ap()

    def sb(self, st, name, shape, dt):
        self.uid += 1
        return st.enter_context(self.nc.sbuf_tensor(f"{name}_{self.uid}", list(shape), dt))

    def bank(self):
        b = self.psi
        self.psi = (self.psi + 1) % 8
        return b

    def MM(self, out, lhsT, rhs, start, stop, r, w):
        self.P.op("pe", lambda e: e.matmul(out, lhsT, rhs, start=start, stop=stop), r, w)

    def TR(self, out, in_, ident, r, w):
        self.P.op("pe", lambda e: e.transpose(out, in_, ident), r, w)

    def ACT(self, out, in_, func, r, w, bias=None, scale=None):
        kw = {}
        if bias is not None:
            kw["bias"] = bias
        if scale is not None:
            kw["scale"] = scale
        self.P.op("act", lambda e: e.activation(out=out, in_=in_, func=func, **kw), r, w)

    def TS(self, eng, out, in0, s1, s2, op0, op1, r, w):
        if op1 is None:
            self.P.op(eng, lambda e: e.tensor_scalar(out, in0, s1, None, op0), r, w)
        else:
            self.P.op(eng, lambda e: e.tensor_scalar(out, in0, s1, s2, op0, op1), r, w)

    def TT(self, eng, out, in0, in1, op, r, w):
        self.P.op(eng, lambda e: e.tensor_tensor(out, in0, in1, op), r, w)

    def STT(self, out, in0, scalar, in1, op0, op1, r, w):
        self.P.op("dve", lambda e: e.scalar_tensor_tensor(out, in0, scalar, in1, op0, op1), r, w)

    def CP(self, eng, out, in_, r, w):
        if eng == "act":
            self.P.op("act", lambda e: e.activation(out=out, in_=in_, func=AF.Copy), r, w)
        else:
            self.P.op(eng, lambda e: e.tensor_copy(out, in_), r, w)

    def MS(self, eng, ap, val, w):
        self.P.op(eng, lambda e: e.memset(ap, val), (), w)

    def DMA(self, eng, out, in_, r, w):
        self.P.op(eng, lambda e: e.dma_start(out=out, in_=in_), r, w, dma=True)


def tiles(L, w=512):
    w = min(w, L)
    return [(i * w, w) for i in range(L // w)]


def build(stage=99, debug=(), skip=()):
    nc = bass.Bass("TRN2", target_bir_lowering=False)
    B = Builder(nc, debug)
    P = B.P
    P.trace_sites = TRACE_SITES
    consts = host_consts()

    xT_in = B.din("xT", [D, SEQ])
    cxT_in = B.din("ctxT", [D, CTX])
    cT_in = B.din("cT", [128, 16, 2])
    gmix_in = B.din("gmix", [128, DEPTH, 16])
    gffn_in = B.din("gffn", [128, DEPTH, 16])
    gfin_in = B.din("gfin", [128, 16])
    bmod_in = B.din("bmod", [128, DEPTH, 96])
    w_mod = B.din("w_mod", [DEPTH, D, 6 * D])
    w_in = B.din("w_in", [DEPTH, D, INW])
    w_out = B.din("w_out", [DEPTH, D, D])
    w_fnet = B.din("w_fnet", [DEPTH, 512, 512])
    hycw_in = B.din("hycw", [128, DEPTH, 3, 12])
    hycb_in = B.din("hycb", [128, DEPTH, 12])
    hyw1_in = B.din("hy_w1", [DEPTH, 33, 64])
    hyw2_in = B.din("hy_w2", [DEPTH, 64, 64])
    hyw3_in = B.din("hy_w3", [DEPTH, 64, 64])
    hywo_in = B.din("hy_w_out", [DEPTH, 64, 1024])
    hyb_in = B.din("hyb", [64, DEPTH, 3])
    hyfr_in = B.din("hyfr", [64, DEPTH, 3])
    hydb_in = B.din("hydb", [128, DEPTH, 4])
    hglb_in = B.din("hglb", [128, DEPTH, 2, 8])
    hgg_in = B.din("hgg", [128, DEPTH, 8])
    wr_in = B.din("w_router", [DEPTH, D, NE])
    if DBG.get("small"):
        wg_in = B.din("w_gate", [1, 1, 128, FF])
        wu_in = B.din("w_up", [1, 1, 128, FF])
        wd_in = B.din("w_down", [1, 1, 128, D])
    else:
        wg_in = B.din("w_gate", [DEPTH, NE, D, FF])
        wu_in = B.din("w_up", [DEPTH, NE, D, FF])
        wd_in = B.din("w_down", [DEPTH, NE, FF, D])
    cd = {}
    for k, v in consts.items():
        dt = BF16 if v.dtype == ml_dtypes.bfloat16 else F32
        cd[k] = B.din(k, v.shape, dt)

    out_d = nc.dram_tensor("outT", [D, SEQ], F32, kind="ExternalOutput").ap()
    xres = {SEQ: B.dscr("xres", [D, SEQ], F32), CTX: B.dscr("xcres", [D, CTX], F32)}
    cat_d = {SEQ: B.dscr("cat_x", [D, SEQ], BF16), CTX: B.dscr("cat_c", [D, CTX], BF16)}
    hyk_d = {L: B.dscr(f"hyk{L}", [L // 128, 128, 2, 512], F32) for L in (SEQ, CTX)}
    ys_d = {SEQ: B.dscr("ys_x", [16, 128, 2 * NE, 128], BF16), CTX: B.dscr("ys_c", [16, 32, NE, 128], BF16)}

    G = B.st
    ident_bf = B.sb(G, "ident_bf", [128, 128], BF16)
    ident_f = B.sb(G, "ident_f", [128, 128], F32)
    ones_f = B.sb(G, "ones_f", [128, 128], F32)
    modT = B.sb(G, "modT", [128, 96, 2], F32)
    A1 = B.sb(G, "A1", [128, 16, 2], F32)
    A2 = B.sb(G, "A2", [128, 16, 2], F32)
    gmix = B.sb(G, "gmix", [128, DEPTH, 16], F32)
    gffn = B.sb(G, "gffn", [128, DEPTH, 16], F32)
    gfin = B.sb(G, "gfin", [128, 16], F32)
    S32 = B.sb(G, "S32", [128, 16, 128], F32)
    hglb = B.sb(G, "hglb", [128, DEPTH, 2, 8], F32)
    lbs = B.sb(G, "lbs", [128, 2, 8], F32)
    oml = B.sb(G, "oml", [128, 2, 8], F32)
    hgg = B.sb(G, "hgg", [128, DEPTH, 8], F32)
    epsc = B.sb(G, "epsc", [128, 1], F32)
    B.MS("dve", epsc[:], EPS, ["epsc"])
    B.DMA("sp", ident_bf[:], cd["ident_bf"], (), ["ident_bf"])
    B.DMA("sp", ident_f[:], cd["ident_f"], (), ["ident_f"])
    B.DMA("sp", ones_f[:], cd["ones_f"], (), ["ones_f"])
    B.DMA("sp", gmix[:], gmix_in, (), ["gmix"])
    B.DMA("sp", gffn[:], gffn_in, (), ["gffn"])
    B.DMA("sp", gfin[:], gfin_in, (), ["gfin"])
    B.DMA("sp", hglb[:], hglb_in, (), ["hglb"])
    B.DMA("sp", hgg[:], hgg_in, (), ["hgg"])

    PS = B.PS

    def psk(b):
        return f"ps{b}"

    with ExitStack() as st:
        buf = B.sb(st, "cpy", [128, 2, SEQ], F32)
        for L, src in ((SEQ, xT_in), (CTX, cxT_in)):
            for dc in range(16):
                b_ = dc % 2
                B.DMA("sp", buf[:, b_, :L], src[dc * 128:(dc + 1) * 128, :], (), [f"cpy{b_}"])
                B.DMA("sp", xres[L][dc * 128:(dc + 1) * 128, :], buf[:, b_, :L], [f"cpy{b_}"], [f"xres{L}.{dc}"])
        e0 = B.sb(st, "e0", [128, 2, 8], F32)
        e1 = B.sb(st, "e1", [128, 2, 8], F32)
        B.ACT(e0[:], hglb[:, 0], AF.Exp, ["hglb"], ["e0"])
        B.ACT(e1[:], hglb[:, 1], AF.Exp, ["hglb"], ["e1"])
        B.TT("dve", e0[:], e0[:], e1[:], ALU.add, ["e0", "e1"], ["e0"])
        B.P.op("dve", lambda e: e.reciprocal(e0[:], e0[:]), ["e0"], ["e0"])
        B.TT("dve", lbs[:], e1[:], e0[:], ALU.mult, ["e0", "e1"], ["lbs"])
        B.TS("dve", oml[:], lbs[:], -1.0, 1.0, ALU.mult, ALU.add, ["lbs"], ["oml"])
    P.barrier()

    def phase_mod(l):
        with ExitStack() as st:
            cT = B.sb(st, "cT", [128, 16, 2], F32)
            sc = B.sb(st, "sc", [128, 16, 2], BF16)
            bmod = B.sb(st, "bmod", [128, 96], F32)
            wb = B.sb(st, "wmod", [128, 2, 16, 512], BF16)
            B.DMA("sp", cT[:], cT_in, (), ["cT"])
            B.DMA("sp", bmod[:], bmod_in[:, l, :], (), ["bmod"])
            B.ACT(sc[:], cT[:], AF.Silu, ["cT"], ["sc"])
            wv = w_mod[l].rearrange("(dc p) n -> p dc n", p=128)
            bk = B.bank()
            for blk in range(24):
                bb = blk % 2
                B.DMA("pool", wb[:, bb], wv[:, :, blk * 512:(blk + 1) * 512], (), [f"wmod{bb}"])
                for j in range(4):
                    oc = blk * 4 + j
                    for dc in range(16):
                        B.MM(PS[:, bk, oc * 2:oc * 2 + 2], wb[:, bb, dc, j * 128:(j + 1) * 128], sc[:, dc, :],
                             dc == 0, dc == 15, [f"wmod{bb}", "sc"], [psk(bk)])
            B.TT("dve", modT[:], PS[:, bk, 0:192].rearrange("p (a b) -> p a b", b=2),
                 bmod[:].unsqueeze(2).to_broadcast([128, 96, 2]), ALU.add, [psk(bk), "bmod"], ["modT"])
            for (Ax, g, off, nm) in ((A1, gmix, 16, "A1"), (A2, gffn, 64, "A2")):
                B.TS("dve", Ax[:], modT[:, off:off + 16, :], 1.0, None, ALU.add, None, ["modT"], [nm])
                B.TT("dve", Ax[:], Ax[:], g[:, l, :].unsqueeze(2).to_broadcast([128, 16, 2]), ALU.mult,
                     [nm, "gmix", "gffn"], [nm])
        P.barrier()

    def normmod(st, L, s, Ax, Anm, shoff, xn, xnk, router=None, nbuf=2):
        xld_ = B.sb(st, "xld", [128, nbuf, L], F32)
        sq_ = B.sb(st, "sq", [128, nbuf, L], F32)
        rstd = B.sb(st, "rstd", [128, L], F32)
        tmp_ = B.sb(st, "nmtmp", [128, nbuf, L], F32)

        class _V:
            def __init__(self, t, n):
                self.t, self.n = t, n

            def __getitem__(self, key):
                p, b, f = key
                return self.t[p, b % self.n, f]
        sq = _V(sq_, nbuf)
        xld = _V(xld_, nbuf)
        tmp = _V(tmp_, nbuf)
        tl = tiles(L)
        for dc in range(16):
            b_ = dc % 2
            B.DMA("sp", xld[:, b_, :], xres[L][dc * 128:(dc + 1) * 128, :], [f"xres{L}.{dc}"], [f"xld{b_ % nbuf}"])
            B.ACT(sq[:, b_, :], xld[:, b_, :], AF.Square, [f"xld{b_ % nbuf}"], [f"sq{b_ % nbuf}"])
            for i, (t0, tw) in enumerate(tl):
                B.MM(PS[:, 4 + i, :tw], ones_f[:], sq[:, b_, t0:t0 + tw], dc == 0, dc == 15,
                     [f"sq{b_ % nbuf}", "ones_f"], [psk(4 + i)])
        for i, (t0, tw) in enumerate(tl):
            B.ACT(rstd[:, t0:t0 + tw], PS[:, 4 + i, :tw], AF.Ln, [psk(4 + i)], ["rstd"], bias=epsc[:, 0:1], scale=1.0 / D)
        B.ACT(rstd[:], rstd[:], AF.Exp, ["rstd"], ["rstd"], scale=-0.5)
        for dc in range(16):
            b_ = dc % 2
            B.DMA("sp", xld[:, b_, :], xres[L][dc * 128:(dc + 1) * 128, :], [f"xres{L}.{dc}"], [f"xld{b_ % nbuf}"])
            B.TT("dve", tmp[:, b_, :], xld[:, b_, :], rstd[:], ALU.mult, [f"xld{b_ % nbuf}", "rstd"], [f"nmtmp{b_ % nbuf}"])
            if router is None:
                B.ACT(xn[:, dc, :L], tmp[:, b_, :], AF.Identity, [f"nmtmp{b_ % nbuf}", Anm, "modT"], [f"{xnk}.{dc}"],
                      bias=modT[:, shoff + dc, s:s + 1], scale=Ax[:, dc, s:s + 1])
            else:
                wr32, lb = router
                B.ACT(sq[:, b_, :], tmp[:, b_, :], AF.Identity, [f"nmtmp{b_ % nbuf}", Anm, "modT"], [f"sq{b_ % nbuf}"],
                      bias=modT[:, shoff + dc, s:s + 1], scale=Ax[:, dc, s:s + 1])
                B.CP("pool", xn[:, dc, :L], sq[:, b_, :], [f"sq{b_ % nbuf}"], [f"{xnk}.{dc}"])
                for tt in range(L // 128):
                    B.MM(PS[:, lb, tt * 16:(tt + 1) * 16], sq[:, b_, tt * 128:(tt + 1) * 128], wr32[:, dc, :],
                         dc == 0 and tt == 0, dc == 15 and tt == L // 128 - 1, [f"sq{b_ % nbuf}", "wr32"], [psk(lb)])

    def load_w(wb, wbk, wv, c0, ncols):
        B.DMA("pool", wb[:, :, :ncols], wv[:, :, c0:c0 + ncols], (), [wbk])

    def proj_fm(L, xn, xnk, wb, wbk, ncols, evac, kch=16):
        for m in range(ncols // 128):
            for (t0, tw) in tiles(L):
                bk = B.bank()
                for dc in range(kch):
                    B.MM(PS[:, bk, :tw], wb[:, dc, m * 128:(m + 1) * 128], xn[:, dc, t0:t0 + tw], dc == 0, dc == kch - 1,
                         [wbk, f"{xnk}.{dc}"], [psk(bk)])
                evac(m, t0, tw, PS[:, bk, :tw], psk(bk))

    def phase_fourier(l, L, xn, xnk, wv):
        TT_ = L // 128
        FW = min(256, L)
        with ExitStack() as st:
            wb = B.sb(st, "wfn", [128, 16, 512], BF16)
            hT = B.sb(st, "hTfn", [128, 4, L], BF16)
            AB = B.sb(st, "AB", [128, TT_, 4, 256], BF16)
            cs = B.sb(st, "fc_cs", [128, 256], BF16)
            tab = B.sb(st, "fntab", [128, 2, 2, TT_, FW], BF16)
            yT = B.sb(st, "yTfn", [128, 4, L], BF16)
            wf = B.sb(st, "wfnet", [128, 4, 512], BF16)
            og = B.sb(st, "catfn", [128, 4, L], BF16)
            B.DMA("sp", cs[:], cd["fc_cs"], (), ["fc_cs"])
            B.DMA("pool", wf[:], w_fnet[l].rearrange("(kc p) n -> p kc n", p=128), (), ["wfnet"])
            load_w(wb, "wfn", wv, 0, 512)

            def ev(m, t0, tw, ps, pk):
                B.CP("act", hT[:, m, t0:t0 + tw], ps, [pk], [f"hTfn.{m}"])
            proj_fm(L, xn, xnk, wb, "wfn", 512, ev)
            for tt in range(TT_):
                bk = B.bank()
                for g in range(4):
                    B.MM(PS[:, bk, g * 128:(g + 1) * 128], hT[:, g, tt * 128:(tt + 1) * 128], cs[:, 0:128], True, True,
                         [f"hTfn.{g}", "fc_cs"], [psk(bk)])
                bk2 = B.bank()
                for g in range(4):
                    B.MM(PS[:, bk2, g * 128:(g + 1) * 128], hT[:, g, tt * 128:(tt + 1) * 128], cs[:, 128:256], True, True,
                         [f"hTfn.{g}", "fc_cs"], [psk(bk2)])
                B.CP("act", AB[:, tt, :, 0:128], PS[:, bk, :].rearrange("p (g c) -> p g c", c=128), [psk(bk)], [f"AB.{tt}"])
                B.CP("dve", AB[:, tt, :, 128:256], PS[:, bk2, :].rearrange("p (g c) -> p g c", c=128), [psk(bk2)], [f"AB.{tt}"])
            for i in range(L // FW):
                tb = i % 2
                B.DMA("sp", tab[:, tb], cd[f"fn_tab{L}"][i], (), [f"fntab{tb}"])
                for g in range(4):
                    bk = B.bank()
                    n = 0
                    for tt in range(TT_):
                        for a in range(2):
                            B.MM(PS[:, bk, :FW], AB[:, tt, g, a * 128:(a + 1) * 128], tab[:, tb, a, tt, :], n == 0,
                                 n == 2 * TT_ - 1, [f"AB.{tt}", f"fntab{tb}"], [psk(bk)])
                            n += 1
                    B.CP("act", yT[:, g, i * FW:(i + 1) * FW], PS[:, bk, :FW], [psk(bk)], [f"yTfn.{g}"])

            def ev2(m, t0, tw, ps, pk):
                B.CP("dve", og[:, m, t0:t0 + tw], ps, [pk], [f"catfn.{m}"])
            proj_fm(L, yT, "yTfn", wf, "wfnet", 512, ev2, kch=4)
            for m in range(4):
                B.DMA("sp", cat_d[L][m * 128:(m + 1) * 128, :], og[:, m, :], [f"catfn.{m}"], [f"cat{L}.{m}"])
        P.barrier()

    def wrap_sin(a, m, out, ps, pk, fr, fb, np_, w, outk):
        B.TS("dve", a[:np_, :w], ps, fr, fb, ALU.mult, ALU.add, [pk, "hyfrb"], ["ws_a"])
        B.TS("dve", m[:np_, :w], a[:np_, :w], PI, -2 * PI, ALU.is_gt, ALU.mult, ["ws_a"], ["ws_m"])
        B.TT("dve", a[:np_, :w], a[:np_, :w], m[:np_, :w], ALU.add, ["ws_a", "ws_m"], ["ws_a"])
        B.TS("dve", m[:np_, :w], a[:np_, :w], -PI, 2 * PI, ALU.is_lt, ALU.mult, ["ws_a"], ["ws_m"])
        B.TT("dve", a[:np_, :w], a[:np_, :w], m[:np_, :w], ALU.add, ["ws_a", "ws_m"], ["ws_a"])
        B.ACT(out, a[:np_, :w], AF.Sin, ["ws_a"], [outk])

    def fwd_dft(st, L, src, srck, consume):
        TT_ = L // 128
        tab = B.sb(st, "hyfwd", [128, 2, 2, TT_, 128], BF16)
        for ft in range(L // 128):
            tb = ft % 2
            B.DMA("sp", tab[:, tb], cd[f"hy_fwd{L}"][ft], (), [f"hyfwd{tb}"])
            bre, bim = B.bank(), B.bank()
            for a, bk in ((0, bre), (1, bim)):
                for tt in range(TT_):
                    B.MM(PS[:, bk, :], tab[:, tb, a, tt, :], src[:, tt, :], tt == 0, tt == TT_ - 1,
                         [f"hyfwd{tb}", srck], [psk(bk)])
            consume(ft, bre, bim)

    def to_tm(L, srcT, srck, dst, dstk):
        for tt in range(L // 128):
            bk = B.bank()
            pv = PS[:, bk, :].bitcast(BF16)
            for j in range(4):
                B.TR(pv[:, j * 128:(j + 1) * 128], srcT[:, j, tt * 128:(tt + 1) * 128], ident_bf[:],
                     [srck, "ident_bf"], [psk(bk)])
            B.CP("act" if tt % 2 else "dve", dst[:, tt, :], pv[:, 0:512], [psk(bk)], [dstk])

    def to_tm1(L, src, srck, dst, dstk, j):
        TT_ = L // 128
        for g0 in range(0, TT_, 4):
            n = min(4, TT_ - g0)
            bk = B.bank()
            pv = PS[:, bk, :].bitcast(BF16)
            for q in range(n):
                B.TR(pv[:, q * 128:(q + 1) * 128], src[:, (g0 + q) * 128:(g0 + q + 1) * 128], ident_bf[:], [srck, "ident_bf"], [psk(bk)])
            B.CP("act" if (g0 // 4) % 2 else "dve", dst[:, g0:g0 + n, j * 128:(j + 1) * 128],
                 pv[:, 0:n * 128].rearrange("p (q c) -> p q c", c=128), [psk(bk)], [dstk])

    def phase_hyfilter(l, L):
        TT_ = L // 128
        with ExitStack() as st:
            h3 = B.sb(st, "hyh1", [64, L], F32)
            with ExitStack() as st1:
                zT = B.sb(st1, "hyz", [33, L], F32)
                w1 = B.sb(st1, "hyw1", [33, 64], F32)
                w2 = B.sb(st1, "hyw2", [64, 64], F32)
                w3 = B.sb(st1, "hyw3", [64, 64], F32)
                fr = B.sb(st1, "hyfr", [64, 3], F32)
                fb = B.sb(st1, "hyfb", [64, 3], F32)
                h2 = B.sb(st1, "hyh2", [64, L], F32)
                wsa = B.sb(st1, "ws_a", [64, 512], F32)
                wsm = B.sb(st1, "ws_m", [64, 512], F32)
                h1 = h3
                B.DMA("sp", zT[:], cd[f"hy_z{L}"], (), ["hyz"])
                B.DMA("sp", w1[:], hyw1_in[l], (), ["hyw"])
                B.DMA("sp", w2[:], hyw2_in[l], (), ["hyw"])
                B.DMA("sp", w3[:], hyw3_in[l], (), ["hyw"])
                B.DMA("sp", fr[:], hyfr_in[:, l, :], (), ["hyfrb"])
                B.DMA("sp", fb[:], hyb_in[:, l, :], (), ["hyfrb"])
                B.TT("dve", fb[:], fb[:], fr[:], ALU.mult, ["hyfrb"], ["hyfrb"])
                srcs = [(zT, 33, w1, "hyz"), (h1, 64, w2, "hyh1"), (h2, 64, w3, "hyh2")]
                dsts = [(h1, "hyh1"), (h2, "hyh2"), (h1, "hyh1")]
                for li in range(3):
                    src, kp, wt, sk = srcs[li]
                    dst, dk = dsts[li]
                    for (t0, tw) in tiles(L):
                        bk = B.bank()
                        B.MM(PS[:64, bk, :tw], wt[:kp, :], src[:kp, t0:t0 + tw], True, True, ["hyw", sk], [psk(bk)])
                        wrap_sin(wsa, wsm, dst[:, t0:t0 + tw], PS[:64, bk, :tw], psk(bk), fr[:, li:li + 1], fb[:, li:li + 1], 64, tw, dk)
            P.barrier()
            wo = B.sb(st, "hywo", [64, 1024], F32)
            tpos = B.sb(st, "tpos", [128, L], F32)
            nd = B.sb(st, "ndelta", [128, 4], F32)
            dec = B.sb(st, "decay", [128, L], F32)
            hf = B.sb(st, "hf", [128, L], F32)
            hb = B.sb(st, "hb", [128, L], F32)
            hs = B.sb(st, "hs", [128, L], BF16)
            hd = B.sb(st, "hd", [128, L], BF16)
            hs_tm = B.sb(st, "hs_tm", [128, TT_, 512], BF16)
            hd_tm = B.sb(st, "hd_tm", [128, TT_, 512], BF16)
            kst = B.sb(st, "kst", [128, 2, 2, 512], F32)
            B.DMA("sp", wo[:], hywo_in[l], (), ["hywo"])
            B.DMA("sp", tpos[:], cd[f"hy_tpos{L}"], (), ["tpos"])
            B.DMA("sp", nd[:], cd["hy_ndelta"], (), ["ndelta"])
            for j in range(4):
                B.ACT(dec[:], tpos[:], AF.Exp, ["tpos", "ndelta"], ["decay"], scale=nd[:, j:j + 1])
                for (t0, tw) in tiles(L):
                    b1, b2 = B.bank(), B.bank()
                    B.MM(PS[:, b1, :tw], wo[:, j * 128:(j + 1) * 128], h3[:, t0:t0 + tw], True, True, ["hywo", "hyh1"], [psk(b1)])
                    B.MM(PS[:, b2, :tw], wo[:, 512 + j * 128:512 + (j + 1) * 128], h3[:, t0:t0 + tw], True, True, ["hywo", "hyh1"], [psk(b2)])
                    B.TT("dve", hf[:, t0:t0 + tw], PS[:, b1, :tw], dec[:, t0:t0 + tw], ALU.mult, [psk(b1), "decay"], ["hf"])
                    B.TT("dve", hb[:, t0:t0 + tw], PS[:, b2, :tw], dec[:, t0:t0 + tw], ALU.mult, [psk(b2), "decay"], ["hb"])
                B.MS("dve", hb[:, 0:1], 0.0, ["hb"])
                B.TT("dve", hs[:], hf[:], hb[:], ALU.add, ["hf", "hb"], ["hs"])
                B.TT("dve", hd[:], hf[:], hb[:], ALU.subtract, ["hf", "hb"], ["hd"])
                to_tm1(L, hs, "hs", hs_tm, "hs_tm", j)
                to_tm1(L, hd, "hd", hd_tm, "hd_tm", j)
            tab = B.sb(st, "hyfwdK", [128, 2, 2, TT_, 128], BF16)
            for ft in range(L // 128):
                tb = ft % 2
                B.DMA("sp", tab[:, tb], cd[f"hy_fwd{L}"][ft], (), [f"hyfwdK{tb}"])
                bre, bim = B.bank(), B.bank()
                for tt in range(TT_):
                    B.MM(PS[:, bre, :], tab[:, tb, 0, tt, :], hs_tm[:, tt, :], tt == 0, tt == TT_ - 1, [f"hyfwdK{tb}", "hs_tm"], [psk(bre)])
                for tt in range(TT_):
                    B.MM(PS[:, bim, :], tab[:, tb, 1, tt, :], hd_tm[:, tt, :], tt == 0, tt == TT_ - 1, [f"hyfwdK{tb}", "hd_tm"], [psk(bim)])
                B.CP("act", kst[:, tb, 0, :], PS[:, bre, :], [psk(bre)], [f"kst{tb}"])
                B.CP("dve", kst[:, tb, 1, :], PS[:, bim, :], [psk(bim)], [f"kst{tb}"])
                B.DMA("act", hyk_d[L][ft], kst[:, tb], [f"kst{tb}"], [f"hyk{L}.{ft}"])
        P.barrier()

    def phase_hyena(l, L, xn, xnk, wv):
        TT_ = L // 128
        IW = min(256, L)
        with ExitStack() as st:
            cw = B.sb(st, "hycw", [128, 3, 12], F32)
            cb = B.sb(st, "hycb", [128, 12], F32)
            db = B.sb(st, "hydb", [128, 4], F32)
            zT = B.sb(st, "zT", [128, 4, L], BF16)
            x0T = B.sb(st, "x0T", [128, 4, L], BF16)
            B.DMA("sp", cw[:], hycw_in[:, l], (), ["hycw"])
            B.DMA("sp", cb[:], hycb_in[:, l], (), ["hycw"])
            B.DMA("sp", db[:], hydb_in[:, l], (), ["hycw"])
            with ExitStack() as st1:
                wb = B.sb(st1, "why", [128, 2, 16, 384], BF16)
                hp = B.sb(st1, "hpad", [128, 3, L + 2], BF16)
                acc = B.sb(st1, "hyacc", [128, 2, L], F32)
                B.MS("pool", hp[:, :, 0:1], 0.0, ["hpad0", "hpad1", "hpad2"])
                B.MS("pool", hp[:, :, L + 1:L + 2], 0.0, ["hpad0", "hpad1", "hpad2"])
                for j in range(4):
                    wbb = j % 2
                    for q in range(3):
                        c0 = HY_OFF + q * 512 + j * 128
                        B.DMA("pool", wb[:, wbb, :, q * 128:(q + 1) * 128], wv[:, :, c0:c0 + 128], (), [f"why{wbb}"])

                    def ev(m, t0, tw, ps, pk):
                        B.CP("act", hp[:, m, 1 + t0:1 + t0 + tw], ps, [pk], [f"hpad{m}"])
                    proj_fm(L, xn, xnk, wb[:, wbb], f"why{wbb}", 384, ev)
                    for q in range(3):
                        ch = q * 4 + j
                        a_ = acc[:, q % 2, :]
                        ak = f"hyacc{q % 2}"
                        B.ACT(a_, hp[:, q, 1:L + 1], AF.Identity, [f"hpad{q}", "hycw"], [ak], bias=cb[:, ch:ch + 1], scale=cw[:, 1, ch:ch + 1])
                        B.STT(a_, hp[:, q, 0:L], cw[:, 0, ch:ch + 1], a_, ALU.mult, ALU.add, [f"hpad{q}", "hycw", ak], [ak])
                        if q < 2:
                            B.STT(a_, hp[:, q, 2:L + 2], cw[:, 2, ch:ch + 1], a_, ALU.mult, ALU.add, [f"hpad{q}", "hycw", ak], [ak])
                            if q == 1:
                                B.TT("dve", zT[:, j, :], acc[:, 0, :], acc[:, 1, :], ALU.mult, ["hyacc0", "hyacc1"], ["zT"])
                        else:
                            B.STT(x0T[:, j, :], hp[:, q, 2:L + 2], cw[:, 2, ch:ch + 1], a_, ALU.mult, ALU.add, [f"hpad{q}", "hycw", ak], ["x0T"])
            P.barrier()
            Pre = B.sb(st, "Pre", [128, TT_, 512], BF16)
            Pim = B.sb(st, "Pim", [128, TT_, 512], BF16)
            with ExitStack() as st2:
                z_tm = B.sb(st2, "z_tm", [128, TT_, 512], BF16)
                kk = B.sb(st2, "kk", [128, 2, 2, 512], F32)
                t1 = B.sb(st2, "hyt1", [128, 512], F32)
                t2 = B.sb(st2, "hyt2", [128, 512], F32)
                to_tm(L, zT, "zT", z_tm, "z_tm")

                def consume(ft, bre, bim):
                    kb_ = ft % 2
                    B.DMA("sp", kk[:, kb_], hyk_d[L][ft], [f"hyk{L}.{ft}"], [f"kk{kb_}"])
                    B.TT("dve", t1[:], PS[:, bre, :], kk[:, kb_, 0, :], ALU.mult, [psk(bre), f"kk{kb_}"], ["hyt1"])
                    B.TT("dve", t2[:], PS[:, bim, :], kk[:, kb_, 1, :], ALU.mult, [psk(bim), f"kk{kb_}"], ["hyt2"])
                    B.TT("pool", Pre[:, ft, :], t1[:], t2[:], ALU.subtract, ["hyt1", "hyt2"], ["Pre"])
                    B.TT("dve", t1[:], PS[:, bre, :], kk[:, kb_, 1, :], ALU.mult, [psk(bre), f"kk{kb_}"], ["hyt1"])
                    B.TT("dve", t2[:], PS[:, bim, :], kk[:, kb_, 0, :], ALU.mult, [psk(bim), f"kk{kb_}"], ["hyt2"])
                    B.TT("pool", Pim[:, ft, :], t1[:], t2[:], ALU.add, ["hyt1", "hyt2"], ["Pim"])
                fwd_dft(st2, L, z_tm, "z_tm", consume)
            P.barrier()
            with ExitStack() as st3:
                itab = B.sb(st3, "hyinv", [128, 2, 2, TT_, IW], BF16)
                yo = B.sb(st3, "hyyo", [128, 2, IW], F32)
                og = B.sb(st3, "cathy", [128, 4, L], BF16)
                for it in range(L // IW):
                    tb = it % 2
                    B.DMA("sp", itab[:, tb], cd[f"hy_inv{L}"][it], (), [f"hyinv{tb}"])
                    for j in range(4):
                        bk = B.bank()
                        n = 0
                        for ft in range(TT_):
                            for a, Pm, Pk in ((0, Pre, "Pre"), (1, Pim, "Pim")):
                                B.MM(PS[:, bk, :IW], Pm[:, ft, j * 128:(j + 1) * 128], itab[:, tb, a, ft, :], n == 0, n == 2 * TT_ - 1,
                                     [Pk, f"hyinv{tb}"], [psk(bk)])
                                n += 1
                        yb = (it * 4 + j) % 2
                        B.STT(yo[:, yb, :], zT[:, j, it * IW:(it + 1) * IW], db[:, j:j + 1], PS[:, bk, :IW], ALU.mult, ALU.add,
                              ["zT", "hycw", psk(bk)], [f"hyyo{yb}"])
                        B.TT("pool", og[:, j, it * IW:(it + 1) * IW], yo[:, yb, :], x0T[:, j, it * IW:(it + 1) * IW], ALU.mult,
                             [f"hyyo{yb}", "x0T"], [f"cathy.{j}"])
                for j in range(4):
                    B.DMA("sp", cat_d[L][512 + j * 128:512 + (j + 1) * 128, :], og[:, j, :], [f"cathy.{j}"], [f"cat{L}.{4 + j}"])
        P.barrier()

    def phase_hgrn(l, L, xn, xnk, wv, need_out, first):
        TT_ = L // 128
        NCH = L // 64
        with ExitStack() as st:
            wj = B.sb(st, "whg", [128, 2, 16, 128], BF16)
            rst = B.sb(st, "rst", [128, L], BF16)
            mskD = B.sb(st, "hgmaskD", [128, 2, 64], BF16)
            negm = B.sb(st, "hgnegm", [128, 2, 3, 64], F32)
            T0 = B.sb(st, "T0", [128, L], F32)
            T1 = B.sb(st, "T1", [128, L], F32)
            T2 = B.sb(st, "T2", [128, L], F32)
            qT = B.sb(st, "qT", [128, L], BF16)
            kT = B.sb(st, "kT", [128, L], BF16)
            qb = B.sb(st, "qb", [128, L], BF16)
            kb = B.sb(st, "kb", [128, L], BF16)
            qB = B.sb(st, "qB", [128, L], BF16)
            kdT = B.sb(st, "kdT", [128, L], BF16)
            kbi = B.sb(st, "kbi", [128, 3, L], BF16)
            qbi = B.sb(st, "qbi", [128, NCH, 3, 16], BF16)
            kd_tm = B.sb(st, "kd_tm", [128, TT_, 2, 128], BF16)
            v_tm = B.sb(st, "v_tm", [128, TT_, 128], BF16)
            oT = B.sb(st, "oT", [128, L], F32)
            Sall = B.sb(st, "Sall", [128, NCH, 128], BF16)
            HC = min(16, NCH)
            Sch = B.sb(st, "Sch", [128, HC + 1, 128], F32)
            eb = B.sb(st, "eb", [128, NCH], F32)
            Am = B.sb(st, "Am", [128, 2, 4, 64], BF16)
            B.DMA("sp", rst[:], cd[f"hg_rst{L}"], (), ["rst"])
            B.DMA("sp", mskD[:], cd["hg_maskD"], (), ["hgmask"])
            B.DMA("sp", negm[:], cd["hg_negm"], (), ["hgmask"])
            if first:
                B.MS("dve", S32[:], 0.0, ["S32"])
            B.MS("pool", kd_tm[:], 0.0, ["kd_tm"])
            B.MS("pool", Am[:], 0.0, ["Am0", "Am1"])
            b64 = lambda t: t[:].rearrange("p (n c) -> p n c", c=64)
            b16 = lambda t: t[:].rearrange("p (n c) -> p n c", c=16)
            b416 = lambda t: t[:].rearrange("p (n i c) -> p n i c", i=4, c=16)
            wcnt = [0]

            def loadj(h, j):
                wb_ = wcnt[0] % 2
                wcnt[0] += 1
                c0 = HG_OFF + j * HGW + h * 128
                B.DMA("pool", wj[:, wb_], wv[:, :, c0:c0 + 128], (), [f"whg{wb_}"])
                return wj[:, wb_], f"whg{wb_}"

            for h in range(DBG.get('heads', NH)):
                wb, wbk = loadj(h, 3)
                for tt in range(TT_):
                    bk = B.bank()
                    for dc in range(16):
                        B.MM(PS[:, bk, :128], xn[:, dc, tt * 128:(tt + 1) * 128], wb[:, dc, :], dc == 0, dc == 15,
                             [f"{xnk}.{dc}", wbk], [psk(bk)])
                    B.CP("act", v_tm[:, tt, :], PS[:, bk, :128], [psk(bk)], ["v_tm"])
                if need_out:
                    wb, wbk = loadj(h, 0)

                    def evq(m, t0, tw, ps, pk):
                        B.ACT(qT[:, t0:t0 + tw], ps, AF.Silu, [pk], ["qT"])
                    proj_fm(L, xn, xnk, wb, wbk, 128, evq)
                for dr in range(2):
                    sidx = h * 2 + dr
                    wb, wbk = loadj(h, 1 + dr)

                    def evf(m, t0, tw, ps, pk):
                        B.ACT(T0[:, t0:t0 + tw], ps, AF.Sigmoid, [pk], ["T0"])
                    proj_fm(L, xn, xnk, wb, wbk, 128, evf)
                    if l > 0:
                        B.TS("dve", T0[:], T0[:], oml[:, dr, h:h + 1], lbs[:, dr, h:h + 1], ALU.mult, ALU.add, ["T0", "oml", "lbs"], ["T0"])
                    B.TS("dve", T0[:], T0[:], 1e-6, None, ALU.max, None, ["T0"], ["T0"])
                    B.TS("pool", kT[:], T0[:], -1.0, 1.0, ALU.mult, ALU.add, ["T0"], ["kT"])
                    B.ACT(T0[:], T0[:], AF.Ln, ["T0"], ["T0"])
                    P.op("dve", lambda e: e.tensor_tensor_scan(T1[:], rst[:], T0[:], 0.0, ALU.mult, ALU.add),
                         ["rst", "T0"], ["T1"])
                    if dr == 1:
                        B.TT("dve", b64(T2), b64(T1)[:, :, 63:64].to_broadcast([128, NCH, 64]), b64(T1), ALU.subtract, ["T1"], ["T2"])
                        B.TT("dve", T1[:], T2[:], T0[:], ALU.add, ["T2", "T0"], ["T1"])
                    bend = b64(T1)[:, :, 63:64] if dr == 0 else b64(T1)[:, :, 0:1]
                    B.ACT(eb[:].unsqueeze(2), bend, AF.Exp, ["T1"], ["eb"])
                    B.TT("dve", b64(T2), bend.to_broadcast([128, NCH, 64]), b64(T1), ALU.subtract, ["T1"], ["T2"])
                    B.ACT(T2[:], T2[:], AF.Exp, ["T2"], ["T2"])
                    B.TT("dve", kdT[:], kT[:], T2[:], ALU.mult, ["kT", "T2"], ["kdT"])
                    for tt in range(TT_):
                        bk = B.bank()
                        pv = PS[:, bk, :].bitcast(BF16)
                        B.TR(pv[:, 0:128], kdT[:, tt * 128:(tt + 1) * 128], ident_bf[:], ["kdT", "ident_bf"], [psk(bk)])
                        B.CP("act", kd_tm[0:64, tt, 0, :], pv[0:64, 0:128], [psk(bk)], ["kd_tm"])
                        B.CP("dve", kd_tm[64:128, tt, 1, :], pv[64:128, 0:128], [psk(bk)], ["kd_tm"])
                    if need_out:
                        B.TT("dve", b16(T0), b16(T1), b16(T1)[:, :, 8:9].to_broadcast([128, L // 16, 16]), ALU.subtract, ["T1"], ["T0"])
                        B.ACT(T2[:], T0[:], AF.Exp, ["T0"], ["T2"])
                        B.TT("dve", qb[:], qT[:], T2[:], ALU.mult, ["qT", "T2"], ["qb"])
                        B.ACT(T2[:], T0[:], AF.Exp, ["T0"], ["T2"], scale=-1.0)
                        B.TT("dve", kb[:], kT[:], T2[:], ALU.mult, ["kT", "T2"], ["kb"])
                        B.ACT(T2[:], T1[:], AF.Exp, ["T1"], ["T2"])
                        B.TT("dve", qB[:], qT[:], T2[:], ALU.mult, ["qT", "T2"], ["qB"])
                        if dr == 0:
                            rsel = b64(T1)[:, :, 15:48:16]
                            i0 = 1
                        else:
                            rsel = b64(T1)[:, :, 16:64:16]
                            i0 = 0
                        tq = T0[:, 0:NCH * 48].rearrange("p (n i c) -> p n i c", i=3, c=16)
                        B.TT("dve", tq, b416(T1)[:, :, i0:i0 + 3, :], rsel.unsqueeze(3).to_broadcast([128, NCH, 3, 16]), ALU.subtract, ["T1"], ["T0"])
                        B.ACT(T0[:, 0:NCH * 48], T0[:, 0:NCH * 48], AF.Exp, ["T0"], ["T0"])
                        B.TT("dve", qbi[:], b416(qT)[:, :, i0:i0 + 3, :], tq, ALU.mult, ["qT", "T0"], ["qbi"])
                        for vi in range(3):
                            Tx, Txk = (T2, "T2") if vi % 2 == 0 else (T0, "T0")
                            B.TT("dve", b64(Tx), rsel[:, :, vi:vi + 1].to_broadcast([128, NCH, 64]), b64(T1), ALU.subtract, ["T1"], [Txk])
                            B.TT("dve", b64(Tx), b64(Tx), negm[:, dr, vi:vi + 1, :].to_broadcast([128, NCH, 64]), ALU.min, [Txk, "hgmask"], [Txk])
                            B.ACT(Tx[:], Tx[:], AF.Exp, [Txk], [Txk])
                            B.TT("dve", kbi[:, vi, :], kT[:], Tx[:], ALU.mult, ["kT", Txk], ["kbi"])
                    order = list(range(NCH)) if dr == 0 else list(range(NCH - 1, -1, -1))
                    pos_of = {n: p for p, n in enumerate(order)}
                    for h0 in range(0, NCH, HC):
                        B.CP("act", Sch[:, 0, :], S32[:, sidx, :], ["S32"], ["Sch"])
                        for gi in range(h0, h0 + HC, 4):
                            grp = order[gi:gi + 4]
                            bk = B.bank()
                            for q, n in enumerate(grp):
                                B.MM(PS[:, bk, q * 128:(q + 1) * 128], kd_tm[:, n // 2, n % 2, :], v_tm[:, n // 2, :], True, True,
                                     ["kd_tm", "v_tm"], [psk(bk)])
                            for q, n in enumerate(grp):
                                p = gi + q - h0
                                B.STT(Sch[:, p + 1, :], Sch[:, p, :], eb[:, n:n + 1], PS[:, bk, q * 128:(q + 1) * 128], ALU.mult, ALU.add,
                                      ["Sch", "eb", psk(bk)], ["Sch"])
                        if need_out:
                            B.CP("act", Sall[:, h0:h0 + HC, :], Sch[:, 0:HC, :], ["Sch"], ["Sall"])
                        B.CP("pool", S32[:, sidx, :], Sch[:, HC, :], ["Sch"], ["S32"])
                    if need_out:
                        c0 = 16 if dr == 0 else 0
                        for gi in range(0, NCH, 4):
                            grp = list(range(gi, gi + 4))
                            ab = (gi // 4) % 2
                            bkD, bkF = B.bank(), B.bank()
                            for q, n in enumerate(grp):
                                pb = (n % 2) * 64
                                ne = n - (n % 2)
                                B.MM(PS[:, bkD, q * 64:(q + 1) * 64], kb[:, ne * 64:(ne + 2) * 64], qb[:, n * 64:(n + 1) * 64], True, True,
                                     ["kb", "qb"], [psk(bkD)])
                                for vi in range(3):
                                    i = vi + i0
                                    B.MM(PS[:, bkF, q * 64 + i * 16:q * 64 + (i + 1) * 16], kbi[:, vi, ne * 64:(ne + 2) * 64], qbi[:, n, vi, :], True, True,
                                         ["kbi", "qbi"], [psk(bkF)])
                            for half in range(2):
                                pb = half * 64
                                pd = PS[pb:pb + 64, bkD, 0:256].rearrange("p (q c) -> p q c", c=64)[:, half::2, :]
                                pf = PS[pb:pb + 64, bkF, 0:256].rearrange("p (q c) -> p q c", c=64)[:, half::2, c0:c0 + 48]
                                am = Am[pb:pb + 64, ab, half::2, :]
                                B.TT("dve", am, pd, mskD[pb:pb + 64, dr:dr + 1, :].to_broadcast([64, 2, 64]), ALU.mult,
                                     [psk(bkD), "hgmask"], [f"Am{ab}"])
                                B.TT("dve", am[:, :, c0:c0 + 48], am[:, :, c0:c0 + 48], pf, ALU.add, [psk(bkF), f"Am{ab}"], [f"Am{ab}"])
                            bkO = B.bank()
                            for q, n in enumerate(grp):
                                pb = (n % 2) * 64
                                B.MM(PS[:, bkO, q * 64:(q + 1) * 64], Sall[:, pos_of[n], :], qB[:, n * 64:(n + 1) * 64], True, False,
                                     ["Sall", "qB"], [psk(bkO)])
                                B.MM(PS[:, bkO, q * 64:(q + 1) * 64], v_tm[:, n // 2, :], Am[:, ab, q, :], False, True,
                                     ["v_tm", f"Am{ab}"], [psk(bkO)])
                            if dr == 0:
                                B.CP("act", oT[:, gi * 64:(gi + 4) * 64], PS[:, bkO, 0:256], [psk(bkO)], ["oT"])
                            else:
                                B.TT("dve", oT[:, gi * 64:(gi + 4) * 64], oT[:, gi * 64:(gi + 4) * 64], PS[:, bkO, 0:256], ALU.add,
                                     [psk(bkO), "oT"], ["oT"])
                if need_out:
                    wb, wbk = loadj(h, 4)

                    def evg(m, t0, tw, ps, pk):
                        B.ACT(qb[:, t0:t0 + tw], ps, AF.Silu, [pk], ["qb"])
                    proj_fm(L, xn, xnk, wb, wbk, 128, evg)
                    B.ACT(T0[:], oT[:], AF.Square, ["oT"], ["T0"])
                    for (t0, tw) in tiles(L):
                        bk = B.bank()
                        B.MM(PS[:, bk, :tw], ones_f[:], T0[:, t0:t0 + tw], True, True, ["T0", "ones_f"], [psk(bk)])
                        B.ACT(T1[:, t0:t0 + tw], PS[:, bk, :tw], AF.Ln, [psk(bk)], ["T1"], bias=epsc[:, 0:1], scale=1.0 / 128)
                    B.ACT(T1[:], T1[:], AF.Exp, ["T1"], ["T1"], scale=-0.5)
                    B.TT("dve", T1[:], T1[:], oT[:], ALU.mult, ["T1", "oT"], ["T1"])
                    B.STT(kb[:], T1[:], hgg[:, l, h:h + 1], qb[:], ALU.mult, ALU.mult, ["T1", "hgg", "qb"], ["kb"])
                    B.DMA("sp", cat_d[L][1024 + h * 128:1024 + (h + 1) * 128, :], kb[:], ["kb"], [f"cat{L}.{8 + h}"])
        P.barrier()

    def phase_outproj(l, L, s):
        with ExitStack() as st:
            xn = B.sb(st, "catT", [128, 16, L], BF16)
            wb = B.sb(st, "wo", [128, 2, 16, 512], BF16)
            xl = B.sb(st, "xl", [128, 2, L], F32)
            wv = w_out[l].rearrange("(dc p) n -> p dc n", p=128)
            for dc in range(16):
                B.DMA("sp", xn[:, dc, :L], cat_d[L][dc * 128:(dc + 1) * 128, :], [f"cat{L}.{dc}"], [f"catT.{dc}"])
            for blk in range(4):
                wbb = blk % 2
                B.DMA("pool", wb[:, wbb], wv[:, :, blk * 512:(blk + 1) * 512], (), [f"wo{wbb}"])
                for m4 in range(4):
                    m = blk * 4 + m4
                    xb = m % 2
                    B.DMA("sp", xl[:, xb, :], xres[L][m * 128:(m + 1) * 128, :], [f"xres{L}.{m}"], [f"xl{xb}"])
                    for (t0, tw) in tiles(L):
                        bk = B.bank()
                        for dc in range(16):
                            B.MM(PS[:, bk, :tw], wb[:, wbb, dc, m4 * 128:(m4 + 1) * 128], xn[:, dc, t0:t0 + tw], dc == 0, dc == 15,
                                 [f"wo{wbb}", f"catT.{dc}"], [psk(bk)])
                        B.STT(xl[:, xb, t0:t0 + tw], PS[:, bk, :tw], modT[:, 32 + m, s:s + 1], xl[:, xb, t0:t0 + tw], ALU.mult, ALU.add,
                              [psk(bk), "modT", f"xl{xb}"], [f"xl{xb}"])
                    B.DMA("act", xres[L][m * 128:(m + 1) * 128, :], xl[:, xb, :], [f"xl{xb}"], [f"xres{L}.{m}"])
        P.barrier()

    def phase_moe(l, streams):
        class S_:
            pass
        with ExitStack() as st:
            R = B.sb(st, "moeR", [128, 16 * SEQ], BF16)
            wr32 = B.sb(st, "wr32", [128, 16, NE], F32)
            selm = B.sb(st, "selm", [16, 16, 128], F32)
            pidx = B.sb(st, "pidx", [128, 16], F32)
            B.DMA("sp", wr32[:], wr_in[l].rearrange("(dc p) e -> p dc e", p=128), (), ["wr32"])
            B.DMA("sp", selm[:], cd["selmat"], (), ["selm"])
            B.DMA("sp", pidx[:], cd["pidx"], (), ["pidx"])
            sts = []
            for (L, s) in streams:
                c = S_()
                c.L, c.s = L, s
                c.TT = L // 128
                c.cap = 2 * L // NE
                c.nit = c.cap // 8
                c.JH = (c.cap + 127) // 128
                c.jw = min(c.cap, 128)
                c.NR = NE * c.JH
                c.k = f"m{L}"
                c.xn_tm = B.sb(st, "xn_tm", [128, c.TT, D], BF16)
                c.vals = B.sb(st, "vals", [16, c.cap], F32)
                c.idxu = B.sb(st, "idxu", [16, c.cap], U32)
                c.idxf = B.sb(st, "idxf", [16, c.cap], F32)
                c.idxT = B.sb(st, "idxT", [128, c.JH, NE], F32)
                c.valT = B.sb(st, "valT", [128, c.JH, NE], F32)
                sts.append(c)
            lb = 3
            for c in sts:
                L, TT_, cap, JH, jw, k = c.L, c.TT, c.cap, c.JH, c.jw, c.k
                xn = R[:, 0:16 * L].rearrange("p (a b) -> p a b", b=L)
                with ExitStack() as st2:
                    affT = B.sb(st2, "affT", [16, L], F32)
                    aff = B.sb(st2, "aff", [128, TT_, NE], F32)
                    mx = B.sb(st2, "mx", [128, TT_], F32)
                    with ExitStack() as st2a:
                        normmod(st2a, L, c.s, A2, "A2", 48, xn, "xn2", router=(wr32, lb), nbuf=1)
                        lg = PS[:, lb, 0:TT_ * 16].rearrange("p (t e) -> p t e", e=16)
                        P.op("dve", lambda e, mx=mx, lg=lg: e.tensor_reduce(mx[:], lg, AX.X, ALU.max), [psk(lb)], ["mx"])
                        B.TT("dve", aff[:], lg, mx[:].unsqueeze(2).to_broadcast([128, TT_, 16]), ALU.subtract, [psk(lb), "mx"], ["aff"])
                        B.ACT(aff[:], aff[:], AF.Exp, ["aff"], ["aff"])
                        P.op("dve", lambda e, mx=mx, aff=aff: e.tensor_reduce(mx[:], aff[:], AX.X, ALU.add), ["aff"], ["mx"])
                        P.op("dve", lambda e, mx=mx: e.reciprocal(mx[:], mx[:]), ["mx"], ["mx"])
                        B.TT("dve", aff[:], aff[:], mx[:].unsqueeze(2).to_broadcast([128, TT_, 16]), ALU.mult, ["aff", "mx"], ["aff"])
                    P.barrier()
                    for g0 in range(0, TT_, 4):
                        bk = B.bank()
                        n = min(4, TT_ - g0)
                        for q in range(n):
                            B.TR(PS[:16, bk, q * 128:(q + 1) * 128], aff[:, g0 + q, :], ident_f[:], ["aff", "ident_f"], [psk(bk)])
                        B.CP("act", affT[:, g0 * 128:(g0 + n) * 128], PS[:16, bk, 0:n * 128], [psk(bk)], ["affT"])
                    for tt in range(TT_):
                        for q4 in range(4):
                            bk = B.bank()
                            pv = PS[:, bk, :].bitcast(BF16)
                            for q in range(4):
                                dc = q4 * 4 + q
                                B.TR(pv[:, q * 128:(q + 1) * 128], xn[:, dc, tt * 128:(tt + 1) * 128], ident_bf[:], [f"xn2.{dc}", "ident_bf"], [psk(bk)])
                            B.CP("act", c.xn_tm[:, tt, q4 * 512:(q4 + 1) * 512], pv[:, 0:512], [psk(bk)], [f"xn_tm{k}"])
                    for it in range(c.nit):
                        sl = slice(it * 8, (it + 1) * 8)
                        P.op("dve", lambda e, sl=sl, c=c, affT=affT: e.max(c.vals[:, sl], affT[:]), ["affT"], [f"vals{k}"])
                        P.op("dve", lambda e, sl=sl, c=c, affT=affT: e.max_index(c.idxu[:, sl], c.vals[:, sl], affT[:]), ["affT", f"vals{k}"], [f"idxu{k}"])
                        if it < c.nit - 1:
                            P.op("dve", lambda e, sl=sl, c=c, affT=affT: e.match_replace(affT[:], c.vals[:, sl], affT[:], -1.0), ["affT", f"vals{k}"], ["affT"])
                    B.CP("dve", c.idxf[:], c.idxu[:], [f"idxu{k}"], [f"idxf{k}"])
                    for hh in range(JH):
                        bk = B.bank()
                        B.TR(PS[:jw, bk, 0:16], c.idxf[:, hh * 128:hh * 128 + jw], ident_f[:16, :16], [f"idxf{k}", "ident_f"], [psk(bk)])
                        B.TR(PS[:jw, bk, 16:32], c.vals[:, hh * 128:hh * 128 + jw], ident_f[:16, :16], [f"vals{k}", "ident_f"], [psk(bk)])
                        B.CP("dve", c.idxT[:jw, hh, :], PS[:jw, bk, 0:16], [psk(bk)], [f"idxT{k}"])
                        B.CP("dve", c.valT[:jw, hh, :], PS[:jw, bk, 16:32], [psk(bk)], [f"valT{k}"])
                P.barrier()
            EG = 2
            CW = sum(c.cap for c in sts)
            offs = []
            o_ = 0
            for c in sts:
                offs.append(o_)
                o_ += c.cap
            Selg = R[:, 0:8192]
            wg = R[:, 8192:16384].rearrange("p (w a b) -> p w a b", w=2, b=256)
            wu = R[:, 16384:24576].rearrange("p (w a b) -> p w a b", w=2, b=256)
            wd = R[:, 24576:32768].rearrange("p (w a b) -> p w a b", w=2, b=512)
            rgs = []
            for si, c in enumerate(sts):
                for hh in range(c.JH):
                    rgs.append((si, hh, offs[si] + hh * 128, c.jw))
            with ExitStack() as st3:
                xsT = B.sb(st3, "xsT", [128, 16, EG, CW], BF16)
                hidT = B.sb(st3, "hidT", [128, 8, CW], BF16)
                sg_ = B.sb(st3, "moesg", [128, 1, CW], F32)
                ysb = B.sb(st3, "ysb", [128, 2, len(rgs), D], BF16)
                for eg in range(NE // EG):
                    for si, c in enumerate(sts):
                        cap, TT_, k = c.cap, c.TT, c.k
                        gw = EG * cap
                        Sg = Selg[:, 0:TT_ * gw].rearrange("p (a b) -> p a b", b=gw)
                        bk = B.bank()
                        for q in range(EG):
                            e_ = eg * EG + q
                            B.MM(PS[:, bk, q * cap:(q + 1) * cap], selm[:, e_, :], c.idxf[:], True, True, ["selm", f"idxf{k}"], [psk(bk)])
                        for tt in range(TT_):
                            B.TS("dve", Sg[:, tt, :], PS[:, bk, :gw], pidx[:, tt:tt + 1], None, ALU.is_equal, None, [psk(bk), "pidx"], ["Selg"])
                        for m in range(16):
                            bk2 = B.bank()
                            for tt in range(TT_):
                                B.MM(PS[:, bk2, :gw], c.xn_tm[:, tt, m * 128:(m + 1) * 128], Sg[:, tt, :], tt == 0, tt == TT_ - 1,
                                     [f"xn_tm{k}", "Selg"], [psk(bk2)])
                            B.CP("act" if m % 2 else "dve", xsT[:, m, :, offs[si]:offs[si] + cap],
                                 PS[:, bk2, :gw].rearrange("p (q c) -> p q c", c=cap), [psk(bk2)], ["xsT"])
                    for q in range(EG):
                        e_ = eg * EG + q
                        gv = wg_in[l, e_].rearrange("(dc p) f -> p dc f", p=128)
                        uv = wu_in[l, e_].rearrange("(dc p) f -> p dc f", p=128)
                        dv = wd_in[l, e_].rearrange("(fc p) d -> p fc d", p=128)
                        for fb_ in range(4):
                            wb_ = fb_ % 2
                            B.DMA("pool", wg[:, wb_], gv[:, :, fb_ * 256:(fb_ + 1) * 256], (), [f"wg{wb_}"])
                            B.DMA("pool", wu[:, wb_], uv[:, :, fb_ * 256:(fb_ + 1) * 256], (), [f"wu{wb_}"])
                            for f2 in range(2):
                                fc = fb_ * 2 + f2
                                bg, bu = B.bank(), B.bank()
                                for dc in range(16):
                                    B.MM(PS[:, bg, :CW], wg[:, wb_, dc, f2 * 128:(f2 + 1) * 128], xsT[:, dc, q, :], dc == 0, dc == 15,
                                         [f"wg{wb_}", "xsT"], [psk(bg)])
                                for dc in range(16):
                                    B.MM(PS[:, bu, :CW], wu[:, wb_, dc, f2 * 128:(f2 + 1) * 128], xsT[:, dc, q, :], dc == 0, dc == 15,
                                         [f"wu{wb_}", "xsT"], [psk(bu)])
                                sb_ = 0
                                B.ACT(sg_[:, sb_, :], PS[:, bg, :CW], AF.Silu, [psk(bg)], [f"moesg{sb_}"])
                                B.TT("dve", hidT[:, fc, :], sg_[:, sb_, :], PS[:, bu, :CW], ALU.mult, [f"moesg{sb_}", psk(bu)], ["hidT"])
                        yb = e_ % 2
                        for db_ in range(4):
                            wb_ = db_ % 2
                            B.DMA("pool", wd[:, wb_], dv[:, :, db_ * 512:(db_ + 1) * 512], (), [f"wd{wb_}"])
                            for ri, (si, hh, co, rows) in enumerate(rgs):
                                bk3 = B.bank()
                                for fc in range(8):
                                    B.MM(PS[:rows, bk3, :], hidT[:, fc, co:co + rows], wd[:, wb_, fc, :], fc == 0, fc == 7,
                                         ["hidT", f"wd{wb_}"], [psk(bk3)])
                                B.CP("act" if ri % 2 else "dve", ysb[:rows, yb, ri, db_ * 512:(db_ + 1) * 512], PS[:rows, bk3, :], [psk(bk3)], [f"ysb{yb}"])
                        for ri, (si, hh, co, rows) in enumerate(rgs):
                            c = sts[si]
                            B.DMA("sp", ys_d[c.L][:, :rows, e_ * c.JH + hh, :].rearrange("m j d -> j m d"),
                                  ysb[:rows, yb, ri, :].rearrange("j (m d) -> j m d", d=128), [f"ysb{yb}"], [f"ys{c.k}.{e_}.{hh}"])
            P.barrier()
            for c in sts:
                L, JH, jw, NR, k = c.L, c.JH, c.jw, c.NR, c.k
                SelT = R[:, 0:2 * NR * 512].rearrange("p (w r c) -> p w r c", w=2, c=512)
                with ExitStack() as st4:
                    ysm = B.sb(st4, "ysm", [128, 2, NR, 128], BF16)
                    xl = B.sb(st4, "xl2", [128, 2, 512], F32)
                    iot = B.sb(st4, "iot", [128, L], F32)
                    B.DMA("sp", iot[:], cd["iota_t"][:, :L], (), ["iot"])
                    for ti, (t0, tw) in enumerate(tiles(L)):
                        sb_ = ti % 2
                        for e_ in range(NE):
                            for hh in range(JH):
                                r_ = e_ * JH + hh
                                B.TS("dve", SelT[:jw, sb_, r_, :tw], iot[:jw, t0:t0 + tw], c.idxT[:jw, hh, e_:e_ + 1], c.valT[:jw, hh, e_:e_ + 1],
                                     ALU.is_equal, ALU.mult, ["iot", f"idxT{k}", f"valT{k}"], [f"SelT{sb_}"])
                        for m in range(16):
                            mb = m % 2
                            B.DMA("sp", ysm[:jw, mb], ys_d[L][m, :jw, 0:NR, :],
                                  [f"ys{k}.{e_}.{hh}" for e_ in range(NE) for hh in range(JH)], [f"ysm{mb}"])
                            B.DMA("sp", xl[:, mb, :tw], xres[L][m * 128:(m + 1) * 128, t0:t0 + tw], [f"xres{L}.{m}"], [f"xl2{mb}"])
                            bk = B.bank()
                            for r_ in range(NR):
                                B.MM(PS[:, bk, :tw], ysm[:jw, mb, r_, :], SelT[:jw, sb_, r_, :tw], r_ == 0, r_ == NR - 1,
                                     [f"ysm{mb}", f"SelT{sb_}"], [psk(bk)])
                            B.STT(xl[:, mb, :tw], PS[:, bk, :tw], modT[:, 80 + m, c.s:c.s + 1], xl[:, mb, :tw], ALU.mult, ALU.add,
                                  [psk(bk), "modT", f"xl2{mb}"], [f"xl2{mb}"])
                            B.DMA("act", xres[L][m * 128:(m + 1) * 128, t0:t0 + tw], xl[:, mb, :tw], [f"xl2{mb}"], [f"xres{L}.{m}"])
                P.barrier()

    def phase_final():
        L = SEQ
        with ExitStack() as st:
            xld = B.sb(st, "fxld", [128, 2, L], F32)
            sq = B.sb(st, "fsq", [128, 2, L], F32)
            rstd = B.sb(st, "frstd", [128, L], F32)
            for dc in range(16):
                b_ = dc % 2
                B.DMA("sp", xld[:, b_, :], xres[L][dc * 128:(dc + 1) * 128, :], [f"xres{L}.{dc}"], [f"fxld{b_}"])
                B.ACT(sq[:, b_, :], xld[:, b_, :], AF.Square, [f"fxld{b_}"], [f"fsq{b_}"])
                for i, (t0, tw) in enumerate(tiles(L)):
                    B.MM(PS[:, 4 + i, :tw], ones_f[:], sq[:, b_, t0:t0 + tw], dc == 0, dc == 15, [f"fsq{b_}", "ones_f"], [psk(4 + i)])
            for i, (t0, tw) in enumerate(tiles(L)):
                B.ACT(rstd[:, t0:t0 + tw], PS[:, 4 + i, :tw], AF.Ln, [psk(4 + i)], ["frstd"], bias=epsc[:, 0:1], scale=1.0 / D)
            B.ACT(rstd[:], rstd[:], AF.Exp, ["frstd"], ["frstd"], scale=-0.5)
            for dc in range(16):
                b_ = dc % 2
                B.DMA("sp", xld[:, b_, :], xres[L][dc * 128:(dc + 1) * 128, :], [f"xres{L}.{dc}"], [f"fxld{b_}"])
                B.STT(sq[:, b_, :], xld[:, b_, :], gfin[:, dc:dc + 1], rstd[:], ALU.mult, ALU.mult, [f"fxld{b_}", "gfin", "frstd"], [f"fsq{b_}"])
                B.DMA("act", out_d[dc * 128:(dc + 1) * 128, :], sq[:, b_, :], [f"fsq{b_}"], [f"out.{dc}"])
        P.barrier()

    nstage = 0

    def go():
        nonlocal nstage
        nstage += 1
        return nstage <= stage and nstage not in skip

    for l in range(DEPTH):
        last = l == DEPTH - 1
        wv = w_in[l].rearrange("(dc p) n -> p dc n", p=128)
        if go():
            phase_mod(l)
        if go():
            with ExitStack() as sx:
                XN = B.sb(sx, "XNc", [128, 16, CTX], BF16)
                with ExitStack() as st:
                    normmod(st, CTX, 1, A1, "A1", 0, XN, "XN")
                P.barrier()
                parts = DBG.get("ctx_parts", ("hgrn", "fourier", "hyena", "outproj"))
                if "hgrn" in parts:
                    phase_hgrn(l, CTX, XN, "XN", wv, need_out=(not last) and DBG.get("ctx_need_out", True), first=True)
                if not last:
                    if "fourier" in parts:
                        phase_fourier(l, CTX, XN, "XN", wv)
                    if "hyena" in parts:
                        phase_hyfilter(l, CTX)
                        phase_hyena(l, CTX, XN, "XN", wv)
            if not last and "outproj" in parts:
                phase_outproj(l, CTX, 1)
        with ExitStack() as sx:
            XN = B.sb(sx, "XNx", [128, 16, SEQ], BF16)
            if go():
                with ExitStack() as st:
                    normmod(st, SEQ, 0, A1, "A1", 0, XN, "XN")
                P.barrier()
            if go():
                phase_fourier(l, SEQ, XN, "XN", wv)
            if go():
                phase_hyfilter(l, SEQ)
                phase_hyena(l, SEQ, XN, "XN", wv)
            if go():
                phase_hgrn(l, SEQ, XN, "XN", wv, need_out=True, first=False)
        if go():
            phase_outproj(l, SEQ, 0)
        if go():
            phase_moe(l, [(CTX, 1)] if DBG.get("moe_ctx_only") else [(SEQ, 0)] + ([] if last else [(CTX, 1)]))
        go()
    if go():
        phase_final()
    else:
        with ExitStack() as st:
            z = B.sb(st, "zz", [128, SEQ], F32)
            B.MS("dve", z[:], 0.0, ["zz"])
            for dc in range(16):
                B.DMA("sp", out_d[dc * 128:(dc + 1) * 128, :], z[:], ["zz"], [f"out.{dc}"])
    info = P.emit()
    info["names"] = P.names
    info["sites"] = [o["site"] for o in P.ops]
    info["in_shapes"] = {k: tuple(v.shape) for k, v in B.inputs.items()}
    B.st.close()
    return nc, consts, info


_PROG = {}


def prep_inputs(inp, b, consts):
    m = {}
    m["xT"] = np.ascontiguousarray(inp["x"][b].T)
    m["ctxT"] = np.ascontiguousarray(inp["ctx"][b].T)
    cc = np.stack([pmaj(inp["c"][b], 16), pmaj(inp["c_ctx"], 16)], axis=-1)
    m["cT"] = np.ascontiguousarray(cc)
    m["gmix"] = pmaj(inp["norm_mix_g"], 16)
    m["gffn"] = pmaj(inp["norm_ffn_g"], 16)
    m["gfin"] = pmaj(inp["final_norm_g"], 16)
    m["bmod"] = pmaj(inp["b_mod"], 96)
    m["hycw"] = pmaj(inp["hy_conv_w"], 12)
    m["hycb"] = pmaj(inp["hy_conv_b"], 12)
    m["hyb"] = np.ascontiguousarray(np.stack([inp["hy_b1"], inp["hy_b2"], inp["hy_b3"]], axis=-1).transpose(1, 0, 2)).astype(np.float32)
    m["hyfr"] = np.ascontiguousarray(np.asarray(inp["hy_freq"], np.float32).transpose(2, 0, 1))
    m["hydb"] = pmaj(inp["hy_bias"], 4)
    m["hglb"] = pmaj(inp["hg_lb"], 8)
    m["hgg"] = pmaj(inp["hg_norm_g"], 8)
    for k in ("w_mod", "w_in", "w_out", "w_fnet", "hy_w1", "hy_w2", "hy_w3", "hy_w_out", "w_router", "w_gate", "w_up", "w_down"):
        m[k] = np.ascontiguousarray(np.asarray(inp[k], np.float32))
    m.update(consts)
    return m


def kernel(**inputs):
    if "full" not in _PROG:
        _PROG["full"] = build()
    nc, consts, info = _PROG["full"]
    n = 8
    in_maps = [prep_inputs(inputs, b, consts) for b in range(n)]
    res = run_bass_kernel_spmd(nc, in_maps, core_ids=list(range(n)))
    out = np.stack([np.ascontiguousarray(res.results[b]["outT"].T) for b in range(n)], axis=0)
    return out.astype(np.float32)
```

```python
import math
from contextlib import ExitStack

import numpy as np
import ml_dtypes

import concourse.bass as bass
import concourse.mybir as mybir
from concourse.bass_utils import run_bass_kernel_spmd

F32 = mybir.dt.float32
BF16 = mybir.dt.bfloat16
U32 = mybir.dt.uint32
AF = mybir.ActivationFunctionType
ALU = mybir.AluOpType
AX = mybir.AxisListType

D = 2048
SEQ = 2048
CTX = 256
DEPTH = 2
NE = 16
FF = 1024
INW = 7168
HY_OFF = 512
HG_OFF = 2048
HGW = 1024
NH = 8
EPS = 1e-6
PI = math.pi
TRACE_SITES = False
DBG = {}


class Prog:
    ENGS = ("pe", "act", "dve", "pool", "sp")
    HND = {"pe": "tensor", "act": "scalar", "dve": "vector", "pool": "gpsimd", "sp": "sync"}

    def __init__(self, nc, n_dma_sems=12):
        self.nc = nc
        self.ops = []
        self.n_dma_sems = n_dma_sems
        self.bar = set()
        self.since_bar = []
        self.trace_sites = False
        self.names = {}

    def op(self, eng, fn, reads=(), writes=(), dma=False):
        if len(self.ops) >= DBG.get("max_ops", 10 ** 9):
            return -1
        site = None
        if self.trace_sites:
            import sys as _sys
            f = _sys._getframe(1)
            site = []
            while f is not None and len(site) < 4:
                if f.f_code.co_name not in ("MM", "TR", "ACT", "TS", "TT", "STT", "CP", "MS", "DMA"):
                    site.append(f"{f.f_code.co_name}:{f.f_lineno}")
                f = f.f_back
        self.ops.append(dict(eng=eng, fn=fn, reads=list(reads), writes=list(writes), dma=dma,
                             bar=frozenset(self.bar), site=site))
        self.since_bar.append(len(self.ops) - 1)
        return len(self.ops) - 1

    def barrier(self):
        last = {}
        dm = []
        for i in self.since_bar:
            o = self.ops[i]
            if o["dma"]:
                dm.append(i)
            else:
                last[o["eng"]] = i
        new = set(last.values()) | set(dm)
        for i in self.bar:
            o = self.ops[i]
            if not o["dma"] and o["eng"] not in last:
                new.add(i)
        self.bar = new
        self.since_bar = []

    def emit(self, final_wait_eng="sp"):
        nc = self.nc
        ops = self.ops
        last_w = {}
        readers = {}
        for i, o in enumerate(ops):
            deps = set(o["bar"])
            for r in o["reads"]:
                if r in last_w:
                    deps.add(last_w[r])
            for w in o["writes"]:
                if w in last_w:
                    deps.add(last_w[w])
                for j in readers.get(w, ()):
                    deps.add(j)
            deps.discard(i)
            if o["eng"] == "pe":
                deps = {j for j in deps if ops[j]["eng"] != "pe"}
            latest = {}
            keep = set()
            for j in deps:
                if ops[j]["dma"]:
                    keep.add(j)
                else:
                    e2 = ops[j]["eng"]
                    if j > latest.get(e2, -1):
                        latest[e2] = j
            deps = keep | set(latest.values())
            o["deps"] = deps
            for r in o["reads"]:
                readers.setdefault(r, []).append(i)
            for w in o["writes"]:
                last_w[w] = i
                readers[w] = []
        needed = set()
        for o in ops:
            needed |= o["deps"]
        tail = [i for i, o in enumerate(ops) if o["dma"] and i not in needed]
        needed |= set(tail)
        cnt = {e: 0 for e in self.ENGS}
        dcnt = {e: 0 for e in self.ENGS}
        for i, o in enumerate(ops):
            e = o["eng"]
            if o["dma"]:
                k = dcnt[e]
                dcnt[e] += 1
                o["sem"] = ("d", e, k % self.n_dma_sems)
                o["val"] = 16 * (k // self.n_dma_sems + 1)
                o["prev_val"] = 16 * (k // self.n_dma_sems)
            elif i in needed:
                cnt[e] += 1
                o["sem"] = ("c", e)
                o["val"] = cnt[e]
            else:
                o["sem"] = None
        st = ExitStack()
        sems = {}
        for e in self.ENGS:
            if cnt[e]:
                sems[("c", e)] = st.enter_context(nc.semaphore(f"s_{e}"))
            for k in range(min(dcnt[e], self.n_dma_sems)):
                sems[("d", e, k)] = st.enter_context(nc.semaphore(f"d_{e}{k}"))
        block = st.enter_context(nc.Block())

        def make(e):
            def body(eh):
                waited = {}

                def wait(semkey, val):
                    if waited.get(semkey, 0) >= val:
                        return
                    eh.wait_ge(sems[semkey], val)
                    waited[semkey] = val

                for i, o in enumerate(ops):
                    if o["eng"] != e:
                        continue
                    for j in sorted(o["deps"]):
                        wait(ops[j]["sem"], ops[j]["val"])
                    if o["dma"] and o["prev_val"] > 0:
                        wait(o["sem"], o["prev_val"])
                    ins = o["fn"](eh)
                    if o["site"] is not None:
                        self.names[ins.ins.name] = o["site"]
                    if o["sem"] is not None:
                        ins.then_inc(sems[o["sem"]], 16 if o["dma"] else 1)
                if e == final_wait_eng:
                    for i in tail:
                        wait(ops[i]["sem"], ops[i]["val"])
            return body

        for e in self.ENGS:
            if any(o["eng"] == e for o in ops) or e == final_wait_eng:
                getattr(block, self.HND[e])(make(e))
        st.close()
        return dict(cnt=cnt, dcnt=dcnt, nops=len(ops))


def _bf(a):
    return np.ascontiguousarray(a.astype(np.float32)).astype(ml_dtypes.bfloat16)


def host_consts():
    c = {}
    c["ident_bf"] = _bf(np.eye(128))
    c["ident_f"] = np.eye(128, dtype=np.float32)
    c["ones_f"] = np.ones((128, 128), np.float32)
    k = np.arange(128, dtype=np.float64)
    ang = 2 * np.pi * ((k[:, None] * k[None, :]) % 128) / 128
    c["fc_cs"] = _bf(np.concatenate([np.cos(ang), np.sin(ang)], axis=1) / math.sqrt(128))
    for L in (SEQ, CTX):
        TT = L // 128
        FW = min(256, L)
        t = np.arange(L, dtype=np.int64)
        f = np.arange(L, dtype=np.int64)
        ang = 2 * np.pi * ((t[:, None] * f[None, :]) % L) / L
        cs = np.stack([np.cos(ang), -np.sin(ang)], 0) / math.sqrt(L)
        tab = cs.reshape(2, TT, 128, L // FW, FW).transpose(3, 2, 0, 1, 4)
        c[f"fn_tab{L}"] = _bf(tab)
        N4 = 4 * L
        ang = 2 * np.pi * (((2 * f[None, :] + 1) * t[:, None]) % N4) / N4
        cs = np.stack([np.cos(ang), -np.sin(ang)], 0)
        FT = L // 128
        tab = cs.reshape(2, TT, 128, FT, 128).transpose(3, 2, 0, 1, 4)
        c[f"hy_fwd{L}"] = _bf(tab)
        IW = min(256, L)
        csi = cs.transpose(0, 2, 1) / L
        tab = csi.reshape(2, FT, 128, L // IW, IW).transpose(3, 2, 0, 1, 4)
        c[f"hy_inv{L}"] = _bf(tab)
        pos = np.arange(L, dtype=np.float32)
        tt_ = (pos / max(L - 1, 1))[:, None]
        bands = np.linspace(1e-4, 15, 16, dtype=np.float32)
        w = (2.0 * np.float32(math.pi) * pos[:, None] / np.float32(L)).astype(np.float32)
        z = np.concatenate([tt_, np.cos(bands * w), -np.sin(bands * w)], axis=-1).astype(np.float32)
        c[f"hy_z{L}"] = np.ascontiguousarray(z.T)
        c[f"hy_tpos{L}"] = np.ascontiguousarray(np.broadcast_to(tt_[:, 0][None, :], (128, L))).astype(np.float32)
        r = np.ones((128, L), np.float32)
        r[:, ::64] = 0.0
        c[f"hg_rst{L}"] = _bf(r)
    max_decay = math.log(1e-2) / 0.3
    min_decay = math.log(1e-2) / 1.5
    deltas = np.linspace(min_decay, max_decay, 512, dtype=np.float32)
    c["hy_ndelta"] = np.ascontiguousarray((-np.abs(deltas)).reshape(4, 128).T).astype(np.float32)
    s = np.arange(64)
    same = (s[:, None] // 16) == (s[None, :] // 16)
    mf = ((s[:, None] <= s[None, :]) & same).astype(np.float32)
    mb = ((s[:, None] >= s[None, :]) & same).astype(np.float32)
    c["hg_maskD"] = _bf(np.stack([np.concatenate([mf, mf], 0), np.concatenate([mb, mb], 0)], 1))
    ng = np.zeros((2, 3, 64), np.float32)
    for vi in range(3):
        ng[0, vi, s >= 16 * (vi + 1)] = -30000.0
        ng[1, vi, s < 16 * (vi + 1)] = -30000.0
    c["hg_negm"] = np.ascontiguousarray(np.broadcast_to(ng[None], (128, 2, 3, 64))).astype(np.float32)
    c["iota_t"] = np.ascontiguousarray(np.broadcast_to(np.arange(SEQ, dtype=np.float32)[None, :], (128, SEQ)))
    c["pidx"] = (np.arange(16)[None, :] * 128 + np.arange(128)[:, None]).astype(np.float32)
    sm = np.zeros((16, 16, 128), np.float32)
    for e in range(16):
        sm[e, e, :] = 1.0
    c["selmat"] = sm
    return c


_CONSTS = None


def pmaj(v, n):
    v = np.asarray(v, np.float32)
    lead = v.shape[:-1]
    a = v.reshape(lead + (n, 128))
    a = np.moveaxis(a, -1, 0)
    return np.ascontiguousarray(a)


class Builder:
    def __init__(self, nc, debug=()):
        self.nc = nc
        self.P = Prog(nc)
        self.debug = set(debug)
        self.inputs = {}
        self.st = ExitStack()
        self.PS = self.st.enter_context(nc.psum_tensor("PS", [128, 8, 512], F32))
        self.psi = 0
        self.uid = 0

    def din(self, name, shape, dt=F32):
        t = self.nc.dram_tensor(name, list(shape), dt, kind="ExternalInput").ap()
        self.inputs[name] = t
        return t

    def dscr(self, name, shape, dt):
        kind = "ExternalOutput" if name in self.debug else "Internal"
        return self.nc.dram_tensor(name, list(shape), dt, kind=kind).ap()

    def sb(self, st, name, shape, dt):
        self.uid += 1
        return st.enter_context(self.nc.sbuf_tensor(f"{name}_{self.uid}", list(shape), dt))

    def bank(self):
        b = self.psi
        self.psi = (self.psi + 1) % 8
        return b

    def MM(self, out, lhsT, rhs, start, stop, r, w):
        self.P.op("pe", lambda e: e.matmul(out, lhsT, rhs, start=start, stop=stop), r, w)

    def TR(self, out, in_, ident, r, w):
        self.P.op("pe", lambda e: e.transpose(out, in_, ident), r, w)

    def ACT(self, out, in_, func, r, w, bias=None, scale=None):
        kw = {}
        if bias is not None:
            kw["bias"] = bias
        if scale is not None:
            kw["scale"] = scale
        self.P.op("act", lambda e: e.activation(out=out, in_=in_, func=func, **kw), r, w)

    def TS(self, eng, out, in0, s1, s2, op0, op1, r, w):
        if op1 is None:
            self.P.op(eng, lambda e: e.tensor_scalar(out, in0, s1, None, op0), r, w)
        else:
            self.P.op(eng, lambda e: e.tensor_scalar(out, in0, s1, s2, op0, op1), r, w)

    def TT(self, eng, out, in0, in1, op, r, w):
        self.P.op(eng, lambda e: e.tensor_tensor(out, in0, in1, op), r, w)

    def STT(self, out, in0, scalar, in1, op0, op1, r, w):
        self.P.op("dve", lambda e: e.scalar_tensor_tensor(out, in0, scalar, in1, op0, op1), r, w)

    def CP(self, eng, out, in_, r, w):
        if eng == "act":
            self.P.op("act", lambda e: e.activation(out=out, in_=in_, func=AF.Copy), r, w)
        else:
            self.P.op(eng, lambda e: e.tensor_copy(out, in_), r, w)

    def MS(self, eng, ap, val, w):
        self.P.op(eng, lambda e: e.memset(ap, val), (), w)

    def DMA(self, eng, out, in_, r, w):
        self.P.op(eng, lambda e: e.dma_start(out=out, in_=in_), r, w, dma=True)


def tiles(L, w=512):
    w = min(w, L)
    return [(i * w, w) for i in range(L // w)]


def build(stage=99, debug=(), skip=()):
    nc = bass.Bass("TRN2", target_bir_lowering=False)
    B = Builder(nc, debug)
    P = B.P
    P.trace_sites = TRACE_SITES
    consts = host_consts()

    xT_in = B.din("xT", [D, SEQ])
    cxT_in = B.din("ctxT", [D, CTX])
    cT_in = B.din("cT", [128, 16, 2])
    gmix_in = B.din("gmix", [128, DEPTH, 16])
    gffn_in = B.din("gffn", [128, DEPTH, 16])
    gfin_in = B.din("gfin", [128, 16])
    bmod_in = B.din("bmod", [128, DEPTH, 96])
    w_mod = B.din("w_mod", [DEPTH, D, 6 * D])
    w_in = B.din("w_in", [DEPTH, D, INW])
    w_out = B.din("w_out", [DEPTH, D, D])
    w_fnet = B.din("w_fnet", [DEPTH, 512, 512])
    hycw_in = B.din("hycw", [128, DEPTH, 3, 12])
    hycb_in = B.din("hycb", [128, DEPTH, 12])
    hyw1_in = B.din("hy_w1", [DEPTH, 33, 64])
    hyw2_in = B.din("hy_w2", [DEPTH, 64, 64])
    hyw3_in = B.din("hy_w3", [DEPTH, 64, 64])
    hywo_in = B.din("hy_w_out", [DEPTH, 64, 1024])
    hyb_in = B.din("hyb", [64, DEPTH, 3])
    hyfr_in = B.din("hyfr", [64, DEPTH, 3])
    hydb_in = B.din("hydb", [128, DEPTH, 4])
    hglb_in = B.din("hglb", [128, DEPTH, 2, 8])
    hgg_in = B.din("hgg", [128, DEPTH, 8])
    wr_in = B.din("w_router", [DEPTH, D, NE])
    if DBG.get("small"):
        wg_in = B.din("w_gate", [1, 1, 128, FF])
        wu_in = B.din("w_up", [1, 1, 128, FF])
        wd_in = B.din("w_down", [1, 1, 128, D])
    else:
        wg_in = B.din("w_gate", [DEPTH, NE, D, FF])
        wu_in = B.din("w_up", [DEPTH, NE, D, FF])
        wd_in = B.din("w_down", [DEPTH, NE, FF, D])
    cd = {}
    for k, v in consts.items():
        dt = BF16 if v.dtype == ml_dtypes.bfloat16 else F32
        cd[k] = B.din(k, v.shape, dt)

    out_d = nc.dram_tensor("outT", [D, SEQ], F32, kind="ExternalOutput").ap()
    xres = {SEQ: B.dscr("xres", [D, SEQ], F32), CTX: B.dscr("xcres", [D, CTX], F32)}
    cat_d = {SEQ: B.dscr("cat_x", [D, SEQ], BF16), CTX: B.dscr("cat_c", [D, CTX], BF16)}
    hyk_d = {L: B.dscr(f"hyk{L}", [L // 128, 128, 2, 512], F32) for L in (SEQ, CTX)}
    ys_d = {SEQ: B.dscr("ys_x", [16, 128, 2 * NE, 128], BF16), CTX: B.dscr("ys_c", [16, 32, NE, 128], BF16)}

    G = B.st
    ident_bf = B.sb(G, "ident_bf", [128, 128], BF16)
    ident_f = B.sb(G, "ident_f", [128, 128], F32)
    ones_f = B.sb(G, "ones_f", [128, 128], F32)
    modT = B.sb(G, "modT", [128, 96, 2], F32)
    A1 = B.sb(G, "A1", [128, 16, 2], F32)
    A2 = B.sb(G, "A2", [128, 16, 2], F32)
    gmix = B.sb(G, "gmix", [128, DEPTH, 16], F32)
    gffn = B.sb(G, "gffn", [128, DEPTH, 16], F32)
    gfin = B.sb(G, "gfin", [128, 16], F32)
    S32 = B.sb(G, "S32", [128, 16, 128], F32)
    hglb = B.sb(G, "hglb", [128, DEPTH, 2, 8], F32)
    lbs = B.sb(G, "lbs", [128, 2, 8], F32)
    oml = B.sb(G, "oml", [128, 2, 8], F32)
    hgg = B.sb(G, "hgg", [128, DEPTH, 8], F32)
    epsc = B.sb(G, "epsc", [128, 1], F32)
    B.MS("dve", epsc[:], EPS, ["epsc"])
    B.DMA("sp", ident_bf[:], cd["ident_bf"], (), ["ident_bf"])
    B.DMA("sp", ident_f[:], cd["ident_f"], (), ["ident_f"])
    B.DMA("sp", ones_f[:], cd["ones_f"], (), ["ones_f"])
    B.DMA("sp", gmix[:], gmix_in, (), ["gmix"])
    B.DMA("sp", gffn[:], gffn_in, (), ["gffn"])
    B.DMA("sp", gfin[:], gfin_in, (), ["gfin"])
    B.DMA("sp", hglb[:], hglb_in, (), ["hglb"])
    B.DMA("sp", hgg[:], hgg_in, (), ["hgg"])

    PS = B.PS

    def psk(b):
        return f"ps{b}"

    with ExitStack() as st:
        buf = B.sb(st, "cpy", [128, 2, SEQ], F32)
        for L, src in ((SEQ, xT_in), (CTX, cxT_in)):
            for dc in range(16):
                b_ = dc % 2
                B.DMA("sp", buf[:, b_, :L], src[dc * 128:(dc + 1) * 128, :], (), [f"cpy{b_}"])
                B.DMA("sp", xres[L][dc * 128:(dc + 1) * 128, :], buf[:, b_, :L], [f"cpy{b_}"], [f"xres{L}.{dc}"])
        e0 = B.sb(st, "e0", [128, 2, 8], F32)
        e1 = B.sb(st, "e1", [128, 2, 8], F32)
        B.ACT(e0[:], hglb[:, 0], AF.Exp, ["hglb"], ["e0"])
        B.ACT(e1[:], hglb[:, 1], AF.Exp, ["hglb"], ["e1"])
        B.TT("dve", e0[:], e0[:], e1[:], ALU.add, ["e0", "e1"], ["e0"])
        B.P.op("dve", lambda e: e.reciprocal(e0[:], e0[:]), ["e0"], ["e0"])
        B.TT("dve", lbs[:], e1[:], e0[:], ALU.mult, ["e0", "e1"], ["lbs"])
        B.TS("dve", oml[:], lbs[:], -1.0, 1.0, ALU.mult, ALU.add, ["lbs"], ["oml"])
    P.barrier()

    def phase_mod(l):
        with ExitStack() as st:
            cT = B.sb(st, "cT", [128, 16, 2], F32)
            sc = B.sb(st, "sc", [128, 16, 2], BF16)
            bmod = B.sb(st, "bmod", [128, 96], F32)
            wb = B.sb(st, "wmod", [128, 2, 16, 512], BF16)
            B.DMA("sp", cT[:], cT_in, (), ["cT"])
            B.DMA("sp", bmod[:], bmod_in[:, l, :], (), ["bmod"])
            B.ACT(sc[:], cT[:], AF.Silu, ["cT"], ["sc"])
            wv = w_mod[l].rearrange("(dc p) n -> p dc n", p=128)
            bk = B.bank()
            for blk in range(24):
                bb = blk % 2
                B.DMA("pool", wb[:, bb], wv[:, :, blk * 512:(blk + 1) * 512], (), [f"wmod{bb}"])
                for j in range(4):
                    oc = blk * 4 + j
                    for dc in range(16):
                        B.MM(PS[:, bk, oc * 2:oc * 2 + 2], wb[:, bb, dc, j * 128:(j + 1) * 128], sc[:, dc, :],
                             dc == 0, dc == 15, [f"wmod{bb}", "sc"], [psk(bk)])
            B.TT("dve", modT[:], PS[:, bk, 0:192].rearrange("p (a b) -> p a b", b=2),
                 bmod[:].unsqueeze(2).to_broadcast([128, 96, 2]), ALU.add, [psk(bk), "bmod"], ["modT"])
            for (Ax, g, off, nm) in ((A1, gmix, 16, "A1"), (A2, gffn, 64, "A2")):
                B.TS("dve", Ax[:], modT[:, off:off + 16, :], 1.0, None, ALU.add, None, ["modT"], [nm])
                B.TT("dve", Ax[:], Ax[:], g[:, l, :].unsqueeze(2).to_broadcast([128, 16, 2]), ALU.mult,
                     [nm, "gmix", "gffn"], [nm])
        P.barrier()

    def normmod(st, L, s, Ax, Anm, shoff, xn, xnk, router=None, nbuf=2, bufs=None):
        if bufs is not None:
            xld_, sq_, rstd, tmp_ = bufs
        else:
            xld_ = B.sb(st, "xld", [128, nbuf, L], F32)
            sq_ = B.sb(st, "sq", [128, nbuf, L], F32)
            rstd = B.sb(st, "rstd", [128, L], F32)
            tmp_ = B.sb(st, "nmtmp", [128, nbuf, L], F32)

        class _V:
            def __init__(self, t, n):
                self.t, self.n = t, n

            def __getitem__(self, key):
                p, b, f = key
                return self.t[p, b % self.n, f]
        sq = _V(sq_, nbuf)
        xld = _V(xld_, nbuf)
        tmp = _V(tmp_, nbuf)
        tl = tiles(L)
        for dc in range(16):
            b_ = dc % 2
            B.DMA("sp", xld[:, b_, :], xres[L][dc * 128:(dc + 1) * 128, :], [f"xres{L}.{dc}"], [f"xld{b_ % nbuf}"])
            B.ACT(sq[:, b_, :], xld[:, b_, :], AF.Square, [f"xld{b_ % nbuf}"], [f"sq{b_ % nbuf}"])
            for i, (t0, tw) in enumerate(tl):
                B.MM(PS[:, 4 + i, :tw], ones_f[:], sq[:, b_, t0:t0 + tw], dc == 0, dc == 15,
                     [f"sq{b_ % nbuf}", "ones_f"], [psk(4 + i)])
        for i, (t0, tw) in enumerate(tl):
            B.ACT(rstd[:, t0:t0 + tw], PS[:, 4 + i, :tw], AF.Ln, [psk(4 + i)], ["rstd"], bias=epsc[:, 0:1], scale=1.0 / D)
        B.ACT(rstd[:], rstd[:], AF.Exp, ["rstd"], ["rstd"], scale=-0.5)
        for dc in range(16):
            b_ = dc % 2
            B.DMA("sp", xld[:, b_, :], xres[L][dc * 128:(dc + 1) * 128, :], [f"xres{L}.{dc}"], [f"xld{b_ % nbuf}"])
            B.TT("dve", tmp[:, b_, :], xld[:, b_, :], rstd[:], ALU.mult, [f"xld{b_ % nbuf}", "rstd"], [f"nmtmp{b_ % nbuf}"])
            if router is None:
                B.ACT(xn[:, dc, :L], tmp[:, b_, :], AF.Identity, [f"nmtmp{b_ % nbuf}", Anm, "modT"], [f"{xnk}.{dc}"],
                      bias=modT[:, shoff + dc, s:s + 1], scale=Ax[:, dc, s:s + 1])
            else:
                wr32, lb = router
                B.ACT(sq[:, b_, :], tmp[:, b_, :], AF.Identity, [f"nmtmp{b_ % nbuf}", Anm, "modT"], [f"sq{b_ % nbuf}"],
                      bias=modT[:, shoff + dc, s:s + 1], scale=Ax[:, dc, s:s + 1])
                B.CP("pool", xn[:, dc, :L], sq[:, b_, :], [f"sq{b_ % nbuf}"], [f"{xnk}.{dc}"])
                for tt in range(L // 128):
                    B.MM(PS[:, lb, tt * 16:(tt + 1) * 16], sq[:, b_, tt * 128:(tt + 1) * 128], wr32[:, dc, :],
                         dc == 0 and tt == 0, dc == 15 and tt == L // 128 - 1, [f"sq{b_ % nbuf}", "wr32"], [psk(lb)])

    def load_w(wb, wbk, wv, c0, ncols):
        B.DMA("pool", wb[:, :, :ncols], wv[:, :, c0:c0 + ncols], (), [wbk])

    def proj_fm(L, xn, xnk, wb, wbk, ncols, evac, kch=16):
        for m in range(ncols // 128):
            for (t0, tw) in tiles(L):
                bk = B.bank()
                for dc in range(kch):
                    B.MM(PS[:, bk, :tw], wb[:, dc, m * 128:(m + 1) * 128], xn[:, dc, t0:t0 + tw], dc == 0, dc == kch - 1,
                         [wbk, f"{xnk}.{dc}"], [psk(bk)])
                evac(m, t0, tw, PS[:, bk, :tw], psk(bk))

    def phase_fourier(l, L, xn, xnk, wv):
        TT_ = L // 128
        FW = min(256, L)
        with ExitStack() as st:
            wb = B.sb(st, "wfn", [128, 16, 512], BF16)
            hT = B.sb(st, "hTfn", [128, 4, L], BF16)
            AB = B.sb(st, "AB", [128, TT_, 4, 256], BF16)
            cs = B.sb(st, "fc_cs", [128, 256], BF16)
            tab = B.sb(st, "fntab", [128, 2, 2, TT_, FW], BF16)
            yT = B.sb(st, "yTfn", [128, 4, L], BF16)
            wf = B.sb(st, "wfnet", [128, 4, 512], BF16)
            og = B.sb(st, "catfn", [128, 4, L], BF16)
            B.DMA("sp", cs[:], cd["fc_cs"], (), ["fc_cs"])
            B.DMA("pool", wf[:], w_fnet[l].rearrange("(kc p) n -> p kc n", p=128), (), ["wfnet"])
            load_w(wb, "wfn", wv, 0, 512)

            def ev(m, t0, tw, ps, pk):
                B.CP("act", hT[:, m, t0:t0 + tw], ps, [pk], [f"hTfn.{m}"])
            proj_fm(L, xn, xnk, wb, "wfn", 512, ev)
            for tt in range(TT_):
                bk = B.bank()
                for g in range(4):
                    B.MM(PS[:, bk, g * 128:(g + 1) * 128], hT[:, g, tt * 128:(tt + 1) * 128], cs[:, 0:128], True, True,
                         [f"hTfn.{g}", "fc_cs"], [psk(bk)])
                bk2 = B.bank()
                for g in range(4):
                    B.MM(PS[:, bk2, g * 128:(g + 1) * 128], hT[:, g, tt * 128:(tt + 1) * 128], cs[:, 128:256], True, True,
                         [f"hTfn.{g}", "fc_cs"], [psk(bk2)])
                B.CP("act", AB[:, tt, :, 0:128], PS[:, bk, :].rearrange("p (g c) -> p g c", c=128), [psk(bk)], [f"AB.{tt}"])
                B.CP("dve", AB[:, tt, :, 128:256], PS[:, bk2, :].rearrange("p (g c) -> p g c", c=128), [psk(bk2)], [f"AB.{tt}"])
            for i in range(L // FW):
                tb = i % 2
                B.DMA("sp", tab[:, tb], cd[f"fn_tab{L}"][i], (), [f"fntab{tb}"])
                for g in range(4):
                    bk = B.bank()
                    n = 0
                    for tt in range(TT_):
                        for a in range(2):
                            B.MM(PS[:, bk, :FW], AB[:, tt, g, a * 128:(a + 1) * 128], tab[:, tb, a, tt, :], n == 0,
                                 n == 2 * TT_ - 1, [f"AB.{tt}", f"fntab{tb}"], [psk(bk)])
                            n += 1
                    B.CP("act", yT[:, g, i * FW:(i + 1) * FW], PS[:, bk, :FW], [psk(bk)], [f"yTfn.{g}"])

            def ev2(m, t0, tw, ps, pk):
                B.CP("dve", og[:, m, t0:t0 + tw], ps, [pk], [f"catfn.{m}"])
            proj_fm(L, yT, "yTfn", wf, "wfnet", 512, ev2, kch=4)
            for m in range(4):
                B.DMA("sp", cat_d[L][m * 128:(m + 1) * 128, :], og[:, m, :], [f"catfn.{m}"], [f"cat{L}.{m}"])
        P.barrier()

    def wrap_sin(a, m, out, ps, pk, fr, fb, np_, w, outk, sfx=""):
        ak, mk = "ws_a" + sfx, "ws_m" + sfx
        B.TS("dve", a[:np_, :w], ps, fr, fb, ALU.mult, ALU.add, [pk, "hyfrb"], [ak])
        B.TS("dve", m[:np_, :w], a[:np_, :w], PI, -2 * PI, ALU.is_gt, ALU.mult, [ak], [mk])
        B.TT("dve", a[:np_, :w], a[:np_, :w], m[:np_, :w], ALU.add, [ak, mk], [ak])
        B.TS("dve", m[:np_, :w], a[:np_, :w], -PI, 2 * PI, ALU.is_lt, ALU.mult, [ak], [mk])
        B.TT("dve", a[:np_, :w], a[:np_, :w], m[:np_, :w], ALU.add, [ak, mk], [ak])
        B.ACT(out, a[:np_, :w], AF.Sin, [ak], [outk])

    def fwd_dft(st, L, src, srck, consume):
        TT_ = L // 128
        tab = B.sb(st, "hyfwd", [128, 2, 2, TT_, 128], BF16)
        for ft in range(L // 128):
            tb = ft % 2
            B.DMA("sp", tab[:, tb], cd[f"hy_fwd{L}"][ft], (), [f"hyfwd{tb}"])
            bre, bim = B.bank(), B.bank()
            for a, bk in ((0, bre), (1, bim)):
                for tt in range(TT_):
                    B.MM(PS[:, bk, :], tab[:, tb, a, tt, :], src[:, tt, :], tt == 0, tt == TT_ - 1,
                         [f"hyfwd{tb}", srck], [psk(bk)])
            consume(ft, bre, bim)

    def to_tm(L, srcT, srck, dst, dstk):
        for tt in range(L // 128):
            bk = B.bank()
            pv = PS[:, bk, :].bitcast(BF16)
            for j in range(4):
                B.TR(pv[:, j * 128:(j + 1) * 128], srcT[:, j, tt * 128:(tt + 1) * 128], ident_bf[:],
                     [srck, "ident_bf"], [psk(bk)])
            B.CP("act" if tt % 2 else "dve", dst[:, tt, :], pv[:, 0:512], [psk(bk)], [dstk])

    def to_tm1(L, src, srck, dst, dstk, j):
        TT_ = L // 128
        for g0 in range(0, TT_, 4):
            n = min(4, TT_ - g0)
            bk = B.bank()
            pv = PS[:, bk, :].bitcast(BF16)
            for q in range(n):
                B.TR(pv[:, q * 128:(q + 1) * 128], src[:, (g0 + q) * 128:(g0 + q + 1) * 128], ident_bf[:], [srck, "ident_bf"], [psk(bk)])
            B.CP("act" if (g0 // 4) % 2 else "dve", dst[:, g0:g0 + n, j * 128:(j + 1) * 128],
                 pv[:, 0:n * 128].rearrange("p (q c) -> p q c", c=128), [psk(bk)], [dstk])

    def phase_hyfilter(l, L):
        TT_ = L // 128
        with ExitStack() as st:
            h3 = B.sb(st, "hyh1", [64, L], F32)
            with ExitStack() as st1:
                zT = B.sb(st1, "hyz", [33, L], F32)
                w1 = B.sb(st1, "hyw1", [33, 64], F32)
                w2 = B.sb(st1, "hyw2", [64, 64], F32)
                w3 = B.sb(st1, "hyw3", [64, 64], F32)
                fr = B.sb(st1, "hyfr", [64, 3], F32)
                fb = B.sb(st1, "hyfb", [64, 3], F32)
                h2 = B.sb(st1, "hyh2", [64, L], F32)
                wsa2 = B.sb(st1, "ws_a", [64, 2, 512], F32)
                wsm2 = B.sb(st1, "ws_m", [64, 2, 512], F32)
                h1 = h3
                B.DMA("sp", zT[:], cd[f"hy_z{L}"], (), ["hyz"])
                B.DMA("sp", w1[:], hyw1_in[l], (), ["hyw"])
                B.DMA("sp", w2[:], hyw2_in[l], (), ["hyw"])
                B.DMA("sp", w3[:], hyw3_in[l], (), ["hyw"])
                B.DMA("sp", fr[:], hyfr_in[:, l, :], (), ["hyfrb"])
                B.DMA("sp", fb[:], hyb_in[:, l, :], (), ["hyfrb"])
                B.TT("dve", fb[:], fb[:], fr[:], ALU.mult, ["hyfrb"], ["hyfrb"])
                srcs = [(zT, 33, w1, "hyz"), (h1, 64, w2, "hyh1"), (h2, 64, w3, "hyh2")]
                dsts = [(h1, "hyh1"), (h2, "hyh2"), (h1, "hyh1")]
                for li in range(3):
                    src, kp, wt, sk = srcs[li]
                    dst, dk = dsts[li]
                    for ti_, (t0, tw) in enumerate(tiles(L)):
                        bk = B.bank()
                        B.MM(PS[:64, bk, :tw], wt[:kp, :], src[:kp, t0:t0 + tw], True, True, ["hyw", sk], [psk(bk)])
                        wrap_sin(wsa2[:, ti_ % 2, :], wsm2[:, ti_ % 2, :], dst[:, t0:t0 + tw], PS[:64, bk, :tw], psk(bk), fr[:, li:li + 1], fb[:, li:li + 1], 64, tw, dk,
                                 sfx=str(ti_ % 2))
            P.barrier()
            wo = B.sb(st, "hywo", [64, 1024], F32)
            tpos = B.sb(st, "tpos", [128, L], F32)
            nd = B.sb(st, "ndelta", [128, 4], F32)
            dec = B.sb(st, "decay", [128, L], F32)
            hf = B.sb(st, "hf", [128, L], F32)
            hb = B.sb(st, "hb", [128, L], F32)
            hs = B.sb(st, "hs", [128, L], BF16)
            hd = B.sb(st, "hd", [128, L], BF16)
            hs_tm = B.sb(st, "hs_tm", [128, TT_, 512], BF16)
            hd_tm = B.sb(st, "hd_tm", [128, TT_, 512], BF16)
            kst = B.sb(st, "kst", [128, 2, 2, 512], F32)
            B.DMA("sp", wo[:], hywo_in[l], (), ["hywo"])
            B.DMA("sp", tpos[:], cd[f"hy_tpos{L}"], (), ["tpos"])
            B.DMA("sp", nd[:], cd["hy_ndelta"], (), ["ndelta"])
            for j in range(4):
                B.ACT(dec[:], tpos[:], AF.Exp, ["tpos", "ndelta"], ["decay"], scale=nd[:, j:j + 1])
                for (t0, tw) in tiles(L):
                    b1, b2 = B.bank(), B.bank()
                    B.MM(PS[:, b1, :tw], wo[:, j * 128:(j + 1) * 128], h3[:, t0:t0 + tw], True, True, ["hywo", "hyh1"], [psk(b1)])
                    B.MM(PS[:, b2, :tw], wo[:, 512 + j * 128:512 + (j + 1) * 128], h3[:, t0:t0 + tw], True, True, ["hywo", "hyh1"], [psk(b2)])
                    B.TT("dve", hf[:, t0:t0 + tw], PS[:, b1, :tw], dec[:, t0:t0 + tw], ALU.mult, [psk(b1), "decay"], ["hf"])
                    B.TT("dve", hb[:, t0:t0 + tw], PS[:, b2, :tw], dec[:, t0:t0 + tw], ALU.mult, [psk(b2), "decay"], ["hb"])
                B.MS("dve", hb[:, 0:1], 0.0, ["hb"])
                B.TT("dve", hs[:], hf[:], hb[:], ALU.add, ["hf", "hb"], ["hs"])
                B.TT("dve", hd[:], hf[:], hb[:], ALU.subtract, ["hf", "hb"], ["hd"])
                to_tm1(L, hs, "hs", hs_tm, "hs_tm", j)
                to_tm1(L, hd, "hd", hd_tm, "hd_tm", j)
            tab = B.sb(st, "hyfwdK", [128, 2, 2, TT_, 128], BF16)
            for ft in range(L // 128):
                tb = ft % 2
                B.DMA("sp", tab[:, tb], cd[f"hy_fwd{L}"][ft], (), [f"hyfwdK{tb}"])
                bre, bim = B.bank(), B.bank()
                for tt in range(TT_):
                    B.MM(PS[:, bre, :], tab[:, tb, 0, tt, :], hs_tm[:, tt, :], tt == 0, tt == TT_ - 1, [f"hyfwdK{tb}", "hs_tm"], [psk(bre)])
                for tt in range(TT_):
                    B.MM(PS[:, bim, :], tab[:, tb, 1, tt, :], hd_tm[:, tt, :], tt == 0, tt == TT_ - 1, [f"hyfwdK{tb}", "hd_tm"], [psk(bim)])
                B.CP("act", kst[:, tb, 0, :], PS[:, bre, :], [psk(bre)], [f"kst{tb}"])
                B.CP("dve", kst[:, tb, 1, :], PS[:, bim, :], [psk(bim)], [f"kst{tb}"])
                B.DMA("act", hyk_d[L][ft], kst[:, tb], [f"kst{tb}"], [f"hyk{L}.{ft}"])
        P.barrier()

    def phase_hyena(l, L, xn, xnk, wv):
        TT_ = L // 128
        IW = min(256, L)
        with ExitStack() as st:
            cw = B.sb(st, "hycw", [128, 3, 12], F32)
            cb = B.sb(st, "hycb", [128, 12], F32)
            db = B.sb(st, "hydb", [128, 4], F32)
            zT = B.sb(st, "zT", [128, 4, L], BF16)
            x0T = B.sb(st, "x0T", [128, 4, L], BF16)
            B.DMA("sp", cw[:], hycw_in[:, l], (), ["hycw"])
            B.DMA("sp", cb[:], hycb_in[:, l], (), ["hycw"])
            B.DMA("sp", db[:], hydb_in[:, l], (), ["hycw"])
            with ExitStack() as st1:
                wb = B.sb(st1, "why", [128, 2, 16, 384], BF16)
                hp = B.sb(st1, "hpad", [128, 3, L + 2], BF16)
                acc = B.sb(st1, "hyacc", [128, 2, L], F32)
                B.MS("pool", hp[:, :, 0:1], 0.0, ["hpad0", "hpad1", "hpad2"])
                B.MS("pool", hp[:, :, L + 1:L + 2], 0.0, ["hpad0", "hpad1", "hpad2"])
                for j in range(4):
                    wbb = j % 2
                    for q in range(3):
                        c0 = HY_OFF + q * 512 + j * 128
                        B.DMA("pool", wb[:, wbb, :, q * 128:(q + 1) * 128], wv[:, :, c0:c0 + 128], (), [f"why{wbb}"])

                    def ev(m, t0, tw, ps, pk):
                        B.CP("act", hp[:, m, 1 + t0:1 + t0 + tw], ps, [pk], [f"hpad{m}"])
                    proj_fm(L, xn, xnk, wb[:, wbb], f"why{wbb}", 384, ev)
                    for q in range(3):
                        ch = q * 4 + j
                        a_ = acc[:, q % 2, :]
                        ak = f"hyacc{q % 2}"
                        B.ACT(a_, hp[:, q, 1:L + 1], AF.Identity, [f"hpad{q}", "hycw"], [ak], bias=cb[:, ch:ch + 1], scale=cw[:, 1, ch:ch + 1])
                        B.STT(a_, hp[:, q, 0:L], cw[:, 0, ch:ch + 1], a_, ALU.mult, ALU.add, [f"hpad{q}", "hycw", ak], [ak])
                        if q < 2:
                            B.STT(a_, hp[:, q, 2:L + 2], cw[:, 2, ch:ch + 1], a_, ALU.mult, ALU.add, [f"hpad{q}", "hycw", ak], [ak])
                            if q == 1:
                                B.TT("dve", zT[:, j, :], acc[:, 0, :], acc[:, 1, :], ALU.mult, ["hyacc0", "hyacc1"], ["zT"])
                        else:
                            B.STT(x0T[:, j, :], hp[:, q, 2:L + 2], cw[:, 2, ch:ch + 1], a_, ALU.mult, ALU.add, [f"hpad{q}", "hycw", ak], ["x0T"])
            P.barrier()
            Pre = B.sb(st, "Pre", [128, TT_, 512], BF16)
            Pim = B.sb(st, "Pim", [128, TT_, 512], BF16)
            with ExitStack() as st2:
                z_tm = B.sb(st2, "z_tm", [128, TT_, 512], BF16)
                kk = B.sb(st2, "kk", [128, 2, 2, 512], F32)
                t1 = B.sb(st2, "hyt1", [128, 512], F32)
                t2 = B.sb(st2, "hyt2", [128, 512], F32)
                to_tm(L, zT, "zT", z_tm, "z_tm")

                def consume(ft, bre, bim):
                    kb_ = ft % 2
                    B.DMA("sp", kk[:, kb_], hyk_d[L][ft], [f"hyk{L}.{ft}"], [f"kk{kb_}"])
                    B.TT("dve", t1[:], PS[:, bre, :], kk[:, kb_, 0, :], ALU.mult, [psk(bre), f"kk{kb_}"], ["hyt1"])
                    B.TT("dve", t2[:], PS[:, bim, :], kk[:, kb_, 1, :], ALU.mult, [psk(bim), f"kk{kb_}"], ["hyt2"])
                    B.TT("pool", Pre[:, ft, :], t1[:], t2[:], ALU.subtract, ["hyt1", "hyt2"], ["Pre"])
                    B.TT("dve", t1[:], PS[:, bre, :], kk[:, kb_, 1, :], ALU.mult, [psk(bre), f"kk{kb_}"], ["hyt1"])
                    B.TT("dve", t2[:], PS[:, bim, :], kk[:, kb_, 0, :], ALU.mult, [psk(bim), f"kk{kb_}"], ["hyt2"])
                    B.TT("pool", Pim[:, ft, :], t1[:], t2[:], ALU.add, ["hyt1", "hyt2"], ["Pim"])
                fwd_dft(st2, L, z_tm, "z_tm", consume)
            P.barrier()
            with ExitStack() as st3:
                itab = B.sb(st3, "hyinv", [128, 2, 2, TT_, IW], BF16)
                yo = B.sb(st3, "hyyo", [128, 2, IW], F32)
                og = B.sb(st3, "cathy", [128, 4, L], BF16)
                for it in range(L // IW):
                    tb = it % 2
                    B.DMA("sp", itab[:, tb], cd[f"hy_inv{L}"][it], (), [f"hyinv{tb}"])
                    for j in range(4):
                        bk = B.bank()
                        n = 0
                        for ft in range(TT_):
                            for a, Pm, Pk in ((0, Pre, "Pre"), (1, Pim, "Pim")):
                                B.MM(PS[:, bk, :IW], Pm[:, ft, j * 128:(j + 1) * 128], itab[:, tb, a, ft, :], n == 0, n == 2 * TT_ - 1,
                                     [Pk, f"hyinv{tb}"], [psk(bk)])
                                n += 1
                        yb = (it * 4 + j) % 2
                        B.STT(yo[:, yb, :], zT[:, j, it * IW:(it + 1) * IW], db[:, j:j + 1], PS[:, bk, :IW], ALU.mult, ALU.add,
                              ["zT", "hycw", psk(bk)], [f"hyyo{yb}"])
                        B.TT("pool", og[:, j, it * IW:(it + 1) * IW], yo[:, yb, :], x0T[:, j, it * IW:(it + 1) * IW], ALU.mult,
                             [f"hyyo{yb}", "x0T"], [f"cathy.{j}"])
                for j in range(4):
                    B.DMA("sp", cat_d[L][512 + j * 128:512 + (j + 1) * 128, :], og[:, j, :], [f"cathy.{j}"], [f"cat{L}.{4 + j}"])
        P.barrier()

    def phase_hgrn(l, L, xn, xnk, wv, need_out, first):
        TT_ = L // 128
        NCH = L // 64
        with ExitStack() as st:
            wj = B.sb(st, "whg", [128, 2, 16, 128], BF16)
            rst = B.sb(st, "rst", [128, L], BF16)
            mskD = B.sb(st, "hgmaskD", [128, 2, 64], BF16)
            negm = B.sb(st, "hgnegm", [128, 2, 3, 64], F32)
            T0 = B.sb(st, "T0", [128, L], F32)
            T1 = B.sb(st, "T1", [128, L], F32)
            T2 = B.sb(st, "T2", [128, L], F32)
            Ea = B.sb(st, "Ea", [128, L], BF16)
            Eb = B.sb(st, "Eb", [128, L], BF16)
            qT = B.sb(st, "qT", [128, L], BF16)
            kT = B.sb(st, "kT", [128, L], BF16)
            qb = B.sb(st, "qb", [128, L], BF16)
            kb = B.sb(st, "kb", [128, L], BF16)
            qB = B.sb(st, "qB", [128, L], BF16)
            kdT = B.sb(st, "kdT", [128, L], BF16)
            kbi = B.sb(st, "kbi", [128, 3, L], BF16)
            qbi = B.sb(st, "qbi", [128, NCH, 3, 16], BF16)
            kd_tm = B.sb(st, "kd_tm", [128, TT_, 2, 128], BF16)
            v_tm = B.sb(st, "v_tm", [128, TT_, 128], BF16)
            oT = B.sb(st, "oT", [128, L], F32)
            Sall = B.sb(st, "Sall", [128, NCH, 128], BF16)
            HC = min(16, NCH)
            Sch = B.sb(st, "Sch", [128, HC + 1, 128], F32)
            eb = B.sb(st, "eb", [128, NCH], F32)
            Am = B.sb(st, "Am", [128, 2, 4, 64], BF16)
            B.DMA("sp", rst[:], cd[f"hg_rst{L}"], (), ["rst"])
            B.DMA("sp", mskD[:], cd["hg_maskD"], (), ["hgmask"])
            B.DMA("sp", negm[:], cd["hg_negm"], (), ["hgmask"])
            if first:
                B.MS("dve", S32[:], 0.0, ["S32"])
            B.MS("pool", kd_tm[:], 0.0, ["kd_tm"])
            B.MS("pool", Am[:], 0.0, ["Am0", "Am1"])
            b64 = lambda t: t[:].rearrange("p (n c) -> p n c", c=64)
            b16 = lambda t: t[:].rearrange("p (n c) -> p n c", c=16)
            b416 = lambda t: t[:].rearrange("p (n i c) -> p n i c", i=4, c=16)
            wcnt = [0]

            def loadj(h, j):
                wb_ = wcnt[0] % 2
                wcnt[0] += 1
                c0 = HG_OFF + j * HGW + h * 128
                B.DMA("pool", wj[:, wb_], wv[:, :, c0:c0 + 128], (), [f"whg{wb_}"])
                return wj[:, wb_], f"whg{wb_}"

            def projf(h_, dr_):
                wb_, wbk_ = loadj(h_, 1 + dr_)

                def evf(m, t0, tw, ps, pk):
                    B.ACT(T2[:, t0:t0 + tw], ps, AF.Sigmoid, [pk], ["T2"])
                proj_fm(L, xn, xnk, wb_, wbk_, 128, evf)
            nheads = DBG.get('heads', NH)
            projf(0, 0)
            for h in range(nheads):
                wb, wbk = loadj(h, 3)
                for tt in range(TT_):
                    bk = B.bank()
                    for dc in range(16):
                        B.MM(PS[:, bk, :128], xn[:, dc, tt * 128:(tt + 1) * 128], wb[:, dc, :], dc == 0, dc == 15,
                             [f"{xnk}.{dc}", wbk], [psk(bk)])
                    B.CP("act", v_tm[:, tt, :], PS[:, bk, :128], [psk(bk)], ["v_tm"])
                if need_out:
                    wb, wbk = loadj(h, 0)

                    def evq(m, t0, tw, ps, pk):
                        B.ACT(qT[:, t0:t0 + tw], ps, AF.Silu, [pk], ["qT"])
                    proj_fm(L, xn, xnk, wb, wbk, 128, evq)
                for dr in range(2):
                    sidx = h * 2 + dr
                    if l > 0:
                        B.TS("dve", T0[:], T2[:], oml[:, dr, h:h + 1], lbs[:, dr, h:h + 1], ALU.mult, ALU.add, ["T2", "oml", "lbs"], ["T0"])
                        B.TS("dve", T0[:], T0[:], 1e-6, None, ALU.max, None, ["T0"], ["T0"])
                    else:
                        B.TS("dve", T0[:], T2[:], 1e-6, None, ALU.max, None, ["T2"], ["T0"])
                    B.TS("pool", kT[:], T0[:], -1.0, 1.0, ALU.mult, ALU.add, ["T0"], ["kT"])
                    B.ACT(T0[:], T0[:], AF.Ln, ["T0"], ["T0"])
                    P.op("dve", lambda e: e.tensor_tensor_scan(T1[:], rst[:], T0[:], 0.0, ALU.mult, ALU.add),
                         ["rst", "T0"], ["T1"])
                    if dr == 1:
                        B.TT("dve", b64(T2), b64(T1)[:, :, 63:64].to_broadcast([128, NCH, 64]), b64(T1), ALU.subtract, ["T1"], ["T2"])
                        B.TT("dve", T1[:], T2[:], T0[:], ALU.add, ["T2", "T0"], ["T1"])
                    bend = b64(T1)[:, :, 63:64] if dr == 0 else b64(T1)[:, :, 0:1]
                    B.ACT(eb[:].unsqueeze(2), bend, AF.Exp, ["T1"], ["eb"])
                    B.TT("dve", b64(T2), bend.to_broadcast([128, NCH, 64]), b64(T1), ALU.subtract, ["T1"], ["T2"])
                    B.ACT(Ea[:], T2[:], AF.Exp, ["T2"], ["Ea"])
                    B.TT("dve", kdT[:], kT[:], Ea[:], ALU.mult, ["kT", "Ea"], ["kdT"])
                    if need_out:
                        B.TT("dve", b16(T0), b16(T1), b16(T1)[:, :, 8:9].to_broadcast([128, L // 16, 16]), ALU.subtract, ["T1"], ["T0"])
                        B.ACT(Eb[:], T0[:], AF.Exp, ["T0"], ["Eb"])
                        B.TT("dve", qb[:], qT[:], Eb[:], ALU.mult, ["qT", "Eb"], ["qb"])
                        B.ACT(Ea[:], T0[:], AF.Exp, ["T0"], ["Ea"], scale=-1.0)
                        B.TT("dve", kb[:], kT[:], Ea[:], ALU.mult, ["kT", "Ea"], ["kb"])
                        B.ACT(Eb[:], T1[:], AF.Exp, ["T1"], ["Eb"])
                        B.TT("dve", qB[:], qT[:], Eb[:], ALU.mult, ["qT", "Eb"], ["qB"])
                        if dr == 0:
                            rsel = b64(T1)[:, :, 15:48:16]
                            i0 = 1
                        else:
                            rsel = b64(T1)[:, :, 16:64:16]
                            i0 = 0
                        tq = T0[:, 0:NCH * 48].rearrange("p (n i c) -> p n i c", i=3, c=16)
                        B.TT("dve", tq, b416(T1)[:, :, i0:i0 + 3, :], rsel.unsqueeze(3).to_broadcast([128, NCH, 3, 16]), ALU.subtract, ["T1"], ["T0"])
                        B.ACT(T0[:, 0:NCH * 48], T0[:, 0:NCH * 48], AF.Exp, ["T0"], ["T0"])
                        B.TT("dve", qbi[:], b416(qT)[:, :, i0:i0 + 3, :], tq, ALU.mult, ["qT", "T0"], ["qbi"])
                        for vi in range(3):
                            Tx, Txk = (T2, "T2") if vi % 2 == 0 else (T0, "T0")
                            B.TT("pool", b64(Tx), rsel[:, :, vi:vi + 1].to_broadcast([128, NCH, 64]), b64(T1), ALU.subtract, ["T1"], [Txk])
                            B.TT("dve", b64(Tx), b64(Tx), negm[:, dr, vi:vi + 1, :].to_broadcast([128, NCH, 64]), ALU.min, [Txk, "hgmask"], [Txk])
                            Ex, Exk = (Ea, "Ea") if vi % 2 == 0 else (Eb, "Eb")
                            B.ACT(Ex[:], Tx[:], AF.Exp, [Txk], [Exk])
                            B.TT("dve", kbi[:, vi, :], kT[:], Ex[:], ALU.mult, ["kT", Exk], ["kbi"])
                    nxt = (h, 1) if dr == 0 else ((h + 1, 0) if h + 1 < nheads else None)
                    if nxt is not None:
                        projf(*nxt)
                    for tt in range(TT_):
                        bk = B.bank()
                        pv = PS[:, bk, :].bitcast(BF16)
                        B.TR(pv[:, 0:128], kdT[:, tt * 128:(tt + 1) * 128], ident_bf[:], ["kdT", "ident_bf"], [psk(bk)])
                        B.CP("act", kd_tm[0:64, tt, 0, :], pv[0:64, 0:128], [psk(bk)], ["kd_tm"])
                        B.CP("act", kd_tm[64:128, tt, 1, :], pv[64:128, 0:128], [psk(bk)], ["kd_tm"])
                    order = list(range(NCH)) if dr == 0 else list(range(NCH - 1, -1, -1))
                    pos_of = {n: p for p, n in enumerate(order)}
                    for h0 in range(0, NCH, HC):
                        B.CP("act", Sch[:, 0, :], S32[:, sidx, :], ["S32"], ["Sch"])
                        for gi in range(h0, h0 + HC, 4):
                            grp = order[gi:gi + 4]
                            bk = B.bank()
                            for q, n in enumerate(grp):
                                B.MM(PS[:, bk, q * 128:(q + 1) * 128], kd_tm[:, n // 2, n % 2, :], v_tm[:, n // 2, :], True, True,
                                     ["kd_tm", "v_tm"], [psk(bk)])
                            for q, n in enumerate(grp):
                                p = gi + q - h0
                                B.STT(Sch[:, p + 1, :], Sch[:, p, :], eb[:, n:n + 1], PS[:, bk, q * 128:(q + 1) * 128], ALU.mult, ALU.add,
                                      ["Sch", "eb", psk(bk)], ["Sch"])
                        if need_out:
                            B.CP("act", Sall[:, h0:h0 + HC, :], Sch[:, 0:HC, :], ["Sch"], ["Sall"])
                        B.CP("pool", S32[:, sidx, :], Sch[:, HC, :], ["Sch"], ["S32"])
                    if need_out:
                        c0 = 16 if dr == 0 else 0
                        for gi in range(0, NCH, 4):
                            grp = list(range(gi, gi + 4))
                            ab = (gi // 4) % 2
                            bkD, bkF = B.bank(), B.bank()
                            for q, n in enumerate(grp):
                                pb = (n % 2) * 64
                                ne = n - (n % 2)
                                B.MM(PS[:, bkD, q * 64:(q + 1) * 64], kb[:, ne * 64:(ne + 2) * 64], qb[:, n * 64:(n + 1) * 64], True, True,
                                     ["kb", "qb"], [psk(bkD)])
                                for vi in range(3):
                                    i = vi + i0
                                    B.MM(PS[:, bkF, q * 64 + i * 16:q * 64 + (i + 1) * 16], kbi[:, vi, ne * 64:(ne + 2) * 64], qbi[:, n, vi, :], True, True,
                                         ["kbi", "qbi"], [psk(bkF)])
                            for half in range(2):
                                pb = half * 64
                                pd = PS[pb:pb + 64, bkD, 0:256].rearrange("p (q c) -> p q c", c=64)[:, half::2, :]
                                pf = PS[pb:pb + 64, bkF, 0:256].rearrange("p (q c) -> p q c", c=64)[:, half::2, c0:c0 + 48]
                                am = Am[pb:pb + 64, ab, half::2, :]
                                B.TT("dve", am, pd, mskD[pb:pb + 64, dr:dr + 1, :].to_broadcast([64, 2, 64]), ALU.mult,
                                     [psk(bkD), "hgmask"], [f"Am{ab}"])
                                B.TT("dve", am[:, :, c0:c0 + 48], am[:, :, c0:c0 + 48], pf, ALU.add, [psk(bkF), f"Am{ab}"], [f"Am{ab}"])
                            bkO = B.bank()
                            for q, n in enumerate(grp):
                                pb = (n % 2) * 64
                                B.MM(PS[:, bkO, q * 64:(q + 1) * 64], Sall[:, pos_of[n], :], qB[:, n * 64:(n + 1) * 64], True, False,
                                     ["Sall", "qB"], [psk(bkO)])
                                B.MM(PS[:, bkO, q * 64:(q + 1) * 64], v_tm[:, n // 2, :], Am[:, ab, q, :], False, True,
                                     ["v_tm", f"Am{ab}"], [psk(bkO)])
                            if dr == 0:
                                B.CP("act", oT[:, gi * 64:(gi + 4) * 64], PS[:, bkO, 0:256], [psk(bkO)], ["oT"])
                            else:
                                B.TT("dve", oT[:, gi * 64:(gi + 4) * 64], oT[:, gi * 64:(gi + 4) * 64], PS[:, bkO, 0:256], ALU.add,
                                     [psk(bkO), "oT"], ["oT"])
                if need_out:
                    wb, wbk = loadj(h, 4)

                    def evg(m, t0, tw, ps, pk):
                        B.ACT(qb[:, t0:t0 + tw], ps, AF.Silu, [pk], ["qb"])
                    proj_fm(L, xn, xnk, wb, wbk, 128, evg)
                    B.ACT(T0[:], oT[:], AF.Square, ["oT"], ["T0"])
                    for (t0, tw) in tiles(L):
                        bk = B.bank()
                        B.MM(PS[:, bk, :tw], ones_f[:], T0[:, t0:t0 + tw], True, True, ["T0", "ones_f"], [psk(bk)])
                        B.ACT(T1[:, t0:t0 + tw], PS[:, bk, :tw], AF.Ln, [psk(bk)], ["T1"], bias=epsc[:, 0:1], scale=1.0 / 128)
                    B.ACT(T1[:], T1[:], AF.Exp, ["T1"], ["T1"], scale=-0.5)
                    B.TT("dve", T1[:], T1[:], oT[:], ALU.mult, ["T1", "oT"], ["T1"])
                    B.STT(kb[:], T1[:], hgg[:, l, h:h + 1], qb[:], ALU.mult, ALU.mult, ["T1", "hgg", "qb"], ["kb"])
                    B.DMA("sp", cat_d[L][1024 + h * 128:1024 + (h + 1) * 128, :], kb[:], ["kb"], [f"cat{L}.{8 + h}"])
        P.barrier()

    def phase_outproj(l, L, s):
        with ExitStack() as st:
            xn = B.sb(st, "catT", [128, 16, L], BF16)
            wb = B.sb(st, "wo", [128, 2, 16, 512], BF16)
            xl = B.sb(st, "xl", [128, 2, L], F32)
            wv = w_out[l].rearrange("(dc p) n -> p dc n", p=128)
            for dc in range(16):
                B.DMA("sp", xn[:, dc, :L], cat_d[L][dc * 128:(dc + 1) * 128, :], [f"cat{L}.{dc}"], [f"catT.{dc}"])
            for blk in range(4):
                wbb = blk % 2
                B.DMA("pool", wb[:, wbb], wv[:, :, blk * 512:(blk + 1) * 512], (), [f"wo{wbb}"])
                for m4 in range(4):
                    m = blk * 4 + m4
                    xb = m % 2
                    B.DMA("sp", xl[:, xb, :], xres[L][m * 128:(m + 1) * 128, :], [f"xres{L}.{m}"], [f"xl{xb}"])
                    for (t0, tw) in tiles(L):
                        bk = B.bank()
                        for dc in range(16):
                            B.MM(PS[:, bk, :tw], wb[:, wbb, dc, m4 * 128:(m4 + 1) * 128], xn[:, dc, t0:t0 + tw], dc == 0, dc == 15,
                                 [f"wo{wbb}", f"catT.{dc}"], [psk(bk)])
                        B.STT(xl[:, xb, t0:t0 + tw], PS[:, bk, :tw], modT[:, 32 + m, s:s + 1], xl[:, xb, t0:t0 + tw], ALU.mult, ALU.add,
                              [psk(bk), "modT", f"xl{xb}"], [f"xl{xb}"])
                    B.DMA("act", xres[L][m * 128:(m + 1) * 128, :], xl[:, xb, :], [f"xl{xb}"], [f"xres{L}.{m}"])
        P.barrier()

    def phase_moe(l, streams):
        class S_:
            pass
        with ExitStack() as st:
            R = B.sb(st, "moeR", [128, 16 * SEQ], BF16)
            wr32 = B.sb(st, "wr32", [128, 16, NE], F32)
            selm = B.sb(st, "selm", [16, 16, 128], F32)
            pidx = B.sb(st, "pidx", [128, 16], F32)
            B.DMA("sp", wr32[:], wr_in[l].rearrange("(dc p) e -> p dc e", p=128), (), ["wr32"])
            B.DMA("sp", selm[:], cd["selmat"], (), ["selm"])
            B.DMA("sp", pidx[:], cd["pidx"], (), ["pidx"])
            sts = []
            for (L, s) in streams:
                c = S_()
                c.L, c.s = L, s
                c.TT = L // 128
                c.cap = 2 * L // NE
                c.nit = c.cap // 8
                c.JH = (c.cap + 127) // 128
                c.jw = min(c.cap, 128)
                c.NR = NE * c.JH
                c.k = f"m{L}"
                c.xn_tm = B.sb(st, "xn_tm", [128, c.TT, D], BF16)
                c.vals = B.sb(st, "vals", [16, c.cap], F32)
                c.idxu = B.sb(st, "idxu", [16, c.cap], U32)
                c.idxf = B.sb(st, "idxf", [16, c.cap], F32)
                c.idxT = B.sb(st, "idxT", [128, c.JH, NE], F32)
                c.valT = B.sb(st, "valT", [128, c.JH, NE], F32)
                sts.append(c)
            lb = 3
            for c in sts:
                L, TT_, cap, JH, jw, k = c.L, c.TT, c.cap, c.JH, c.jw, c.k
                xn = R[:, 0:16 * L].rearrange("p (a b) -> p a b", b=L)
                with ExitStack() as st2:
                    affT = B.sb(st2, "affT", [16, L], F32)
                    aff = B.sb(st2, "aff", [128, TT_, NE], F32)
                    mx = B.sb(st2, "mx", [128, TT_], F32)
                    with ExitStack() as st2a:
                        if L == SEQ:
                            def f32v(a0, a1, n):
                                v = c.xn_tm[:, a0:a1, :].rearrange("p a b -> p (a b)").bitcast(F32)
                                return v.rearrange("p (n l) -> p n l", n=n) if n > 1 else v
                            nb_bufs = (f32v(0, 4, 2), f32v(4, 8, 2), f32v(12, 14, 1), f32v(8, 12, 2))
                            normmod(st2a, L, c.s, A2, "A2", 48, xn, "xn2", router=(wr32, lb), nbuf=2, bufs=nb_bufs)
                        else:
                            normmod(st2a, L, c.s, A2, "A2", 48, xn, "xn2", router=(wr32, lb), nbuf=1)
                        lg = PS[:, lb, 0:TT_ * 16].rearrange("p (t e) -> p t e", e=16)
                        P.op("dve", lambda e, mx=mx, lg=lg: e.tensor_reduce(mx[:], lg, AX.X, ALU.max), [psk(lb)], ["mx"])
                        B.TT("dve", aff[:], lg, mx[:].unsqueeze(2).to_broadcast([128, TT_, 16]), ALU.subtract, [psk(lb), "mx"], ["aff"])
                        B.ACT(aff[:], aff[:], AF.Exp, ["aff"], ["aff"])
                        P.op("dve", lambda e, mx=mx, aff=aff: e.tensor_reduce(mx[:], aff[:], AX.X, ALU.add), ["aff"], ["mx"])
                        P.op("dve", lambda e, mx=mx: e.reciprocal(mx[:], mx[:]), ["mx"], ["mx"])
                        B.TT("dve", aff[:], aff[:], mx[:].unsqueeze(2).to_broadcast([128, TT_, 16]), ALU.mult, ["aff", "mx"], ["aff"])
                    P.barrier()
                    for g0 in range(0, TT_, 4):
                        bk = B.bank()
                        n = min(4, TT_ - g0)
                        for q in range(n):
                            B.TR(PS[:16, bk, q * 128:(q + 1) * 128], aff[:, g0 + q, :], ident_f[:], ["aff", "ident_f"], [psk(bk)])
                        B.CP("act", affT[:, g0 * 128:(g0 + n) * 128], PS[:16, bk, 0:n * 128], [psk(bk)], ["affT"])
                    for tt in range(TT_):
                        for q4 in range(4):
                            bk = B.bank()
                            pv = PS[:, bk, :].bitcast(BF16)
                            for q in range(4):
                                dc = q4 * 4 + q
                                B.TR(pv[:, q * 128:(q + 1) * 128], xn[:, dc, tt * 128:(tt + 1) * 128], ident_bf[:], [f"xn2.{dc}", "ident_bf"], [psk(bk)])
                            B.CP("act", c.xn_tm[:, tt, q4 * 512:(q4 + 1) * 512], pv[:, 0:512], [psk(bk)], [f"xn_tm{k}"])
                    for it in range(c.nit):
                        sl = slice(it * 8, (it + 1) * 8)
                        P.op("dve", lambda e, sl=sl, c=c, affT=affT: e.max(c.vals[:, sl], affT[:]), ["affT"], [f"vals{k}"])
                        P.op("dve", lambda e, sl=sl, c=c, affT=affT: e.max_index(c.idxu[:, sl], c.vals[:, sl], affT[:]), ["affT", f"vals{k}"], [f"idxu{k}"])
                        if it < c.nit - 1:
                            P.op("dve", lambda e, sl=sl, c=c, affT=affT: e.match_replace(affT[:], c.vals[:, sl], affT[:], -1.0), ["affT", f"vals{k}"], ["affT"])
                    B.CP("dve", c.idxf[:], c.idxu[:], [f"idxu{k}"], [f"idxf{k}"])
                    for hh in range(JH):
                        bk = B.bank()
                        B.TR(PS[:jw, bk, 0:16], c.idxf[:, hh * 128:hh * 128 + jw], ident_f[:16, :16], [f"idxf{k}", "ident_f"], [psk(bk)])
                        B.TR(PS[:jw, bk, 16:32], c.vals[:, hh * 128:hh * 128 + jw], ident_f[:16, :16], [f"vals{k}", "ident_f"], [psk(bk)])
                        B.CP("dve", c.idxT[:jw, hh, :], PS[:jw, bk, 0:16], [psk(bk)], [f"idxT{k}"])
                        B.CP("dve", c.valT[:jw, hh, :], PS[:jw, bk, 16:32], [psk(bk)], [f"valT{k}"])
                P.barrier()
            EG = 2
            CW = sum(c.cap for c in sts)
            offs = []
            o_ = 0
            for c in sts:
                offs.append(o_)
                o_ += c.cap
            Selg = R[:, 0:8192]
            wg = R[:, 8192:16384].rearrange("p (w a b) -> p w a b", w=2, b=256)
            wu = R[:, 16384:24576].rearrange("p (w a b) -> p w a b", w=2, b=256)
            wd = R[:, 24576:32768].rearrange("p (w a b) -> p w a b", w=2, b=512)
            rgs = []
            for si, c in enumerate(sts):
                for hh in range(c.JH):
                    rgs.append((si, hh, offs[si] + hh * 128, c.jw))
            with ExitStack() as st3:
                xsT = B.sb(st3, "xsT", [128, 16, EG, CW], BF16)
                hidT = B.sb(st3, "hidT", [128, 8, CW], BF16)
                sg_ = B.sb(st3, "moesg", [128, 1, CW], F32)
                ysb = B.sb(st3, "ysb", [128, 2, len(rgs), D], BF16)
                for eg in range(NE // EG):
                    for si, c in enumerate(sts):
                        cap, TT_, k = c.cap, c.TT, c.k
                        gw = EG * cap
                        Sg = Selg[:, 0:TT_ * gw].rearrange("p (a b) -> p a b", b=gw)
                        bk = B.bank()
                        for q in range(EG):
                            e_ = eg * EG + q
                            B.MM(PS[:, bk, q * cap:(q + 1) * cap], selm[:, e_, :], c.idxf[:], True, True, ["selm", f"idxf{k}"], [psk(bk)])
                        for tt in range(TT_):
                            B.TS("dve", Sg[:, tt, :], PS[:, bk, :gw], pidx[:, tt:tt + 1], None, ALU.is_equal, None, [psk(bk), "pidx"], ["Selg"])
                        for m in range(16):
                            bk2 = B.bank()
                            for tt in range(TT_):
                                B.MM(PS[:, bk2, :gw], c.xn_tm[:, tt, m * 128:(m + 1) * 128], Sg[:, tt, :], tt == 0, tt == TT_ - 1,
                                     [f"xn_tm{k}", "Selg"], [psk(bk2)])
                            B.CP("act" if m % 2 else "dve", xsT[:, m, :, offs[si]:offs[si] + cap],
                                 PS[:, bk2, :gw].rearrange("p (q c) -> p q c", c=cap), [psk(bk2)], ["xsT"])
                    for q in range(EG):
                        e_ = eg * EG + q
                        gv = wg_in[l, e_].rearrange("(dc p) f -> p dc f", p=128)
                        uv = wu_in[l, e_].rearrange("(dc p) f -> p dc f", p=128)
                        dv = wd_in[l, e_].rearrange("(fc p) d -> p fc d", p=128)
                        for fb_ in range(4):
                            wb_ = fb_ % 2
                            B.DMA("pool", wg[:, wb_], gv[:, :, fb_ * 256:(fb_ + 1) * 256], (), [f"wg{wb_}"])
                            B.DMA("pool", wu[:, wb_], uv[:, :, fb_ * 256:(fb_ + 1) * 256], (), [f"wu{wb_}"])
                            for f2 in range(2):
                                fc = fb_ * 2 + f2
                                bg, bu = B.bank(), B.bank()
                                for dc in range(16):
                                    B.MM(PS[:, bg, :CW], wg[:, wb_, dc, f2 * 128:(f2 + 1) * 128], xsT[:, dc, q, :], dc == 0, dc == 15,
                                         [f"wg{wb_}", "xsT"], [psk(bg)])
                                for dc in range(16):
                                    B.MM(PS[:, bu, :CW], wu[:, wb_, dc, f2 * 128:(f2 + 1) * 128], xsT[:, dc, q, :], dc == 0, dc == 15,
                                         [f"wu{wb_}", "xsT"], [psk(bu)])
                                sb_ = 0
                                B.ACT(sg_[:, sb_, :], PS[:, bg, :CW], AF.Silu, [psk(bg)], [f"moesg{sb_}"])
                                B.TT("dve", hidT[:, fc, :], sg_[:, sb_, :], PS[:, bu, :CW], ALU.mult, [f"moesg{sb_}", psk(bu)], ["hidT"])
                        yb = e_ % 2
                        for db_ in range(4):
                            wb_ = db_ % 2
                            B.DMA("pool", wd[:, wb_], dv[:, :, db_ * 512:(db_ + 1) * 512], (), [f"wd{wb_}"])
                            for ri, (si, hh, co, rows) in enumerate(rgs):
                                bk3 = B.bank()
                                for fc in range(8):
                                    B.MM(PS[:rows, bk3, :], hidT[:, fc, co:co + rows], wd[:, wb_, fc, :], fc == 0, fc == 7,
                                         ["hidT", f"wd{wb_}"], [psk(bk3)])
                                B.CP("act" if ri % 2 else "dve", ysb[:rows, yb, ri, db_ * 512:(db_ + 1) * 512], PS[:rows, bk3, :], [psk(bk3)], [f"ysb{yb}"])
                        for ri, (si, hh, co, rows) in enumerate(rgs):
                            c = sts[si]
                            B.DMA("sp", ys_d[c.L][:, :rows, e_ * c.JH + hh, :].rearrange("m j d -> j m d"),
                                  ysb[:rows, yb, ri, :].rearrange("j (m d) -> j m d", d=128), [f"ysb{yb}"], [f"ys{c.k}.{e_}.{hh}"])
            P.barrier()
            for c in sts:
                L, JH, jw, NR, k = c.L, c.JH, c.jw, c.NR, c.k
                SelT = R[:, 0:2 * NR * 512].rearrange("p (w r c) -> p w r c", w=2, c=512)
                with ExitStack() as st4:
                    ysm = B.sb(st4, "ysm", [128, 2, NR, 128], BF16)
                    xl = B.sb(st4, "xl2", [128, 2, 512], F32)
                    iot = B.sb(st4, "iot", [128, L], F32)
                    B.DMA("sp", iot[:], cd["iota_t"][:, :L], (), ["iot"])
                    for ti, (t0, tw) in enumerate(tiles(L)):
                        sb_ = ti % 2
                        for e_ in range(NE):
                            for hh in range(JH):
                                r_ = e_ * JH + hh
                                B.TS("dve", SelT[:jw, sb_, r_, :tw], iot[:jw, t0:t0 + tw], c.idxT[:jw, hh, e_:e_ + 1], c.valT[:jw, hh, e_:e_ + 1],
                                     ALU.is_equal, ALU.mult, ["iot", f"idxT{k}", f"valT{k}"], [f"SelT{sb_}"])
                        for m in range(16):
                            mb = m % 2
                            B.DMA("sp", ysm[:jw, mb], ys_d[L][m, :jw, 0:NR, :],
                                  [f"ys{k}.{e_}.{hh}" for e_ in range(NE) for hh in range(JH)], [f"ysm{mb}"])
                            B.DMA("sp", xl[:, mb, :tw], xres[L][m * 128:(m + 1) * 128, t0:t0 + tw], [f"xres{L}.{m}"], [f"xl2{mb}"])
                            bk = B.bank()
                            for r_ in range(NR):
                                B.MM(PS[:, bk, :tw], ysm[:jw, mb, r_, :], SelT[:jw, sb_, r_, :tw], r_ == 0, r_ == NR - 1,
                                     [f"ysm{mb}", f"SelT{sb_}"], [psk(bk)])
                            B.STT(xl[:, mb, :tw], PS[:, bk, :tw], modT[:, 80 + m, c.s:c.s + 1], xl[:, mb, :tw], ALU.mult, ALU.add,
                                  [psk(bk), "modT", f"xl2{mb}"], [f"xl2{mb}"])
                            B.DMA("act", xres[L][m * 128:(m + 1) * 128, t0:t0 + tw], xl[:, mb, :tw], [f"xl2{mb}"], [f"xres{L}.{m}"])
                P.barrier()

    def phase_final():
        L = SEQ
        with ExitStack() as st:
            xld = B.sb(st, "fxld", [128, 2, L], F32)
            sq = B.sb(st, "fsq", [128, 2, L], F32)
            rstd = B.sb(st, "frstd", [128, L], F32)
            for dc in range(16):
                b_ = dc % 2
                B.DMA("sp", xld[:, b_, :], xres[L][dc * 128:(dc + 1) * 128, :], [f"xres{L}.{dc}"], [f"fxld{b_}"])
                B.ACT(sq[:, b_, :], xld[:, b_, :], AF.Square, [f"fxld{b_}"], [f"fsq{b_}"])
                for i, (t0, tw) in enumerate(tiles(L)):
                    B.MM(PS[:, 4 + i, :tw], ones_f[:], sq[:, b_, t0:t0 + tw], dc == 0, dc == 15, [f"fsq{b_}", "ones_f"], [psk(4 + i)])
            for i, (t0, tw) in enumerate(tiles(L)):
                B.ACT(rstd[:, t0:t0 + tw], PS[:, 4 + i, :tw], AF.Ln, [psk(4 + i)], ["frstd"], bias=epsc[:, 0:1], scale=1.0 / D)
            B.ACT(rstd[:], rstd[:], AF.Exp, ["frstd"], ["frstd"], scale=-0.5)
            for dc in range(16):
                b_ = dc % 2
                B.DMA("sp", xld[:, b_, :], xres[L][dc * 128:(dc + 1) * 128, :], [f"xres{L}.{dc}"], [f"fxld{b_}"])
                B.STT(sq[:, b_, :], xld[:, b_, :], gfin[:, dc:dc + 1], rstd[:], ALU.mult, ALU.mult, [f"fxld{b_}", "gfin", "frstd"], [f"fsq{b_}"])
                B.DMA("act", out_d[dc * 128:(dc + 1) * 128, :], sq[:, b_, :], [f"fsq{b_}"], [f"out.{dc}"])
        P.barrier()

    nstage = 0

    def go():
        nonlocal nstage
        nstage += 1
        return nstage <= stage and nstage not in skip

    for l in range(DEPTH):
        last = l == DEPTH - 1
        wv = w_in[l].rearrange("(dc p) n -> p dc n", p=128)
        if go():
            phase_mod(l)
        if go():
            with ExitStack() as sx:
                XN = B.sb(sx, "XNc", [128, 16, CTX], BF16)
                with ExitStack() as st:
                    normmod(st, CTX, 1, A1, "A1", 0, XN, "XN")
                P.barrier()
                parts = DBG.get("ctx_parts", ("hgrn", "fourier", "hyena", "outproj"))
                if "hgrn" in parts:
                    phase_hgrn(l, CTX, XN, "XN", wv, need_out=(not last) and DBG.get("ctx_need_out", True), first=True)
                if not last:
                    if "fourier" in parts:
                        phase_fourier(l, CTX, XN, "XN", wv)
                    if "hyena" in parts:
                        phase_hyfilter(l, CTX)
                        phase_hyena(l, CTX, XN, "XN", wv)
            if not last and "outproj" in parts:
                phase_outproj(l, CTX, 1)
        with ExitStack() as sx:
            XN = B.sb(sx, "XNx", [128, 16, SEQ], BF16)
            if go():
                with ExitStack() as st:
                    normmod(st, SEQ, 0, A1, "A1", 0, XN, "XN")
                P.barrier()
            if go():
                phase_fourier(l, SEQ, XN, "XN", wv)
            if go():
                phase_hyfilter(l, SEQ)
                phase_hyena(l, SEQ, XN, "XN", wv)
            if go():
                phase_hgrn(l, SEQ, XN, "XN", wv, need_out=True, first=False)
        if go():
            phase_outproj(l, SEQ, 0)
        if go():
            phase_moe(l, [(CTX, 1)] if DBG.get("moe_ctx_only") else [(SEQ, 0)] + ([] if last else [(CTX, 1)]))
        go()
    if go():
        phase_final()
    else:
        with ExitStack() as st:
            z = B.sb(st, "zz", [128, SEQ], F32)
            B.MS("dve", z[:], 0.0, ["zz"])
            for dc in range(16):
                B.DMA("sp", out_d[dc * 128:(dc + 1) * 128, :], z[:], ["zz"], [f"out.{dc}"])
    info = P.emit()
    info["names"] = P.names
    info["sites"] = [o["site"] for o in P.ops]
    info["in_shapes"] = {k: tuple(v.shape) for k, v in B.inputs.items()}
    B.st.close()
    return nc, consts, info


_PROG = {}


def prep_inputs(inp, b, consts):
    m = {}
    m["xT"] = np.ascontiguousarray(inp["x"][b].T)
    m["ctxT"] = np.ascontiguousarray(inp["ctx"][b].T)
    cc = np.stack([pmaj(inp["c"][b], 16), pmaj(inp["c_ctx"], 16)], axis=-1)
    m["cT"] = np.ascontiguousarray(cc)
    m["gmix"] = pmaj(inp["norm_mix_g"], 16)
    m["gffn"] = pmaj(inp["norm_ffn_g"], 16)
    m["gfin"] = pmaj(inp["final_norm_g"], 16)
    m["bmod"] = pmaj(inp["b_mod"], 96)
    m["hycw"] = pmaj(inp["hy_conv_w"], 12)
    m["hycb"] = pmaj(inp["hy_conv_b"], 12)
    m["hyb"] = np.ascontiguousarray(np.stack([inp["hy_b1"], inp["hy_b2"], inp["hy_b3"]], axis=-1).transpose(1, 0, 2)).astype(np.float32)
    m["hyfr"] = np.ascontiguousarray(np.asarray(inp["hy_freq"], np.float32).transpose(2, 0, 1))
    m["hydb"] = pmaj(inp["hy_bias"], 4)
    m["hglb"] = pmaj(inp["hg_lb"], 8)
    m["hgg"] = pmaj(inp["hg_norm_g"], 8)
    for k in ("w_mod", "w_in", "w_out", "w_fnet", "hy_w1", "hy_w2", "hy_w3", "hy_w_out", "w_router", "w_gate", "w_up", "w_down"):
        m[k] = np.ascontiguousarray(np.asarray(inp[k], np.float32))
    m.update(consts)
    return m


def kernel(**inputs):
    if "full" not in _PROG:
        _PROG["full"] = build()
    nc, consts, info = _PROG["full"]
    n = 8
    in_maps = [prep_inputs(inputs, b, consts) for b in range(n)]
    res = run_bass_kernel_spmd(nc, in_maps, core_ids=list(range(n)))
    out = np.stack([np.ascontiguousarray(res.results[b]["outT"].T) for b in range(n)], axis=0)
    return out.astype(np.float32)
```
